# Optimizing a Trainium2 kernel written in Bass

```python
import jax, jax.numpy as jnp
from jax import lax
import numpy as np

D_MODEL = 2048
BATCH = 8
SEQ = 4096
DEPTH = 1

HEAD_DIM = 128
ROPE_THETA = 10000.0
NSA_HEADS = 8
NSA_KV_GROUPS = 2
NSA_REP = NSA_HEADS // NSA_KV_GROUPS
CMP_LEN = 32
CMP_STRIDE = 16
CMP_HIDDEN = 256
SEL_LEN = 64
SEL_TOPK = 16
WIN_LEN = 512
SEL_QUERY_CHUNK = 64
BAND_BLOCK = 128
DIL_CONFIGS = ((128, 1), (512, 4), (2048, 16))
N_DIL = 3
DIL_HEADS = 4
N_EXPERTS = 32
TOP_K = 4
D_EXPERT = D_MODEL
SWIGLU_LIMIT = 7.0
SWIGLU_ALPHA = 1.702
MOE_BLOCK = 128
DN_ALPHA = (2.0 * DEPTH) ** 0.25
DN_BETA = (8.0 * DEPTH) ** -0.25
LN_EPS = 1e-5
NEG = -1e30
FORCE = 1e9
NSA_Q_W = NSA_HEADS * HEAD_DIM
NSA_KV_W = NSA_KV_GROUPS * HEAD_DIM
NSA_GATE_W = 3 * NSA_HEADS
DIL_GROUP_W = DIL_HEADS * HEAD_DIM
IN_W = NSA_Q_W + 6 * NSA_KV_W + NSA_GATE_W + 3 * N_DIL * DIL_GROUP_W + 2 * D_MODEL

kernel_name = "hybrid_nsa_dilated_moe_deepnorm"


def layer_norm(x, g, b):
    xf = x.astype(jnp.float32)
    mu = xf.mean(-1, keepdims=True)
    var = jnp.square(xf - mu).mean(-1, keepdims=True)
    return ((xf - mu) * lax.rsqrt(var + LN_EPS) * g + b).astype(x.dtype)


def rope_tables(seq):
    pos = jnp.arange(seq, dtype=jnp.float32)
    inv = ROPE_THETA ** (-jnp.arange(0, HEAD_DIM, 2, dtype=jnp.float32) / HEAD_DIM)
    ang = pos[:, None] * inv[None, :]
    return jnp.cos(ang), jnp.sin(ang)


def apply_rope(x, cos, sin):
    half = HEAD_DIM // 2
    c = cos[:, None, :].astype(x.dtype)
    s = sin[:, None, :].astype(x.dtype)
    x1, x2 = x[..., :half], x[..., half:]
    return jnp.concatenate([x1 * c - x2 * s, x2 * c + x1 * s], axis=-1)


def masked_softmax(scores, mask):
    s = jnp.where(mask, scores, NEG)
    m = s.max(-1, keepdims=True)
    p = jnp.where(mask, jnp.exp(s - m), 0.0)
    den = p.sum(-1, keepdims=True)
    safe = jnp.where(den > 0, den, 1.0)
    return p / safe, (m + jnp.log(safe))[..., 0]


def banded_attention(q, k, v, max_dist, blk):
    B, L, G, R, Dh = q.shape
    n_prev = -(-max_dist // blk)
    nb = -(-L // blk)
    pad = nb * blk - L
    q = jnp.pad(q, ((0, 0), (0, pad), (0, 0), (0, 0), (0, 0)))
    k = jnp.pad(k, ((0, 0), (n_prev * blk, pad), (0, 0), (0, 0)))
    v = jnp.pad(v, ((0, 0), (n_prev * blk, pad), (0, 0), (0, 0)))
    qb = q.reshape(B, nb, blk, G, R, Dh)
    kw = jnp.concatenate([k[:, i * blk:(i + nb) * blk].reshape(B, nb, blk, G, Dh) for i in range(n_prev + 1)], axis=2)
    vw = jnp.concatenate([v[:, i * blk:(i + nb) * blk].reshape(B, nb, blk, G, Dh) for i in range(n_prev + 1)], axis=2)
    nk = (n_prev + 1) * blk
    qi = jnp.arange(blk)[:, None]
    ki = jnp.arange(nk)[None, :]
    diff = n_prev * blk + qi - ki
    k_pos = (jnp.arange(nb)[:, None, None] - n_prev) * blk + ki[None]
    mask = (diff >= 0)[None] & (diff <= max_dist)[None] & (k_pos >= 0)
    s = jnp.einsum("bnqgrd,bnkgd->bngrqk", qb, kw).astype(jnp.float32) * (Dh ** -0.5)
    p, lse = masked_softmax(s, mask[None, :, None, None])
    o = jnp.einsum("bngrqk,bnkgd->bnqgrd", p.astype(vw.dtype), vw)
    o = o.reshape(B, nb * blk, G, R, Dh)[:, :L]
    lse = lse.transpose(0, 1, 4, 2, 3).reshape(B, nb * blk, G, R)[:, :L]
    return o, lse


def nsa_attention(q, k_c, v_c, k_s, v_s, k_w, v_w, gate_logits, pos_k, pos_v, ck_w1, ck_w2, cv_w1, cv_w2):
    B, S, G, R, Dh = q.shape
    scale = Dh ** -0.5
    t = jnp.arange(S)
    n_cmp = (S - CMP_LEN) // CMP_STRIDE + 1
    c_start = jnp.arange(n_cmp) * CMP_STRIDE
    idx = c_start[:, None] + jnp.arange(CMP_LEN)[None, :]

    def compress(src, pos, w1, w2):
        blocks = src[:, idx] + pos[None, None, :, None, :]
        flat = blocks.transpose(0, 1, 3, 2, 4).reshape(B, n_cmp, G, CMP_LEN * Dh)
        return jax.nn.gelu(flat @ w1) @ w2

    kc = compress(k_c, pos_k, ck_w1, ck_w2)
    vc = compress(v_c, pos_v, cv_w1, cv_w2)
    s_cmp = jnp.einsum("bsgrd,bcgd->bgrsc", q, kc).astype(jnp.float32) * scale
    mask_cmp = (c_start + CMP_LEN - 1)[None, :] <= t[:, None]
    p_cmp, _ = masked_softmax(s_cmp, mask_cmp)
    o_cmp = jnp.einsum("bgrsc,bcgd->bsgrd", p_cmp.astype(vc.dtype), vc)
    n_slc = S // SEL_LEN
    j = jnp.arange(n_slc)
    overlap = (c_start[:, None] < (j[None, :] + 1) * SEL_LEN) & (c_start[:, None] + CMP_LEN > j[None, :] * SEL_LEN)
    imp = jnp.einsum("bgrsc,cj->bgsj", p_cmp, overlap.astype(jnp.float32))
    cur = t // SEL_LEN
    forced = (j[None, :] == 0) | (j[None, :] == cur[:, None]) | (j[None, :] == cur[:, None] - 1)
    future = j[None, :] > cur[:, None]
    imp = jnp.where(forced, FORCE, jnp.where(future, NEG, imp))
    n_sel = min(SEL_TOPK, n_slc)
    _, sel = lax.top_k(imp, n_sel)
    kb = k_s.reshape(B, n_slc, SEL_LEN, G, Dh).transpose(0, 3, 1, 2, 4)
    vb = v_s.reshape(B, n_slc, SEL_LEN, G, Dh).transpose(0, 3, 1, 2, 4)
    qc_len = SEL_QUERY_CHUNK
    nq = S // qc_len
    q_ch = q.reshape(B, nq, qc_len, G, R, Dh).transpose(1, 0, 2, 3, 4, 5)
    sel_ch = sel.reshape(B, G, nq, qc_len, n_sel).transpose(2, 0, 1, 3, 4)
    pos_ch = t.reshape(nq, qc_len)
    bi = jnp.arange(B)[:, None, None, None]
    gi = jnp.arange(G)[None, :, None, None]
    nk = n_sel * SEL_LEN

    def sel_block(args):
        qc, sc, pc = args
        kg = kb[bi, gi, sc].reshape(B, G, qc_len, nk, Dh)
        vg = vb[bi, gi, sc].reshape(B, G, qc_len, nk, Dh)
        kpos = (sc[..., None] * SEL_LEN + jnp.arange(SEL_LEN)).reshape(B, G, qc_len, nk)
        mask = (kpos <= pc[None, None, :, None])[:, :, None]
        s = jnp.einsum("bqgrd,bgqkd->bgrqk", qc, kg).astype(jnp.float32) * scale
        p, _ = masked_softmax(s, mask)
        return jnp.einsum("bgrqk,bgqkd->bqgrd", p.astype(vg.dtype), vg)

    o_slc = lax.map(sel_block, (q_ch, sel_ch, pos_ch))
    o_slc = o_slc.transpose(1, 0, 2, 3, 4, 5).reshape(B, S, G, R, Dh)
    o_win, _ = banded_attention(q, k_w, v_w, WIN_LEN - 1, BAND_BLOCK)
    g = jax.nn.sigmoid(gate_logits)
    return g[..., 0:1] * o_cmp + g[..., 1:2] * o_slc + g[..., 2:3] * o_win


def dilated_attention(qs, ks, vs):
    outs, lses = [], []
    for (w, d), q, k, v in zip(DIL_CONFIGS, qs, ks, vs):
        B, S, H, Dh = q.shape
        ls = S // d

        def to_sub(a):
            return a.reshape(B, ls, d, H, Dh).transpose(0, 2, 1, 3, 4).reshape(B * d, ls, H, Dh)

        o, lse = banded_attention(to_sub(q)[:, :, :, None], to_sub(k), to_sub(v), w // d, BAND_BLOCK)
        outs.append(o[:, :, :, 0].reshape(B, d, ls, H, Dh).transpose(0, 2, 1, 3, 4).reshape(B, S, H, Dh))
        lses.append(lse[..., 0].reshape(B, d, ls, H).transpose(0, 2, 1, 3).reshape(B, S, H))
    wts = jax.nn.softmax(jnp.stack(lses), axis=0)
    return jnp.einsum("gbsh,gbshd->bshd", wts.astype(outs[0].dtype), jnp.stack(outs))


def hybrid_mixer(x, w_in, b_in, pos_k, pos_v, ck_w1, ck_w2, cv_w1, cv_w2, w_br_nsa, w_br_dil, w_out):
    B, S, _ = x.shape
    proj = x @ w_in + b_in
    sizes = [NSA_Q_W] + [NSA_KV_W] * 6 + [NSA_GATE_W] + [DIL_GROUP_W] * (3 * N_DIL) + [D_MODEL, D_MODEL]
    offs, acc = [], 0
    for sz in sizes[:-1]:
        acc += sz
        offs.append(acc)
    parts = jnp.split(proj, offs, axis=-1)
    cos, sin = rope_tables(S)

    def heads(a):
        return a.reshape(B, S, -1, HEAD_DIM)

    def rope(a):
        return apply_rope(heads(a), cos, sin)

    q_n = rope(parts[0]).reshape(B, S, NSA_KV_GROUPS, NSA_REP, HEAD_DIM)
    k_c, k_s, k_w = rope(parts[1]), rope(parts[3]), rope(parts[5])
    v_c, v_s, v_w = heads(parts[2]), heads(parts[4]), heads(parts[6])
    gl = parts[7].reshape(B, S, NSA_KV_GROUPS, NSA_REP, 3)
    o_nsa = nsa_attention(q_n, k_c, v_c, k_s, v_s, k_w, v_w, gl, pos_k, pos_v, ck_w1, ck_w2, cv_w1, cv_w2)
    dil = parts[8:8 + 3 * N_DIL]
    o_dil = dilated_attention([rope(dil[3 * i]) for i in range(N_DIL)],
                              [rope(dil[3 * i + 1]) for i in range(N_DIL)],
                              [heads(dil[3 * i + 2]) for i in range(N_DIL)])
    gate_a, gate_b = parts[8 + 3 * N_DIL], parts[9 + 3 * N_DIL]
    y_a = o_nsa.reshape(B, S, -1) @ w_br_nsa
    y_b = o_dil.reshape(B, S, -1) @ w_br_dil
    merged = jax.nn.sigmoid(gate_a) * y_a + jax.nn.sigmoid(gate_b) * y_b
    return merged @ w_out


def clamped_swiglu(hu):
    glu, lin = jnp.split(hu, 2, axis=-1)
    glu = jnp.minimum(glu, SWIGLU_LIMIT)
    lin = jnp.clip(lin, -SWIGLU_LIMIT, SWIGLU_LIMIT)
    return glu * jax.nn.sigmoid(SWIGLU_ALPHA * glu) * (lin + 1.0)


def moe(h, w_router, b_router, w_up, b_up, w_down, b_down):
    B, S, D = h.shape
    T = B * S
    hf = h.reshape(T, D)
    logits = (hf @ w_router + b_router).astype(jnp.float32)
    top_vals, top_idx = lax.top_k(logits, TOP_K)
    gates = jax.nn.softmax(top_vals, axis=-1)
    A = T * TOP_K
    e_flat = top_idx.reshape(A)
    tok_flat = jnp.arange(A) // TOP_K
    w_flat = gates.reshape(A)
    order = jnp.argsort(e_flat)
    e_sorted, tok_sorted, w_sorted = e_flat[order], tok_flat[order], w_flat[order]
    counts = jnp.bincount(e_flat, length=N_EXPERTS)
    starts = jnp.cumsum(counts) - counts
    padded = (counts + MOE_BLOCK - 1) // MOE_BLOCK * MOE_BLOCK
    pad_end = jnp.cumsum(padded)
    pad_start = pad_end - padded
    dest = pad_start[e_sorted] + (jnp.arange(A) - starts[e_sorted])
    P = A + N_EXPERTS * MOE_BLOCK
    n_blk = P // MOE_BLOCK
    buf_tok = jnp.full((P,), T, dtype=jnp.int32).at[dest].set(tok_sorted.astype(jnp.int32))
    buf_w = jnp.zeros((P,), jnp.float32).at[dest].set(w_sorted)
    blk_expert = jnp.minimum(jnp.searchsorted(pad_end, jnp.arange(n_blk) * MOE_BLOCK, side="right"), N_EXPERTS - 1)
    h_pad = jnp.concatenate([hf, jnp.zeros((1, D), hf.dtype)], axis=0)
    xb = h_pad[buf_tok].reshape(n_blk, MOE_BLOCK, D)

    def expert_block(args):
        xblk, e = args
        hu = xblk @ w_up[e] + b_up[e]
        return clamped_swiglu(hu) @ w_down[e] + b_down[e]

    yb = lax.map(expert_block, (xb, blk_expert))
    y = yb.reshape(P, D) * buf_w[:, None].astype(yb.dtype)
    out = jax.ops.segment_sum(y, buf_tok, num_segments=T + 1)[:T]
    return out.reshape(B, S, D)


def setup_inputs(seed: int = 0) -> dict:
    key = jax.random.key(seed)
    ks = jax.random.split(key, 22)
    L = DEPTH

    def nrm(k, shape, scale):
        return jax.random.normal(k, shape, jnp.float32) * scale

    return {
        "x": nrm(ks[0], (BATCH, SEQ, D_MODEL), 1.0),
        "w_in": nrm(ks[1], (L, D_MODEL, IN_W), D_MODEL ** -0.5),
        "b_in": nrm(ks[2], (L, IN_W), 0.01),
        "cmp_pos_k": nrm(ks[3], (L, CMP_LEN, HEAD_DIM), 0.1),
        "cmp_pos_v": nrm(ks[4], (L, CMP_LEN, HEAD_DIM), 0.1),
        "cmp_k_w1": nrm(ks[5], (L, CMP_LEN * HEAD_DIM, CMP_HIDDEN), (CMP_LEN * HEAD_DIM) ** -0.5),
        "cmp_k_w2": nrm(ks[6], (L, CMP_HIDDEN, HEAD_DIM), CMP_HIDDEN ** -0.5),
        "cmp_v_w1": nrm(ks[7], (L, CMP_LEN * HEAD_DIM, CMP_HIDDEN), (CMP_LEN * HEAD_DIM) ** -0.5),
        "cmp_v_w2": nrm(ks[8], (L, CMP_HIDDEN, HEAD_DIM), CMP_HIDDEN ** -0.5),
        "w_br_nsa": nrm(ks[9], (L, NSA_HEADS * HEAD_DIM, D_MODEL), (NSA_HEADS * HEAD_DIM) ** -0.5),
        "w_br_dil": nrm(ks[10], (L, DIL_HEADS * HEAD_DIM, D_MODEL), (DIL_HEADS * HEAD_DIM) ** -0.5),
        "w_out": nrm(ks[11], (L, D_MODEL, D_MODEL), D_MODEL ** -0.5 * DN_BETA),
        "ln1_g": 1.0 + nrm(ks[12], (L, D_MODEL), 0.02),
        "ln1_b": nrm(ks[13], (L, D_MODEL), 0.02),
        "w_router": nrm(ks[14], (L, D_MODEL, N_EXPERTS), D_MODEL ** -0.5),
        "b_router": nrm(ks[15], (L, N_EXPERTS), 0.01),
        "w_up": nrm(ks[16], (L, N_EXPERTS, D_MODEL, 2 * D_EXPERT), D_MODEL ** -0.5),
        "b_up": nrm(ks[17], (L, N_EXPERTS, 2 * D_EXPERT), 0.01),
        "w_down": nrm(ks[18], (L, N_EXPERTS, D_EXPERT, D_MODEL), D_EXPERT ** -0.5 * DN_BETA),
        "b_down": nrm(ks[19], (L, N_EXPERTS, D_MODEL), 0.01),
        "ln2_g": 1.0 + nrm(ks[20], (L, D_MODEL), 0.02),
        "ln2_b": nrm(ks[21], (L, D_MODEL), 0.02),
    }


def reference(x, w_in, b_in, cmp_pos_k, cmp_pos_v, cmp_k_w1, cmp_k_w2, cmp_v_w1, cmp_v_w2,
              w_br_nsa, w_br_dil, w_out, ln1_g, ln1_b, w_router, b_router, w_up, b_up,
              w_down, b_down, ln2_g, ln2_b):
    h = x
    for l in range(DEPTH):
        mix = hybrid_mixer(h, w_in[l], b_in[l], cmp_pos_k[l], cmp_pos_v[l], cmp_k_w1[l], cmp_k_w2[l],
                           cmp_v_w1[l], cmp_v_w2[l], w_br_nsa[l], w_br_dil[l], w_out[l])
        h = layer_norm(DN_ALPHA * h + mix, ln1_g[l], ln1_b[l])
        ffn = moe(h, w_router[l], b_router[l], w_up[l], b_up[l], w_down[l], b_down[l])
        h = layer_norm(DN_ALPHA * h + ffn, ln2_g[l], ln2_b[l])
    return h
```

```python
from contextlib import ExitStack
import numpy as np
import ml_dtypes
import concourse.bass as bass
import concourse.mybir as mybir
from concourse.bass_utils import run_bass_kernel_spmd

F32 = mybir.dt.float32
BF16 = mybir.dt.bfloat16
I32 = mybir.dt.int32
U32 = mybir.dt.uint32
AF = mybir.ActivationFunctionType
ALU = mybir.AluOpType
AX = mybir.AxisListType

D = 2048
KC = 16
IN_W = 11288
NEGB = -30000.0
SCALE = 128 ** -0.5
DILS = (1, 4, 16)
DN_ALPHA = 2.0 ** 0.25
LN_EPS = 1e-5
NE = 32
CAPB = 5
DBG_BR = "csw"
SAME_ENG_SYNC = True


class Buf:
    __slots__ = ("name", "w", "r")

    def __init__(self, name=""):
        self.name = name
        self.w = None
        self.r = {}


class DSem:
    __slots__ = ("key", "sem", "cnt")


class Sched:
    ENG = ("pe", "act", "dve", "pool", "sp")

    def __init__(self, nc):
        self.nc = nc
        self.prog = {e: [] for e in self.ENG}
        self.cnt = {e: 0 for e in self.ENG}
        self.sems = {}
        self.seen = {e: {} for e in self.ENG}
        self.dsems = []
        for e in ("pe", "act", "dve", "pool"):
            self.sems[e] = nc.alloc_semaphore("prog_" + e)

    def dsem(self):
        d = DSem()
        d.key = "d%d" % len(self.dsems)
        d.sem = self.nc.alloc_semaphore("dma_" + d.key)
        d.cnt = 0
        self.sems[d.key] = d.sem
        self.dsems.append(d)
        return d

    def _wait(self, eng, k, i):
        seen = self.seen[eng]
        if k == eng and (eng == "pe" or not SAME_ENG_SYNC):
            return
        if i > 0 and seen.get(k, 0) < i:
            seen[k] = i
            sem = self.sems[k]
            self.prog[eng].append(lambda e, sem=sem, i=i: e.wait_ge(sem, i))

    def _waits(self, eng, reads, writes, same=True):
        need = {}

        def add(dep):
            if dep is not None and (same or dep[0] != eng) and need.get(dep[0], 0) < dep[1]:
                need[dep[0]] = dep[1]
        for b in reads:
            add(b.w)
        for b in writes:
            add(b.w)
            for k, i in b.r.items():
                add((k, i))
        for k, i in need.items():
            self._wait(eng, k, i)

    def op(self, eng, fn, reads=(), writes=(), same=True):
        self._waits(eng, reads, writes, same)
        self.cnt[eng] += 1
        idx = self.cnt[eng]
        sem = self.sems[eng]
        self.prog[eng].append(lambda e, fn=fn, sem=sem: fn(e).then_inc(sem, 1))
        for b in reads:
            if b.r.get(eng, 0) < idx:
                b.r[eng] = idx
        for b in writes:
            b.w = (eng, idx)
            b.r = {}

    def dma(self, q, fn, ds, reads=(), writes=()):
        self._waits(q, reads, writes)
        ds.cnt += 16
        idx = ds.cnt
        sem = ds.sem
        self.prog[q].append(lambda e, fn=fn, sem=sem: fn(e).then_inc(sem, 16))
        for b in reads:
            if b.r.get(ds.key, 0) < idx:
                b.r[ds.key] = idx
        for b in writes:
            b.w = (ds.key, idx)
            b.r = {}

    def barrier(self):
        for e in self.ENG:
            for k in ("pe", "act", "dve", "pool"):
                self._wait(e, k, self.cnt[k])
            for d in self.dsems:
                self._wait(e, d.key, d.cnt)

    def emit(self):
        nc = self.nc
        prog = self.prog
        with nc.Block() as block:
            @block.tensor
            def _(e):
                for f in prog["pe"]:
                    f(e)

            @block.scalar
            def _(e):
                for f in prog["act"]:
                    f(e)

            @block.vector
            def _(e):
                for f in prog["dve"]:
                    f(e)

            @block.gpsimd
            def _(e):
                for f in prog["pool"]:
                    f(e)

            @block.sync
            def _(e):
                for f in prog["sp"]:
                    f(e)
        self.prog = {e: [] for e in self.ENG}


def fm_jobs():
    jobs = []
    for hd in range(8):
        jobs.append(("rope", hd * 128, 128, 1, hd))
    for g in range(2):
        jobs.append(("rope", 1024 + g * 128, 128, 1, 8 + g))
    for g in range(2):
        jobs.append(("plain", 1280 + g * 128, 128, 1, 10 + g))
    for g in range(2):
        jobs.append(("rope", 1536 + g * 128, 128, 1, 12 + g))
    for g in range(2):
        jobs.append(("rope", 2048 + g * 128, 128, 1, 14 + g))
    for gi in range(3):
        base = 2584 + gi * 1536
        for hd in range(4):
            jobs.append(("rope", base + hd * 128, 128, DILS[gi], 16 + gi * 4 + hd))
        for hd in range(4):
            jobs.append(("rope", base + 512 + hd * 128, 128, DILS[gi], 28 + gi * 4 + hd))
    for i in range(16):
        jobs.append(("sig", 7192 + i * 128, 128, 1, 40 + i))
    for i in range(16):
        jobs.append(("sig", 9240 + i * 128, 128, 1, 56 + i))
    jobs.append(("gl", 2560, 24, 1, 72))
    return jobs


NPF = 72


def host_consts(S):
    c = {}
    c["identb"] = np.eye(128, dtype=np.float32).astype(ml_dtypes.bfloat16)
    c["identf"] = np.eye(128, dtype=np.float32)
    pos = np.arange(S, dtype=np.float32)
    inv = (np.float32(10000.0) ** (-np.arange(0, 128, 2, dtype=np.float32) / np.float32(128))).astype(np.float32)
    ang = (pos[:, None] * inv[None, :]).astype(np.float32)
    cos = np.cos(ang).astype(np.float32).T
    sin = np.sin(ang).astype(np.float32).T
    c["cosT"] = np.ascontiguousarray(np.concatenate([cos, cos], 0))
    c["sinT"] = np.ascontiguousarray(np.concatenate([sin, -sin], 0))
    NSLC = S // 64
    NCMP = (S - 32) // 16 + 1
    bf = ml_dtypes.bfloat16
    sidx = np.arange(S)
    c["Eall"] = (sidx[None, :] // 64 == np.arange(NSLC)[:, None]).astype(np.float32).astype(bf)
    sl = np.arange(128)[:, None]
    tl = np.arange(512)[None, :]

    def band(lo, hi, dlt):
        v = tl - dlt - sl
        return np.where((v >= lo) & (v <= hi), 0.0, NEGB).astype(np.float32)
    tiles = [band(0, 1 << 30, 128 * k) for k in range(4)]
    tiles += [band(0, 511, -512 + 128 * k) for k in range(8)]
    tiles += [band(0, 128, -128 + 128 * k) for k in range(5)]
    c["BT"] = np.ascontiguousarray(np.stack(tiles, 1)).astype(bf)
    cidx = np.arange(256)
    cm = np.where((16 * cidx[:, None] + 31 <= sidx[None, :]) & (cidx[:, None] < NCMP), 0.0, NEGB).astype(np.float32)
    c["CMB"] = np.ascontiguousarray(cm.reshape(2, 128, S).transpose(1, 0, 2)).astype(bf)
    j = np.arange(NSLC)[None, :]
    cur = (sidx // 64)[:, None]
    forced = (j == 0) | (j == cur) | (j == cur - 1)
    future = j > cur
    c["KEEP"] = np.where(forced | future, 0.0, 1.0).astype(np.float32)
    c["ADD"] = np.where(forced, 1e9, np.where(future, -1e30, 0.0)).astype(np.float32)
    cs = cidx[:, None] * 16
    ov = (cs < (j + 1) * 64) & (cs + 32 > j * 64) & (cidx[:, None] < NCMP)
    c["ovl"] = ov.astype(np.float32).astype(bf)
    p_ = np.arange(128)
    c["ustr"] = (p_[:, None] < p_[None, :]).astype(np.float32).astype(bf)
    c["ebase"] = np.tile((np.arange(NE, dtype=np.float32) * (CAPB * 128))[None, :], (128, 1)).astype(np.float32)
    return c


CONST_SPECS = {
    "identb": ([128, 128], BF16), "identf": ([128, 128], F32),
    "cosT": ([128, None], F32), "sinT": ([128, None], F32),
    "Eall": ([-64, None], BF16), "BT": ([128, 17, 512], BF16), "CMB": ([128, 2, None], BF16),
    "KEEP": ([None, -64], F32), "ADD": ([None, -64], F32), "ovl": ([256, -64], BF16),
    "ustr": ([128, 128], BF16), "ebase": ([128, NE], F32),
}


def build(S, upto=99, debug=False, ep=False):
    nc = bass.Bass("TRN2", target_bir_lowering=False)
    okind = "ExternalOutput" if debug else "Internal"

    def din(name, shape, dt=F32):
        return nc.dram_tensor(name, shape, dt, kind="ExternalInput").ap()

    def dscr(name, shape, dt):
        return nc.dram_tensor(name, shape, dt, kind=okind).ap()

    x = din("x", [S, D])
    w_in = din("w_in", [D, IN_W])
    b_fm = din("b_fm", [128, NPF + 1])
    b_tm = din("b_tm", [1, 2048])
    cst = {}
    for k, (shp, dt) in CONST_SPECS.items():
        cst[k] = din(k, [S if s is None else (S // 64 if s == -64 else s) for s in shp], dt)
    PF = dscr("PF", [NPF * 128, S], BF16)
    G = dscr("G", [24, S], F32)
    PTn = dscr("PTn", [S, 512], BF16)
    PTd = [dscr("PTd%d" % g, [S, 512], BF16) for g in range(3)]
    out = nc.dram_tensor("out", [S, D], F32, kind="ExternalOutput").ap()

    sch = Sched(nc)
    ps = [nc.alloc_psum_tensor("ps%d" % i, [128, 512], F32).ap() for i in range(8)]
    psb = [Buf("ps%d" % i) for i in range(8)]
    bPF, bG, bPTn = Buf("PF"), Buf("G"), Buf("PTn")
    bPTd = [Buf("PTd%d" % g) for g in range(3)]

    def stage1():
        with ExitStack() as es:
            def sb(name, shape, dt):
                return es.enter_context(nc.sbuf_tensor("s1_" + name, shape, dt))
            HT = min(S, 2048)
            NH = S // HT
            xT = sb("xT", [128, KC, HT], BF16)
            xb = [sb("xb%d" % i, [128, D], BF16) for i in range(2)]
            identb = sb("identb", [128, 128], BF16)
            cosT = sb("cosT", [128, HT], F32)
            sinT = sb("sinT", [128, HT], F32)
            bfm = sb("bfm", [128, NPF + 1], F32)
            btm = sb("btm", [128, 2048], F32)
            wt = [sb("wt%d" % i, [128, KC, 512], BF16) for i in range(2)]
            stg = [sb("stg%d" % i, [128, HT], BF16) for i in range(2)]
            stgG = sb("stgG", [24, HT], F32)
            stgT = [sb("stgT%d" % i, [128, 512], BF16) for i in range(2)]
            tA = [sb("tA%d" % i, [128, 512], F32) for i in range(2)]
            t1 = [sb("t1%d" % i, [128, 512], F32) for i in range(2)]
            t2 = [sb("t2%d" % i, [128, 512], F32) for i in range(2)]
            b_xT = [Buf() for _ in range(HT // 128)]
            b_xb = [Buf(), Buf()]
            b_c, b_cos, b_sin = Buf(), Buf(), Buf()
            b_wt = [Buf(), Buf()]
            b_stg = [Buf(), Buf()]
            b_stgG = Buf()
            b_stgT = [Buf(), Buf()]
            b_tA, b_t1, b_t2 = [Buf(), Buf()], [Buf(), Buf()], [Buf(), Buf()]
            d_c = sch.dsem()
            d_xb = [sch.dsem(), sch.dsem()]
            d_wt = [sch.dsem(), sch.dsem()]
            d_stg = [sch.dsem(), sch.dsem()]
            d_stgT = [sch.dsem(), sch.dsem()]
            d_tab = sch.dsem()
            sch.dma("sp", lambda e: e.dma_start(out=identb[:], in_=cst["identb"]), d_c, writes=[b_c])
            sch.dma("sp", lambda e: e.dma_start(out=bfm[:], in_=b_fm), d_c, writes=[b_c])
            sch.dma("sp", lambda e: e.dma_start(out=btm[:], in_=b_tm.partition_broadcast(128)), d_c, writes=[b_c])
            jobs = fm_jobs()
            cnt = {"ps": 0, "wt": 0, "stg": 0, "stgT": 0, "t": 0, "ev": 0}
            for h in range(NH):
                h0 = h * HT
                sch.dma("sp", lambda e, h0=h0: e.dma_start(out=cosT[:], in_=cst["cosT"][:, h0:h0 + HT]), d_tab, writes=[b_cos])
                sch.dma("sp", lambda e, h0=h0: e.dma_start(out=sinT[:], in_=cst["sinT"][:, h0:h0 + HT]), d_tab, writes=[b_sin])
                for c in range(HT // 128):
                    i = c % 2
                    tok0 = h0 + c * 128
                    sch.dma("pool", lambda e, i=i, tok0=tok0: e.dma_start(out=xb[i][:], in_=x[tok0:tok0 + 128, :]),
                            d_xb[i], writes=[b_xb[i]])
                    for q4 in range(4):
                        pi = cnt["ps"] % 4
                        cnt["ps"] += 1
                        pv = ps[pi].bitcast(BF16)
                        for j in range(4):
                            kc = q4 * 4 + j
                            sch.op("pe", lambda e, pv=pv, j=j, kc=kc, i=i: e.transpose(
                                pv[:, j * 128:(j + 1) * 128], xb[i][:, kc * 128:(kc + 1) * 128], identb[:]),
                                reads=[b_xb[i], b_c], writes=[psb[pi]])
                        src = pv[:, 0:512].rearrange("p (a b) -> p a b", a=4)
                        dst = xT[:, q4 * 4:(q4 + 1) * 4, c * 128:(c + 1) * 128]
                        if (c * 4 + q4) % 2 == 0:
                            sch.op("act", lambda e, src=src, dst=dst: e.activation(out=dst, in_=src, func=AF.Copy),
                                   reads=[psb[pi]], writes=[b_xT[c]])
                        else:
                            sch.op("dve", lambda e, src=src, dst=dst: e.tensor_copy(out=dst, in_=src),
                                   reads=[psb[pi]], writes=[b_xT[c]])
                ngrp = (len(jobs) + 3) // 4
                for gi in range(ngrp):
                    grp = jobs[gi * 4:(gi + 1) * 4]
                    wi = cnt["wt"] % 2
                    cnt["wt"] += 1
                    j0 = 0
                    while j0 < len(grp):
                        j1 = j0 + 1
                        while j1 < len(grp) and grp[j1][1] == grp[j1 - 1][1] + grp[j1 - 1][2] and grp[j1 - 1][2] == 128:
                            j1 += 1
                        c0 = grp[j0][1]
                        ncol = sum(g_[2] for g_ in grp[j0:j1])
                        src = w_in[:, c0:c0 + ncol].rearrange("(kc p) n -> p kc n", p=128)
                        dst = wt[wi][:, :, j0 * 128:j0 * 128 + ncol]
                        sch.dma("pool", lambda e, src=src, dst=dst: e.dma_start(out=dst, in_=src), d_wt[wi], writes=[b_wt[wi]])
                        j0 = j1
                    for jj, (kind, col0, ncols, dl, pfc) in enumerate(grp):
                        si = cnt["stg"] % 2
                        if kind != "gl":
                            cnt["stg"] += 1
                        for tt in range(HT // 512):
                            pi = 4 + cnt["ps"] % 4
                            cnt["ps"] += 1
                            for kc in range(KC):
                                sch.op("pe", lambda e, pi=pi, wi=wi, jj=jj, ncols=ncols, kc=kc, tt=tt: e.matmul(
                                    ps[pi][0:ncols, :], wt[wi][:, kc, jj * 128:jj * 128 + ncols], xT[:, kc, tt * 512:(tt + 1) * 512],
                                    start=(kc == 0), stop=(kc == KC - 1)),
                                    reads=[b_wt[wi]] + b_xT[tt * 4:(tt + 1) * 4], writes=[psb[pi]])
                            bias = bfm[0:ncols, pfc:pfc + 1]
                            tsl = slice(tt * 512, (tt + 1) * 512)
                            if kind == "plain":
                                sch.op("act", lambda e, pi=pi, si=si, bias=bias, tsl=tsl: e.activation(
                                    out=stg[si][:, tsl], in_=ps[pi][:], func=AF.Identity, bias=bias),
                                    reads=[psb[pi], b_c], writes=[b_stg[si]])
                            elif kind == "sig":
                                sch.op("act", lambda e, pi=pi, si=si, bias=bias, tsl=tsl: e.activation(
                                    out=stg[si][:, tsl], in_=ps[pi][:], func=AF.Sigmoid, bias=bias),
                                    reads=[psb[pi], b_c], writes=[b_stg[si]])
                            elif kind == "gl":
                                sch.op("act", lambda e, pi=pi, bias=bias, tsl=tsl: e.activation(
                                    out=stgG[:, tsl], in_=ps[pi][0:24, :], func=AF.Sigmoid, bias=bias),
                                    reads=[psb[pi], b_c], writes=[b_stgG])
                            else:
                                ti = cnt["t"] % 2
                                cnt["t"] += 1
                                sch.op("act", lambda e, pi=pi, ti=ti, bias=bias: e.activation(
                                    out=tA[ti][:], in_=ps[pi][:], func=AF.Identity, bias=bias),
                                    reads=[psb[pi], b_c], writes=[b_tA[ti]])
                                sch.op("dve", lambda e, ti=ti, tsl=tsl: e.tensor_tensor(
                                    out=t1[ti][:], in0=tA[ti][:], in1=cosT[:, tsl], op=ALU.mult),
                                    reads=[b_tA[ti], b_cos], writes=[b_t1[ti]])
                                sch.op("pool", lambda e, ti=ti, tsl=tsl: e.tensor_tensor(
                                    out=t2[ti][0:64, :], in0=tA[ti][64:128, :], in1=sinT[64:128, tsl], op=ALU.mult),
                                    reads=[b_tA[ti], b_sin], writes=[b_t2[ti]])
                                sch.op("pool", lambda e, ti=ti, tsl=tsl: e.tensor_tensor(
                                    out=t2[ti][64:128, :], in0=tA[ti][0:64, :], in1=sinT[0:64, tsl], op=ALU.mult),
                                    reads=[b_tA[ti], b_sin], writes=[b_t2[ti]])
                                if dl == 1:
                                    o_ap = stg[si][:, tsl]
                                    i0 = t1[ti][:]
                                    i1 = t2[ti][:]
                                else:
                                    npos = 512 // dl
                                    il0 = tt * npos
                                    o_ap = stg[si][:].rearrange("p (r i) -> p r i", r=dl)[:, :, il0:il0 + npos]
                                    i0 = t1[ti][:].rearrange("p (i r) -> p r i", r=dl)
                                    i1 = t2[ti][:].rearrange("p (i r) -> p r i", r=dl)
                                sch.op("dve", lambda e, o_ap=o_ap, i0=i0, i1=i1: e.tensor_tensor(
                                    out=o_ap, in0=i0, in1=i1, op=ALU.add),
                                    reads=[b_t1[ti], b_t2[ti]], writes=[b_stg[si]])
                        if kind == "gl":
                            sch.dma("sp", lambda e, h0=h0: e.dma_start(out=G[:, h0:h0 + HT], in_=stgG[:]), d_stg[0],
                                    reads=[b_stgG], writes=[bG])
                        elif dl == 1:
                            sch.dma("sp", lambda e, pfc=pfc, si=si, h0=h0: e.dma_start(
                                out=PF[pfc * 128:(pfc + 1) * 128, h0:h0 + HT], in_=stg[si][:]), d_stg[si],
                                reads=[b_stg[si]], writes=[bPF])
                        else:
                            L = S // dl
                            hl = HT // dl
                            dst = PF[pfc * 128:(pfc + 1) * 128, :].rearrange("p (r i) -> p r i", r=dl)[:, :, h * hl:(h + 1) * hl]
                            src = stg[si][:].rearrange("p (r i) -> p r i", r=dl)
                            sch.dma("sp", lambda e, dst=dst, src=src: e.dma_start(out=dst, in_=src), d_stg[si],
                                    reads=[b_stg[si]], writes=[bPF])
                tmj = [(None, 1, PTn, bPTn)] + [(2584 + gi * 1536 + 1024, DILS[gi], PTd[gi], bPTd[gi]) for gi in range(3)]
                for ti_, (col0, dl, dst_t, dst_b) in enumerate(tmj):
                    wi = cnt["wt"] % 2
                    cnt["wt"] += 1
                    if col0 is None:
                        for half, c0 in enumerate((1792, 2304)):
                            src = w_in[:, c0:c0 + 256].rearrange("(kc p) n -> p kc n", p=128)
                            dst = wt[wi][:, :, half * 256:(half + 1) * 256]
                            sch.dma("pool", lambda e, src=src, dst=dst: e.dma_start(out=dst, in_=src), d_wt[wi], writes=[b_wt[wi]])
                    else:
                        src = w_in[:, col0:col0 + 512].rearrange("(kc p) n -> p kc n", p=128)
                        sch.dma("pool", lambda e, src=src, wi=wi: e.dma_start(out=wt[wi][:], in_=src), d_wt[wi], writes=[b_wt[wi]])
                    hl = HT // dl
                    npos = min(128, hl)
                    for r in range(dl):
                        for j in range(hl // npos):
                            pi = 4 + cnt["ps"] % 4
                            cnt["ps"] += 1
                            t_lo = r + dl * npos * j
                            xbufs = b_xT[(t_lo // 128):((t_lo + dl * (npos - 1)) // 128) + 1]
                            for kc in range(KC):
                                lhsT = xT[:, kc, t_lo:t_lo + dl * (npos - 1) + 1:dl]
                                sch.op("pe", lambda e, pi=pi, wi=wi, kc=kc, lhsT=lhsT, npos=npos: e.matmul(
                                    ps[pi][0:npos, :], lhsT, wt[wi][:, kc, :], start=(kc == 0), stop=(kc == KC - 1)),
                                    reads=[b_wt[wi]] + xbufs, writes=[psb[pi]])
                            si = cnt["stgT"] % 2
                            cnt["stgT"] += 1
                            bsl = btm[0:npos, ti_ * 512:(ti_ + 1) * 512]
                            sch.op("dve", lambda e, pi=pi, si=si, bsl=bsl, npos=npos: e.tensor_tensor(
                                out=stgT[si][0:npos, :], in0=ps[pi][0:npos, :], in1=bsl, op=ALU.add),
                                reads=[psb[pi], b_c], writes=[b_stgT[si]])
                            row0 = r * (S // dl) + h * hl + j * npos
                            sch.dma("sp", lambda e, dst_t=dst_t, row0=row0, npos=npos, si=si: e.dma_start(
                                out=dst_t[row0:row0 + npos, :], in_=stgT[si][0:npos, :]), d_stgT[si],
                                reads=[b_stgT[si]], writes=[dst_b])
            sch.barrier()
            sch.emit()

    if upto >= 1:
        stage1()

    NCMP = (S - 32) // 16 + 1
    NSLC = S // 64
    NT = S // 128
    QN = min(512, S)
    NQT = S // QN
    ON = dscr("ON", [1024, S], BF16)
    OD = dscr("OD", [512, S], BF16)
    bON, bOD = Buf("ON"), Buf("OD")
    cmp_w1 = [din("cmp_k_w1", [4096, 256]), din("cmp_v_w1", [4096, 256])]
    cmp_w2 = [din("cmp_k_w2", [256, 128]), din("cmp_v_w2", [256, 128])]
    posT = [din("posT_k", [128, 32]), din("posT_v", [128, 32])]

    class ACtx:
        pass

    def make_actx(sb, tag):
        a = ACtx()
        a.pT = [sb(tag + "pT%d" % i, [128, 512], BF16) for i in range(3)]
        a.b_pT = [Buf() for _ in range(3)]
        a.rd = [sb(tag + "rd%d" % i, [128, 512], F32) for i in range(2)]
        a.b_rd = [Buf(), Buf()]
        a.tmp = [sb(tag + "tmp%d" % i, [128, 512], F32) for i in range(2)]
        a.b_tmp = [Buf(), Buf()]
        a.n = 0
        a.k = 0
        a.e = 0
        return a

    def mm(out_ap, l_ap, r_ap, st, sp_):
        return lambda e: e.matmul(out_ap, l_ap, r_ap, start=st, stop=sp_)

    def attn_tile(a, qT, qbufs, N, chunks, ones_ap, cbuf):
        ni = 2 + a.n % 2
        di = 4 + a.n % 2
        a.n += 1
        nch = len(chunks)

        def emit_s(i):
            ch = chunks[i]
            si = (a.k + i) % 2
            kn = ch["kn"]
            nx = len(ch["extra"])
            sch.op("pe", mm(ps[si][0:kn, 0:N], ch["kT"], qT, True, nx == 0),
                   reads=list(qbufs) + list(ch["kvbufs"]), writes=[psb[si]])
            for xi, (l_ap, r_ap, bufs) in enumerate(ch["extra"]):
                sch.op("pe", mm(ps[si][0:kn, 0:N], l_ap, r_ap, False, xi == nx - 1), reads=list(bufs), writes=[psb[si]])
            pi = (a.k + i) % 3
            sch.op("act", lambda e, o=a.pT[pi][0:kn, 0:N], i_=ps[si][0:kn, 0:N]: e.activation(out=o, in_=i_, func=AF.Exp, scale=SCALE),
                   reads=[psb[si]], writes=[a.b_pT[pi]])
        emit_s(0)
        for i in range(nch):
            if i + 1 < nch:
                emit_s(i + 1)
            ch = chunks[i]
            kn = ch["kn"]
            pi = (a.k + i) % 3
            sch.op("pe", mm(ps[ni][:, 0:N], ch["v"], a.pT[pi][0:kn, 0:N], i == 0, i == nch - 1),
                   reads=[a.b_pT[pi]] + list(ch["kvbufs"]), writes=[psb[ni]])
            sch.op("pe", mm(ps[di][:, 0:N], ones_ap[0:kn, :], a.pT[pi][0:kn, 0:N], i == 0, i == nch - 1),
                   reads=[a.b_pT[pi], cbuf], writes=[psb[di]])
        a.k += nch
        return ni, di

    def attn_epilogue(a, ni, di, N, gate_ap, gbufs, acc_ap, acc_buf, first):
        ri = a.e % 2
        a.e += 1
        rd, brd = a.rd[ri], a.b_rd[ri]
        sch.op("dve", lambda e: e.tensor_scalar_max(out=rd[:, 0:N], in0=ps[di][:, 0:N], scalar1=1e-30), reads=[psb[di]], writes=[brd])
        wide = N >= 256
        sch.op("dve", lambda e: e.reciprocal(out=rd[:, 0:N], in_=rd[:, 0:N]), reads=[brd], writes=[brd], same=not wide)
        if gate_ap is not None:
            sch.op("dve", lambda e: e.tensor_tensor(out=rd[:, 0:N], in0=rd[:, 0:N], in1=gate_ap, op=ALU.mult),
                   reads=[brd] + list(gbufs), writes=[brd], same=not wide)
        if first:
            sch.op("dve", lambda e: e.tensor_tensor(out=acc_ap, in0=ps[ni][:, 0:N], in1=rd[:, 0:N], op=ALU.mult),
                   reads=[psb[ni], brd], writes=[acc_buf], same=not wide)
        else:
            tm, btm_ = a.tmp[ri], a.b_tmp[ri]
            sch.op("dve", lambda e: e.tensor_tensor(out=tm[:, 0:N], in0=ps[ni][:, 0:N], in1=rd[:, 0:N], op=ALU.mult),
                   reads=[psb[ni], brd], writes=[btm_], same=not wide)
            sch.op("pool", lambda e: e.tensor_tensor(out=acc_ap, in0=acc_ap, in1=tm[:, 0:N], op=ALU.add),
                   reads=[btm_, acc_buf], writes=[acc_buf])

    def stage_nsa(g):
        tg = "n%d_" % g
        with ExitStack() as es:
            def sb(name, shape, dt):
                return es.enter_context(nc.sbuf_tensor(tg + name, shape, dt))
            QT = sb("QT", [128, 4, S], BF16)
            KsT = sb("KsT", [128, S], BF16)
            KwT = sb("KwT", [128, S], BF16)
            Vs = sb("Vs", [128, NT, 128], BF16)
            Vw = sb("Vw", [128, NT, 128], BF16)
            kcT = sb("kcT", [128, 256], BF16)
            vc = sb("vc", [128, 2, 128], BF16)
            MbT = sb("MbT", [NSLC, S], BF16)
            Eall = sb("Eall", [NSLC, S], BF16)
            BT = sb("BT", [128, 12, 512], BF16)
            KEEP = sb("KEEP", [128, NT, NSLC], F32)
            ADD = sb("ADD", [128, NT, NSLC], F32)
            ovl = sb("ovl", [128, 2, NSLC], BF16)
            identb = sb("identb", [128, 128], BF16)
            ones = sb("ones", [128, 128], BF16)
            zer = sb("zer", [128, 256], BF16)
            b_q, b_kv, b_c, b_kc, b_MbT = Buf(), Buf(), Buf(), Buf(), Buf()
            d_l = sch.dsem()
            ld = lambda o, i, bufs: sch.dma("sp", lambda e: e.dma_start(out=o, in_=i), d_l, writes=bufs)
            ld(QT[:], PF[4 * g * 128:(4 * g + 4) * 128, :].rearrange("(r p) s -> p r s", p=128), [b_q])
            sch._waits("sp", [bPF, bPTn], [])
            ld(KsT[:], PF[(12 + g) * 128:(13 + g) * 128, :], [b_kv])
            ld(KwT[:], PF[(14 + g) * 128:(15 + g) * 128, :], [b_kv])
            ld(Vs[:], PTn[:, g * 128:(g + 1) * 128].rearrange("(c p) d -> p c d", p=128), [b_kv])
            ld(Vw[:], PTn[:, 256 + g * 128:256 + (g + 1) * 128].rearrange("(c p) d -> p c d", p=128), [b_kv])
            ld(Eall[:], cst["Eall"], [b_c])
            ld(BT[:], cst["BT"][:, 0:12, :], [b_c])
            ld(KEEP[:], cst["KEEP"].rearrange("(c p) j -> p c j", p=128), [b_c])
            ld(ADD[:], cst["ADD"].rearrange("(c p) j -> p c j", p=128), [b_c])
            ld(ovl[:], cst["ovl"].rearrange("(c p) j -> p c j", p=128), [b_c])
            ld(identb[:], cst["identb"], [b_c])
            sch.op("dve", lambda e: e.memset(ones[:], 1.0), writes=[b_c])
            sch.op("dve", lambda e: e.memset(zer[:], 0.0), writes=[b_c])
            sch.op("dve", lambda e: e.memset(kcT[:], 0.0), writes=[b_kc])
            sch.op("dve", lambda e: e.memset(vc[:], 0.0), writes=[b_kc])
            cch = [(0, min(128, NCMP))] + ([(1, NCMP - 128)] if NCMP > 128 else [])
            with ExitStack() as es2:
                def sb2(name, shape, dt):
                    return es2.enter_context(nc.sbuf_tensor(tg + "c_" + name, shape, dt))
                src = sb2("src", [128, S], BF16)
                W1 = sb2("W1", [128, 32, 256], BF16)
                W2 = sb2("W2", [128, 2, 128], BF16)
                pT_ = sb2("posT", [128, 32], BF16)
                cvec = sb2("cvec", [128, 2], F32)
                xh = sb2("xh", [128, 256], F32)
                x2 = sb2("x2", [128, 256], F32)
                th = sb2("th", [128, 256], F32)
                gl = sb2("gl", [128, 2, 256], BF16)
                b_src, b_W, b_cv, b_xh, b_x2, b_th, b_gl = [Buf() for _ in range(7)]
                d_c2 = sch.dsem()
                for which in range(2):
                    pfc = (8 if which == 0 else 10) + g
                    sch.dma("sp", lambda e, pfc=pfc: e.dma_start(out=src[:], in_=PF[pfc * 128:(pfc + 1) * 128, :]), d_c2,
                            reads=[bPF], writes=[b_src])
                    sch.dma("pool", lambda e, which=which: e.dma_start(
                        out=W1[:], in_=cmp_w1[which].rearrange("(l d) h -> d l h", d=128)), d_c2, writes=[b_W])
                    sch.dma("pool", lambda e, which=which: e.dma_start(
                        out=W2[:], in_=cmp_w2[which].rearrange("(c p) d -> p c d", p=128)), d_c2, writes=[b_W])
                    sch.dma("pool", lambda e, which=which: e.dma_start(out=pT_[:], in_=posT[which]), d_c2, writes=[b_W])
                    for hc in range(2):
                        for l in range(32):
                            sch.op("pe", mm(ps[7][:, 0:1], W1[:, l, hc * 128:(hc + 1) * 128], pT_[:, l:l + 1], l == 0, l == 31),
                                   reads=[b_W], writes=[psb[7]])
                        sch.op("act", lambda e, hc=hc: e.activation(out=cvec[:, hc:hc + 1], in_=ps[7][:, 0:1], func=AF.Copy),
                               reads=[psb[7]], writes=[b_cv])
                        for l in range(32):
                            sch.op("pe", mm(ps[6][:, 0:NCMP], W1[:, l, hc * 128:(hc + 1) * 128],
                                            src[:, l:l + 16 * (NCMP - 1) + 1:16], l == 0, l == 31),
                                   reads=[b_W, b_src], writes=[psb[6]])
                        sch.op("act", lambda e, hc=hc: e.activation(out=xh[:, 0:NCMP], in_=ps[6][:, 0:NCMP], func=AF.Identity,
                                                                   bias=cvec[:, hc:hc + 1]),
                               reads=[psb[6], b_cv], writes=[b_xh])
                        sch.op("dve", lambda e: e.tensor_tensor(out=x2[:, 0:NCMP], in0=xh[:, 0:NCMP], in1=xh[:, 0:NCMP], op=ALU.mult),
                               reads=[b_xh], writes=[b_x2])
                        sch.op("dve", lambda e: e.tensor_scalar(out=x2[:, 0:NCMP], in0=x2[:, 0:NCMP], scalar1=0.044715, scalar2=1.0,
                                                                op0=ALU.mult, op1=ALU.add), reads=[b_x2], writes=[b_x2])
                        sch.op("dve", lambda e: e.tensor_tensor(out=x2[:, 0:NCMP], in0=x2[:, 0:NCMP], in1=xh[:, 0:NCMP], op=ALU.mult),
                               reads=[b_x2, b_xh], writes=[b_x2])
                        sch.op("act", lambda e: e.activation(out=th[:, 0:NCMP], in_=x2[:, 0:NCMP], func=AF.Tanh, scale=0.7978845608028654),
                               reads=[b_x2], writes=[b_th])
                        sch.op("dve", lambda e: e.tensor_scalar(out=th[:, 0:NCMP], in0=th[:, 0:NCMP], scalar1=1.0, scalar2=0.5,
                                                                op0=ALU.add, op1=ALU.mult), reads=[b_th], writes=[b_th])
                        sch.op("dve", lambda e, hc=hc: e.tensor_tensor(out=gl[:, hc, 0:NCMP], in0=th[:, 0:NCMP], in1=xh[:, 0:NCMP], op=ALU.mult),
                               reads=[b_th, b_xh], writes=[b_gl])
                    if which == 0:
                        for hc in range(2):
                            sch.op("pe", mm(ps[7][:, 0:NCMP], W2[:, hc, :], gl[:, hc, 0:NCMP], hc == 0, hc == 1),
                                   reads=[b_W, b_gl], writes=[psb[7]])
                        sch.op("act", lambda e: e.activation(out=kcT[:, 0:NCMP], in_=ps[7][:, 0:NCMP], func=AF.Copy),
                               reads=[psb[7]], writes=[b_kc])
                    else:
                        for cc, kn in cch:
                            for hc in range(2):
                                sch.op("pe", mm(ps[7][0:kn, 0:128], gl[:, hc, cc * 128:cc * 128 + kn], W2[:, hc, :], hc == 0, hc == 1),
                                       reads=[b_W, b_gl], writes=[psb[7]])
                            sch.op("act", lambda e, cc=cc, kn=kn: e.activation(out=vc[0:kn, cc, :], in_=ps[7][0:kn, 0:128], func=AF.Copy),
                                   reads=[psb[7]], writes=[b_kc])
                if debug:
                    dkc = dscr("dbg_kc%d" % g, [128, 256], BF16)
                    dvc = dscr("dbg_vc%d" % g, [128, 2, 128], BF16)
                    sch.dma("sp", lambda e: e.dma_start(out=dkc, in_=kcT[:]), d_c2, reads=[b_kc], writes=[Buf()])
                    sch.dma("sp", lambda e: e.dma_start(out=dvc, in_=vc[:]), d_c2, reads=[b_kc], writes=[Buf()])
                sch.barrier()
                sch.emit()
            with ExitStack() as es3:
                def sb3(name, shape, dt):
                    return es3.enter_context(nc.sbuf_tensor(tg + "a_" + name, shape, dt))
                a = make_actx(sb3, "")
                Grep = sb3("Grep", [128, 12, QN], F32)
                CMBt = sb3("CMBt", [128, 2, QN], BF16)
                pc = [sb3("pc%d" % i, [128, QN], BF16) for i in range(2)]
                pn = [sb3("pn%d" % i, [128, QN], BF16) for i in range(2)]
                oacc = [sb3("oacc%d" % i, [128, QN], F32) for i in range(4)]
                ost = [sb3("ost%d" % i, [128, QN], BF16) for i in range(2)]
                impm = sb3("impm", [128, NSLC], F32)
                impt = sb3("impt", [128, NSLC], F32)
                v8 = sb3("v8", [128, 16], F32)
                Mb = sb3("Mb", [128, NSLC], BF16)
                b_G, b_CMB = Buf(), Buf()
                b_pc, b_pn = [Buf(), Buf()], [Buf(), Buf()]
                b_oacc = [Buf() for _ in range(4)]
                b_ost = [Buf(), Buf()]
                b_impm, b_impt, b_v8, b_Mb = Buf(), Buf(), Buf(), Buf()
                d_G, d_CMB = sch.dsem(), sch.dsem()
                d_ost = [sch.dsem(), sch.dsem()]
                n_ost = 0
                for qt in range(NQT):
                    T0 = qt * QN
                    N = QN
                    sch.dma("sp", lambda e, T0=T0: e.dma_start(out=Grep[:], in_=G[g * 12:(g + 1) * 12, T0:T0 + QN].partition_broadcast(128)),
                            d_G, reads=[bG], writes=[b_G])
                    sch.dma("sp", lambda e, T0=T0: e.dma_start(out=CMBt[:], in_=cst["CMB"][:, :, T0:T0 + QN]), d_CMB, writes=[b_CMB])
                    ntc = N // 128
                    sch.op("pe", mm(ps[6][:, 0:ntc * NSLC], zer[:, 0:128], zer[:, 0:ntc * NSLC], True, False), reads=[b_c], writes=[psb[6]])
                    for r in range(4):
                        qT = QT[:, r, T0:T0 + N]
                        for cc, kn in cch:
                            si = a.k % 2
                            a.k += 1
                            sch.op("pe", mm(ps[si][0:kn, 0:N], kcT[:, cc * 128:cc * 128 + kn], qT, True, False),
                                   reads=[b_q, b_kc], writes=[psb[si]])
                            sch.op("pe", mm(ps[si][0:kn, 0:N], identb[0:kn, 0:kn], CMBt[0:kn, cc, :], False, True),
                                   reads=[b_c, b_CMB], writes=[psb[si]])
                            sch.op("act", lambda e, cc=cc, kn=kn, si=si: e.activation(out=pc[cc][0:kn, 0:N], in_=ps[si][0:kn, 0:N],
                                                                                   func=AF.Exp, scale=SCALE),
                                   reads=[psb[si]], writes=[b_pc[cc]])
                        ni = 2 + a.n % 2
                        di = 4 + a.n % 2
                        a.n += 1
                        for ci, (cc, kn) in enumerate(cch):
                            sch.op("pe", mm(ps[di][:, 0:N], ones[0:kn, :], pc[cc][0:kn, 0:N], ci == 0, ci == len(cch) - 1),
                                   reads=[b_c, b_pc[cc]], writes=[psb[di]])
                        ri = a.e % 2
                        a.e += 1
                        rd, brd = a.rd[ri], a.b_rd[ri]
                        sch.op("dve", lambda e, rd=rd, di=di: e.tensor_scalar_max(out=rd[:, 0:N], in0=ps[di][:, 0:N], scalar1=1e-30),
                               reads=[psb[di]], writes=[brd])
                        sch.op("dve", lambda e, rd=rd: e.reciprocal(out=rd[:, 0:N], in_=rd[:, 0:N]), reads=[brd], writes=[brd])
                        for cc, kn in cch:
                            sch.op("pool", lambda e, cc=cc, kn=kn, rd=rd: e.tensor_tensor(out=pn[cc][0:kn, 0:N], in0=pc[cc][0:kn, 0:N],
                                                                                       in1=rd[0:kn, 0:N], op=ALU.mult),
                                   reads=[b_pc[cc], brd], writes=[b_pn[cc]])
                        for ci, (cc, kn) in enumerate(cch):
                            sch.op("pe", mm(ps[ni][:, 0:N], vc[0:kn, cc, :], pn[cc][0:kn, 0:N], ci == 0, ci == len(cch) - 1),
                                   reads=[b_kc, b_pn[cc]], writes=[psb[ni]])
                        sch.op("dve", lambda e, r=r, ni=ni: e.tensor_tensor(out=oacc[r][:, 0:N], in0=ps[ni][:, 0:N], in1=Grep[:, r * 3, :],
                                                                          op=ALU.mult),
                               reads=[psb[ni], b_G], writes=[b_oacc[r]])
                        for tc in range(ntc):
                            for cc, kn in cch:
                                sch.op("pe", mm(ps[6][:, tc * NSLC:(tc + 1) * NSLC], pn[cc][0:kn, tc * 128:(tc + 1) * 128],
                                                ovl[0:kn, cc, :], False, False),
                                       reads=[b_pn[cc], b_c], writes=[psb[6]])
                    for tc in range(ntc):
                        chn = T0 // 128 + tc
                        sch.op("dve", lambda e, tc=tc, chn=chn: e.tensor_tensor(out=impm[:], in0=ps[6][:, tc * NSLC:(tc + 1) * NSLC],
                                                                               in1=KEEP[:, chn, :], op=ALU.mult),
                               reads=[psb[6], b_c], writes=[b_impm])
                        sch.op("dve", lambda e, chn=chn: e.tensor_tensor(out=impm[:], in0=impm[:], in1=ADD[:, chn, :], op=ALU.add),
                               reads=[b_impm, b_c], writes=[b_impm])
                        sch.op("dve", lambda e: e.max(out=v8[:, 0:8], in_=impm[:]), reads=[b_impm], writes=[b_v8])
                        sch.op("dve", lambda e: e.match_replace(out=impt[:], in_to_replace=v8[:, 0:8], in_values=impm[:], imm_value=-3.0e38),
                               reads=[b_impm, b_v8], writes=[b_impt])
                        sch.op("dve", lambda e: e.max(out=v8[:, 8:16], in_=impt[:]), reads=[b_impt], writes=[b_v8])
                        sch.op("dve", lambda e: e.tensor_scalar(out=Mb[:], in0=impm[:], scalar1=v8[:, 15:16], scalar2=NEGB,
                                                                op0=ALU.is_lt, op1=ALU.mult),
                               reads=[b_impm, b_v8], writes=[b_Mb])
                        pv7 = ps[7].bitcast(BF16)
                        sch.op("pe", lambda e, pv7=pv7: e.transpose(pv7[0:NSLC, 0:128], Mb[:, 0:NSLC], identb[:]),
                               reads=[b_Mb, b_c], writes=[psb[7]])
                        sch.op("act", lambda e, pv7=pv7, tc=tc, T0=T0: e.activation(out=MbT[:, T0 + tc * 128:T0 + (tc + 1) * 128],
                                                                           in_=pv7[0:NSLC, 0:128], func=AF.Copy),
                               reads=[psb[7]], writes=[b_MbT])
                    for r in range(4):
                        qT = QT[:, r, T0:T0 + N]
                        chunks = []
                        for sc in range((T0 + N) // 128):
                            extra = [(Eall[:, sc * 128:(sc + 1) * 128], MbT[:, T0:T0 + N], [b_c, b_MbT])]
                            if sc * 128 + 127 > T0:
                                dk = (sc * 128 - T0) // 128
                                extra.append((identb[:], BT[:, dk, 0:N], [b_c]))
                            chunks.append(dict(kT=KsT[:, sc * 128:(sc + 1) * 128], kn=128, v=Vs[:, sc, :], kvbufs=[b_kv], extra=extra))
                        ni, di = attn_tile(a, qT, [b_q], N, chunks, ones, b_c)
                        if "c" not in DBG_BR:
                            sch.op("dve", lambda e, r=r: e.memset(oacc[r][:, 0:N], 0.0), writes=[b_oacc[r]])
                        if "s" in DBG_BR:
                            attn_epilogue(a, ni, di, N, Grep[:, r * 3 + 1, :], [b_G], oacc[r][:, 0:N], b_oacc[r], False)
                        chunks = []
                        for k in range(8):
                            s0 = T0 - 512 + 128 * k
                            if s0 < 0 or s0 >= S or s0 >= T0 + N:
                                continue
                            sc = s0 // 128
                            chunks.append(dict(kT=KwT[:, sc * 128:(sc + 1) * 128], kn=128, v=Vw[:, sc, :], kvbufs=[b_kv],
                                               extra=[(identb[:], BT[:, 4 + k, 0:N], [b_c])]))
                        ni, di = attn_tile(a, qT, [b_q], N, chunks, ones, b_c)
                        if "w" in DBG_BR:
                            attn_epilogue(a, ni, di, N, Grep[:, r * 3 + 2, :], [b_G], oacc[r][:, 0:N], b_oacc[r], False)
                        oi = n_ost % 2
                        n_ost += 1
                        sch.op("act", lambda e, oi=oi, r=r: e.activation(out=ost[oi][:, 0:N], in_=oacc[r][:, 0:N], func=AF.Copy),
                               reads=[b_oacc[r]], writes=[b_ost[oi]])
                        hd = 4 * g + r
                        sch.dma("sp", lambda e, oi=oi, hd=hd, T0=T0: e.dma_start(out=ON[hd * 128:(hd + 1) * 128, T0:T0 + N], in_=ost[oi][:, 0:N]),
                                d_ost[oi], reads=[b_ost[oi]], writes=[bON])
                if debug:
                    dmb = dscr("dbg_mbt%d" % g, [NSLC, S], BF16)
                    sch.dma("sp", lambda e: e.dma_start(out=dmb, in_=MbT[:]), d_G, reads=[b_MbT], writes=[Buf()])
                sch.barrier()
                sch.emit()

    if upto >= 2:
        stage_nsa(0)
        stage_nsa(1)

    def stage_dil(hd):
        tg = "d%d_" % hd
        with ExitStack() as es:
            def sb(name, shape, dt):
                return es.enter_context(nc.sbuf_tensor(tg + name, shape, dt))
            QT = sb("QT", [128, 3, S], BF16)
            KT = sb("KT", [128, 3, S], BF16)
            kns = [min(128, S // dl) for dl in DILS]
            V = [sb("V%d" % gi, [kns[gi], S // kns[gi], 128], BF16) for gi in range(3)]
            BT = sb("BT", [128, 5, 512], BF16)
            ones = sb("ones", [128, 128], BF16)
            anum = sb("anum", [128, S], F32)
            aden = sb("aden", [128, S], F32)
            ost = [sb("ost%d" % i, [128, 512], BF16) for i in range(2)]
            rdf = [sb("rdf%d" % i, [128, 512], F32) for i in range(2)]
            a = make_actx(sb, "")
            b_q, b_kv, b_c, b_acc = Buf(), Buf(), Buf(), Buf()
            b_ost, b_rdf = [Buf(), Buf()], [Buf(), Buf()]
            d_l = sch.dsem()
            d_ost = [sch.dsem(), sch.dsem()]
            ld = lambda o, i, bufs: sch.dma("sp", lambda e: e.dma_start(out=o, in_=i), d_l, writes=bufs)
            for gi in range(3):
                cq = 16 + gi * 4 + hd
                ck = 28 + gi * 4 + hd
                ld(QT[:, gi, :], PF[cq * 128:(cq + 1) * 128, :], [b_q])
                ld(KT[:, gi, :], PF[ck * 128:(ck + 1) * 128, :], [b_kv])
                ld(V[gi][:], PTd[gi][:, hd * 128:(hd + 1) * 128].rearrange("(c p) d -> p c d", p=kns[gi]), [b_kv])
            ld(BT[:], cst["BT"][:, 12:17, :], [b_c])
            sch.op("dve", lambda e: e.memset(ones[:], 1.0), writes=[b_c])
            for gi in range(3):
                dl = DILS[gi]
                L = S // dl
                kn = kns[gi]
                N_ = min(512, L)
                for rho in range(dl):
                    for qi in range(L // N_):
                        I0 = qi * N_
                        qT = QT[:, gi, rho * L + I0:rho * L + I0 + N_]
                        chunks = []
                        s0 = max(0, I0 - 128)
                        while s0 < I0 + N_:
                            bi = (s0 - I0 + 128) // 128
                            chunks.append(dict(kT=KT[:, gi, rho * L + s0:rho * L + s0 + kn], kn=kn,
                                               v=V[gi][:, (rho * L + s0) // kn, :], kvbufs=[b_kv],
                                               extra=[(ones[0:kn, 0:kn] if False else identb_g[0:kn, 0:kn], BT[0:kn, bi, 0:N_], [b_c, b_idg])]))
                            s0 += kn
                        ni, di = attn_tile(a, qT, [b_q], N_, chunks, ones, b_c)
                        c0 = rho + dl * I0
                        c1 = rho + dl * (I0 + N_ - 1) + 1
                        if gi == 0:
                            sch.op("act", lambda e, ni=ni, c0=c0, c1=c1, dl=dl, N_=N_: e.activation(
                                out=anum[:, c0:c1:dl], in_=ps[ni][:, 0:N_], func=AF.Copy), reads=[psb[ni]], writes=[b_acc])
                            sch.op("dve", lambda e, di=di, c0=c0, c1=c1, dl=dl, N_=N_: e.tensor_copy(
                                out=aden[:, c0:c1:dl], in_=ps[di][:, 0:N_]), reads=[psb[di]], writes=[b_acc])
                        else:
                            sch.op("dve", lambda e, ni=ni, c0=c0, c1=c1, dl=dl, N_=N_: e.tensor_tensor(
                                out=anum[:, c0:c1:dl], in0=anum[:, c0:c1:dl], in1=ps[ni][:, 0:N_], op=ALU.add),
                                reads=[psb[ni], b_acc], writes=[b_acc])
                            sch.op("dve", lambda e, di=di, c0=c0, c1=c1, dl=dl, N_=N_: e.tensor_tensor(
                                out=aden[:, c0:c1:dl], in0=aden[:, c0:c1:dl], in1=ps[di][:, 0:N_], op=ALU.add),
                                reads=[psb[di], b_acc], writes=[b_acc])
            for qt in range(S // QN):
                T0 = qt * QN
                oi = qt % 2
                sch.op("dve", lambda e, oi=oi, T0=T0: e.reciprocal(out=rdf[oi][:, 0:QN], in_=aden[:, T0:T0 + QN]),
                       reads=[b_acc], writes=[b_rdf[oi]])
                sch.op("dve", lambda e, oi=oi, T0=T0: e.tensor_tensor(out=ost[oi][:, 0:QN], in0=anum[:, T0:T0 + QN], in1=rdf[oi][:, 0:QN],
                                                                      op=ALU.mult), reads=[b_acc, b_rdf[oi]], writes=[b_ost[oi]])
                sch.dma("sp", lambda e, oi=oi, T0=T0: e.dma_start(out=OD[hd * 128:(hd + 1) * 128, T0:T0 + QN], in_=ost[oi][:, 0:QN]),
                        d_ost[oi], reads=[b_ost[oi]], writes=[bOD])
            sch.barrier()
            sch.emit()

    if upto >= 3:
        identb_g = nc.alloc_sbuf_tensor("identb_g", [128, 128], BF16).ap()
        b_idg = Buf()
        d_idg = sch.dsem()
        sch.dma("sp", lambda e: e.dma_start(out=identb_g, in_=cst["identb"]), d_idg, writes=[b_idg])
        for hd in range(4):
            stage_dil(hd)


    CAP = CAPB * 128
    NROW = NE * CAP
    w_br_nsa = din("w_br_nsa", [1024, D])
    w_br_dil = din("w_br_dil", [512, D])
    w_out = din("w_out", [D, D])
    ln_gb = din("ln_gb", [4, D])
    w_router = din("w_router", [D, NE])
    b_router = din("b_router", [1, NE])
    MT = dscr("MT", [D, S], BF16)
    H1 = dscr("H1", [S, D], F32)
    NQX, NQY = (4, 8) if ep else (1, 1)
    WX, WY = D // NQX, D // NQY
    XG = [dscr("XG%d" % q, [NROW, WX], BF16) for q in range(NQX)]
    RI = dscr("RI", [S, 8], F32)
    ROWI = [dscr("ROWI%d" % k_, [S, 1], I32) for k_ in range(4)]
    bMT, bH1, bXG, bRI = Buf(), Buf(), Buf(), Buf()

    def stage_merge():
        with ExitStack() as es:
            def sb(name, shape, dt):
                return es.enter_context(nc.sbuf_tensor("m_" + name, shape, dt))
            Wa = sb("Wa", [128, 8, D], BF16)
            Wb = sb("Wb", [128, 4, D], BF16)
            ONt = [sb("ONt%d" % i, [128, 8, QN], BF16) for i in range(2)]
            ODt = [sb("ODt%d" % i, [128, 4, QN], BF16) for i in range(2)]
            ga = sb("ga", [128, 16, QN], BF16)
            gb = sb("gb", [128, 16, QN], BF16)
            mst = [sb("mst%d" % i, [128, 16, QN], BF16) for i in range(2)]
            m1 = [sb("m1%d" % i, [128, QN], F32) for i in range(2)]
            m2 = [sb("m2%d" % i, [128, QN], F32) for i in range(2)]
            b_W, b_g = Buf(), Buf()
            b_in_, b_mst, b_m1, b_m2 = [Buf(), Buf()], [Buf(), Buf()], [Buf(), Buf()], [Buf(), Buf()]
            d_W, d_g = sch.dsem(), sch.dsem()
            d_in_, d_mst = [sch.dsem(), sch.dsem()], [sch.dsem(), sch.dsem()]
            sch.dma("pool", lambda e: e.dma_start(out=Wa[:], in_=w_br_nsa.rearrange("(f p) n -> p f n", p=128)), d_W, writes=[b_W])
            sch.dma("pool", lambda e: e.dma_start(out=Wb[:], in_=w_br_dil.rearrange("(f p) n -> p f n", p=128)), d_W, writes=[b_W])
            k = 0
            for qt in range(NQT):
                T0 = qt * QN
                bi = qt % 2
                sch.dma("sp", lambda e, bi=bi, T0=T0: e.dma_start(out=ONt[bi][:], in_=ON[:, T0:T0 + QN].rearrange("(f p) t -> p f t", p=128)),
                        d_in_[bi], writes=[b_in_[bi]])
                sch.dma("sp", lambda e, bi=bi, T0=T0: e.dma_start(out=ODt[bi][:], in_=OD[:, T0:T0 + QN].rearrange("(f p) t -> p f t", p=128)),
                        d_in_[bi], writes=[b_in_[bi]])
                sch.dma("sp", lambda e, T0=T0: e.dma_start(out=ga[:], in_=PF[40 * 128:56 * 128, T0:T0 + QN].rearrange("(c p) t -> p c t", p=128)),
                        d_g, writes=[b_g])
                sch.dma("sp", lambda e, T0=T0: e.dma_start(out=gb[:], in_=PF[56 * 128:72 * 128, T0:T0 + QN].rearrange("(c p) t -> p c t", p=128)),
                        d_g, writes=[b_g])
                for n in range(16):
                    pa = (2 * k) % 8
                    pb = (2 * k + 1) % 8
                    ti = k % 2
                    k += 1
                    for f in range(8):
                        sch.op("pe", mm(ps[pa][:, 0:QN], Wa[:, f, n * 128:(n + 1) * 128], ONt[bi][:, f, :], f == 0, f == 7),
                               reads=[b_W, b_in_[bi]], writes=[psb[pa]])
                    for f in range(4):
                        sch.op("pe", mm(ps[pb][:, 0:QN], Wb[:, f, n * 128:(n + 1) * 128], ODt[bi][:, f, :], f == 0, f == 3),
                               reads=[b_W, b_in_[bi]], writes=[psb[pb]])
                    sch.op("dve", lambda e, pa=pa, ti=ti, n=n: e.tensor_tensor(out=m1[ti][:], in0=ps[pa][:, 0:QN], in1=ga[:, n, :], op=ALU.mult),
                           reads=[psb[pa], b_g], writes=[b_m1[ti]])
                    sch.op("dve", lambda e, pb=pb, ti=ti, n=n: e.tensor_tensor(out=m2[ti][:], in0=ps[pb][:, 0:QN], in1=gb[:, n, :], op=ALU.mult),
                           reads=[psb[pb], b_g], writes=[b_m2[ti]])
                    sch.op("pool", lambda e, ti=ti, n=n, bi=bi: e.tensor_tensor(out=mst[bi][:, n, :], in0=m1[ti][:], in1=m2[ti][:], op=ALU.add),
                           reads=[b_m1[ti], b_m2[ti]], writes=[b_mst[bi]])
                sch.dma("sp", lambda e, bi=bi, T0=T0: e.dma_start(out=MT[:, T0:T0 + QN].rearrange("(c p) t -> p c t", p=128), in_=mst[bi][:]),
                        d_mst[bi], reads=[b_mst[bi]], writes=[bMT])
            sch.barrier()
            sch.emit()

    def layer_norm_tile(sbt, hp, b_hp, outt, b_out, gbc, bbc, b_gb, sq, b_sq, st, b_st):
        sch.op("dve", lambda e: e.tensor_reduce(out=st[:, 0:1], in_=hp[:], axis=AX.X, op=ALU.add), reads=[b_hp], writes=[b_st])
        sch.op("dve", lambda e: e.tensor_scalar(out=st[:, 1:2], in0=st[:, 0:1], scalar1=-1.0 / D, scalar2=None, op0=ALU.mult),
               reads=[b_st], writes=[b_st])
        sch.op("act", lambda e: e.activation(out=hp[:], in_=hp[:], func=AF.Identity, bias=st[:, 1:2]), reads=[b_hp, b_st], writes=[b_hp])
        sch.op("act", lambda e: e.activation(out=sq[:], in_=hp[:], func=AF.Square, accum_out=st[:, 2:3]), reads=[b_hp], writes=[b_sq, b_st])
        sch.op("dve", lambda e: e.tensor_scalar(out=st[:, 3:4], in0=st[:, 2:3], scalar1=1.0 / D, scalar2=LN_EPS, op0=ALU.mult, op1=ALU.add),
               reads=[b_st], writes=[b_st])
        sch.op("act", lambda e: e.activation(out=st[:, 4:5], in_=st[:, 3:4], func=AF.Sqrt), reads=[b_st], writes=[b_st])
        sch.op("dve", lambda e: e.reciprocal(out=st[:, 5:6], in_=st[:, 4:5]), reads=[b_st], writes=[b_st])
        sch.op("dve", lambda e: e.scalar_tensor_tensor(out=outt[:], in0=hp[:], scalar=st[:, 5:6], in1=gbc[:], op0=ALU.mult, op1=ALU.mult),
               reads=[b_hp, b_st, b_gb], writes=[b_out])
        sch.op("pool", lambda e: e.tensor_tensor(out=outt[:], in0=outt[:], in1=bbc[:], op=ALU.add), reads=[b_out, b_gb], writes=[b_out])

    ebase_y_in = din("ebase_y", [128, NE]) if ep else None

    def stage_out():
        with ExitStack() as es:
            def sb(name, shape, dt):
                return es.enter_context(nc.sbuf_tensor("o_" + name, shape, dt))
            Wo = sb("Wo", [128, KC, D], BF16)
            mTt = sb("mTt", [128, KC, QN], BF16)
            xc = [sb("xc%d" % i, [128, D], F32) for i in range(2)]
            hp = sb("hp", [128, D], F32)
            sq = sb("sq", [128, D], BF16)
            h1 = [sb("h1%d" % i, [128, D], F32) for i in range(2)]
            h1b = [sb("h1b%d" % i, [128, D], BF16) for i in range(2)]
            h1T = sb("h1T", [128, KC, 128], F32)
            gbc = sb("gbc", [128, D], F32)
            bbc = sb("bbc", [128, D], F32)
            Wr = sb("Wr", [128, KC, NE], F32)
            brb = sb("brb", [128, NE], F32)
            identf = sb("identf", [128, 128], F32)
            ones = sb("ones", [128, 128], BF16)
            ustr = sb("ustr", [128, 128], BF16)
            ebase = sb("ebase", [128, NE], F32)
            base = sb("base", [128, NE], F32)
            zt = sb("zt", [128, 4096], BF16)
            st = sb("st", [128, 8], F32)
            rt = {n_: sb(n_, [128, NE], F32) for n_ in ("lg", "m4", "ex", "gd", "slot", "okm", "rowf", "oh", "t1", "t2")}
            mb16 = sb("mb16", [128, NE], BF16)
            oh4 = sb("oh4", [128, 4, NE], F32)
            t14 = sb("t14", [128, 4, NE], F32)
            v8 = sb("v8", [128, 8], F32)
            sm = sb("sm", [128, 8], F32)
            ri = [sb("ri%d" % i, [128, 8], F32) for i in range(2)]
            idxf = sb("idxf", [128, 4], F32)
            idxi = [[sb("idxi%d_%d" % (i, k_), [128, 1], I32) for k_ in range(4)] for i in range(2)]
            rowi = [sb("rowi%d" % i, [128, 4], I32) for i in range(2)]
            b_W, b_mT, b_c, b_hp, b_sq, b_st, b_h1T, b_r, b_base = [Buf() for _ in range(9)]
            b_xc, b_h1, b_h1b, b_ri, b_idx, b_rowi = [[Buf(), Buf()] for _ in range(6)]
            d_W, d_mT, d_c = sch.dsem(), sch.dsem(), sch.dsem()
            d_xc, d_h1, d_sc, d_ri = [[sch.dsem(), sch.dsem()] for _ in range(4)]
            d_z = sch.dsem()
            sch.dma("pool", lambda e: e.dma_start(out=Wo[:], in_=w_out.rearrange("(k p) n -> p k n", p=128)), d_W, writes=[b_W])
            cl = lambda o, i: sch.dma("sp", lambda e: e.dma_start(out=o, in_=i), d_c, writes=[b_c])
            cl(gbc[:], ln_gb[0:1, :].partition_broadcast(128))
            cl(bbc[:], ln_gb[1:2, :].partition_broadcast(128))
            cl(Wr[:], w_router.rearrange("(k p) n -> p k n", p=128))
            cl(brb[:], b_router.partition_broadcast(128))
            cl(identf[:], cst["identf"])
            cl(ustr[:], cst["ustr"])
            cl(ebase[:], cst["ebase"])
            if ep:
                ebasey = sb("ebasey", [128, NE], F32)
                rowfy = sb("rowfy", [128, NE], F32)
                riy = sb("riy", [128, 4], F32)
                cl(ebasey[:], ebase_y_in)
            sch.op("dve", lambda e: e.memset(ones[:], 1.0), writes=[b_c])
            sch.op("dve", lambda e: e.memset(base[:], 0.0), writes=[b_base])
            sch.op("dve", lambda e: e.memset(zt[:], 0.0), writes=[b_c])
            nbz = 4096 // WX
            for q in range(NQX):
                for bz in range(NROW // (128 * nbz)):
                    dst = XG[q][bz * 128 * nbz:(bz + 1) * 128 * nbz, :].rearrange("(b p) f -> p b f", p=128)
                    sch.dma("sp", lambda e, dst=dst: e.dma_start(out=dst, in_=zt[:].rearrange("p (b f) -> p b f", b=nbz)),
                            d_z, reads=[b_c], writes=[bXG])
            npp = 0
            bc_reg = {}
            sch.prog["pool"].append(lambda e: bc_reg.__setitem__("r", e.to_reg(NROW - 1)))
            for c in range(NT):
                tok0 = c * 128
                cb = c % 2
                if c % (QN // 128) == 0:
                    sch.dma("sp", lambda e, tok0=tok0: e.dma_start(out=mTt[:], in_=MT[:, tok0:tok0 + QN].rearrange("(k p) t -> p k t", p=128)),
                            d_mT, reads=[bMT], writes=[b_mT])
                cl_ = c % (QN // 128)
                sch.dma("sp", lambda e, cb=cb, tok0=tok0: e.dma_start(out=xc[cb][:], in_=x[tok0:tok0 + 128, :]), d_xc[cb], writes=[b_xc[cb]])
                for nt in range(4):
                    pi = npp % 4
                    npp += 1
                    for k_ in range(KC):
                        sch.op("pe", mm(ps[pi][:, :], mTt[:, k_, cl_ * 128:(cl_ + 1) * 128], Wo[:, k_, nt * 512:(nt + 1) * 512], k_ == 0, k_ == KC - 1),
                               reads=[b_W, b_mT], writes=[psb[pi]])
                    sch.op("dve", lambda e, pi=pi, cb=cb, nt=nt: e.scalar_tensor_tensor(
                        out=hp[:, nt * 512:(nt + 1) * 512], in0=xc[cb][:, nt * 512:(nt + 1) * 512], scalar=DN_ALPHA, in1=ps[pi][:, :],
                        op0=ALU.mult, op1=ALU.add), reads=[psb[pi], b_xc[cb]], writes=[b_hp])
                layer_norm_tile(sb, hp, b_hp, h1[cb], b_h1[cb], gbc, bbc, b_c, sq, b_sq, st, b_st)
                sch.dma("sp", lambda e, cb=cb, tok0=tok0: e.dma_start(out=H1[tok0:tok0 + 128, :], in_=h1[cb][:]), d_h1[cb],
                        reads=[b_h1[cb]], writes=[bH1])
                sch.op("act", lambda e, cb=cb: e.activation(out=h1b[cb][:], in_=h1[cb][:], func=AF.Copy), reads=[b_h1[cb]], writes=[b_h1b[cb]])
                for q4 in range(4):
                    pi = 4 + q4 % 2
                    for j in range(4):
                        k_ = q4 * 4 + j
                        sch.op("pe", lambda e, pi=pi, j=j, k_=k_, cb=cb: e.transpose(ps[pi][:, j * 128:(j + 1) * 128],
                                                                                  h1[cb][:, k_ * 128:(k_ + 1) * 128], identf[:]),
                               reads=[b_h1[cb], b_c], writes=[psb[pi]])
                    sch.op("act", lambda e, pi=pi, q4=q4: e.activation(out=h1T[:, q4 * 4:(q4 + 1) * 4, :],
                                                                     in_=ps[pi][:, :].rearrange("p (a b) -> p a b", a=4), func=AF.Copy),
                           reads=[psb[pi]], writes=[b_h1T])
                for k_ in range(KC):
                    sch.op("pe", mm(ps[6][:, 0:NE], h1T[:, k_, :], Wr[:, k_, :], k_ == 0, k_ == KC - 1), reads=[b_h1T, b_c], writes=[psb[6]])
                R_ = rt
                dv = lambda fn, rd_=(), wr_=(): sch.op("dve", fn, reads=[b_r, b_c] + list(rd_), writes=[b_r] + list(wr_))
                dv(lambda e: e.tensor_tensor(out=R_["lg"][:], in0=ps[6][:, 0:NE], in1=brb[:], op=ALU.add), rd_=[psb[6]])
                dv(lambda e: e.max(out=v8[:], in_=R_["lg"][:]))
                dv(lambda e: e.tensor_scalar(out=R_["m4"][:], in0=R_["lg"][:], scalar1=v8[:, 3:4], scalar2=None, op0=ALU.is_ge))
                dv(lambda e: e.tensor_scalar(out=sm[:, 0:1], in0=v8[:, 0:1], scalar1=-1.0, scalar2=None, op0=ALU.mult))
                sch.op("act", lambda e: e.activation(out=R_["ex"][:], in_=R_["lg"][:], func=AF.Exp, bias=sm[:, 0:1]), reads=[b_r], writes=[b_r])
                dv(lambda e: e.tensor_tensor(out=R_["ex"][:], in0=R_["ex"][:], in1=R_["m4"][:], op=ALU.mult))
                dv(lambda e: e.tensor_reduce(out=sm[:, 1:2], in_=R_["ex"][:], axis=AX.X, op=ALU.add))
                dv(lambda e: e.reciprocal(out=sm[:, 2:3], in_=sm[:, 1:2]))
                dv(lambda e: e.tensor_scalar(out=R_["gd"][:], in0=R_["ex"][:], scalar1=sm[:, 2:3], scalar2=None, op0=ALU.mult))
                dv(lambda e: e.tensor_copy(out=mb16[:], in_=R_["m4"][:]))
                sch.op("pe", mm(ps[7][:, 0:NE], ustr[:], mb16[:], True, True), reads=[b_r, b_c], writes=[psb[7]])
                sch.op("pe", mm(ps[7][:, NE:2 * NE], ones[:], mb16[:], True, True), reads=[b_r, b_c], writes=[psb[7]])
                dv(lambda e: e.tensor_tensor(out=R_["slot"][:], in0=ps[7][:, 0:NE], in1=base[:], op=ALU.add), rd_=[psb[7], b_base])
                dv(lambda e: e.tensor_tensor(out=base[:], in0=ps[7][:, NE:2 * NE], in1=base[:], op=ALU.add), rd_=[psb[7], b_base], wr_=[b_base])
                dv(lambda e: e.tensor_scalar(out=R_["okm"][:], in0=R_["slot"][:], scalar1=float(CAP), scalar2=None, op0=ALU.is_lt))
                dv(lambda e: e.tensor_tensor(out=R_["okm"][:], in0=R_["okm"][:], in1=R_["m4"][:], op=ALU.mult))
                dv(lambda e: e.tensor_tensor(out=R_["rowf"][:], in0=R_["slot"][:], in1=ebase[:], op=ALU.add))
                if ep:
                    dv(lambda e: e.tensor_tensor(out=rowfy[:], in0=R_["slot"][:], in1=ebasey[:], op=ALU.add))
                bc_k = lambda ap: ap.unsqueeze(1).to_broadcast([128, 4, NE])
                dv(lambda e: e.tensor_tensor(out=oh4[:], in0=bc_k(R_["lg"][:]), in1=v8[:, 0:4].unsqueeze(2).to_broadcast([128, 4, NE]),
                                             op=ALU.is_equal))
                dv(lambda e: e.tensor_tensor(out=oh4[:], in0=oh4[:], in1=bc_k(R_["okm"][:]), op=ALU.mult))
                dv(lambda e: e.tensor_reduce(out=sm[:, 4:8], in_=oh4[:], axis=AX.X, op=ALU.add))
                dv(lambda e: e.tensor_tensor(out=t14[:], in0=oh4[:], in1=bc_k(R_["rowf"][:]), op=ALU.mult))
                dv(lambda e, cb=cb: e.tensor_reduce(out=ri[cb][:, 0:4], in_=t14[:], axis=AX.X, op=ALU.add), rd_=[b_ri[cb]], wr_=[b_ri[cb]])
                if ep:
                    dv(lambda e: e.tensor_tensor(out=t14[:], in0=oh4[:], in1=bc_k(rowfy[:]), op=ALU.mult))
                    dv(lambda e: e.tensor_reduce(out=riy[:, 0:4], in_=t14[:], axis=AX.X, op=ALU.add))
                dv(lambda e: e.tensor_tensor(out=t14[:], in0=oh4[:], in1=bc_k(R_["gd"][:]), op=ALU.mult))
                dv(lambda e, cb=cb: e.tensor_reduce(out=ri[cb][:, 4:8], in_=t14[:], axis=AX.X, op=ALU.add), rd_=[b_ri[cb]], wr_=[b_ri[cb]])
                dv(lambda e: e.tensor_scalar(out=idxf[:], in0=sm[:, 4:8], scalar1=-1.0e6, scalar2=1.0e6, op0=ALU.mult, op1=ALU.add))
                dv(lambda e, cb=cb: e.tensor_tensor(out=idxf[:], in0=idxf[:], in1=ri[cb][:, 0:4], op=ALU.add), rd_=[b_ri[cb]])
                for k_ in range(4):
                    dv(lambda e, k_=k_, cb=cb: e.tensor_copy(out=idxi[cb][k_][:], in_=idxf[:, k_:k_ + 1]), rd_=[b_idx[cb]], wr_=[b_idx[cb]])
                if ep:
                    dv(lambda e, cb=cb: e.tensor_copy(out=rowi[cb][:], in_=riy[:, 0:4]), rd_=[b_ri[cb], b_rowi[cb]], wr_=[b_rowi[cb]])
                else:
                    dv(lambda e, cb=cb: e.tensor_copy(out=rowi[cb][:], in_=ri[cb][:, 0:4]), rd_=[b_ri[cb], b_rowi[cb]], wr_=[b_rowi[cb]])
                for k_ in range(4):
                    for q in range(NQX):
                        sch.dma("pool", lambda e, k_=k_, cb=cb, q=q: e.indirect_dma_start(
                            out=XG[q][:, :], out_offset=bass.IndirectOffsetOnAxis(ap=idxi[cb][k_][:, 0:1], axis=0),
                            in_=h1b[cb][:, q * WX:(q + 1) * WX], in_offset=None, bounds_check=bc_reg["r"], oob_is_err=False),
                            d_sc[cb], reads=[b_h1b[cb], b_idx[cb], bXG], writes=[Buf()])
                sch.dma("sp", lambda e, cb=cb, tok0=tok0: e.dma_start(out=RI[tok0:tok0 + 128, :], in_=ri[cb][:]), d_ri[cb],
                        reads=[b_ri[cb]], writes=[bRI])
                for k_ in range(4):
                    sch.dma("sp", lambda e, cb=cb, tok0=tok0, k_=k_: e.dma_start(out=ROWI[k_][tok0:tok0 + 128, :], in_=rowi[cb][:, k_:k_ + 1]), d_ri[cb],
                            reads=[b_rowi[cb]], writes=[bRI])
            sch.barrier()
            sch.emit()

    if upto >= 4:
        stage_merge()
    if upto >= 5:
        stage_out()


    NEL = NE // 8 if ep else NE
    w_up = din("w_up", [NEL, D, 2 * D])
    w_down = din("w_down", [NEL, D, D])
    b_up_fm = din("b_up_fm", [128, NEL * 32])
    b_down = din("b_down", [NEL, D])
    Y = [dscr("Y%d" % q, [NROW, WY], F32) for q in range(NQY)]
    bY = Buf()
    if ep:
        XGr = [nc.dram_tensor("XGall%d" % q, [8 * NROW, WX], BF16, kind="Internal").ap() for q in range(NQX)]
        Yr = [nc.dram_tensor("Yall%d" % q, [8 * NROW, WY], F32, kind="Internal").ap() for q in range(NQY)]
        bXGr, bYr = Buf(), Buf()
        d_cc = sch.dsem()
        xg_idx = din("xg_idx", [128, NE * CAPB], I32)
    else:
        XGr, Yr, bXGr, bYr = XG, Y, bXG, bY

    def all_gather(srcs, dsts, bsrc, bdst):
        for src, dst in zip(srcs, dsts):
            sch.dma("pool", lambda e, src=src, dst=dst: e.collective_compute(
                "AllGather", ALU.bypass, replica_groups=[list(range(8))], ins=[src[:, :]], outs=[dst[:, :]]),
                d_cc, reads=[bsrc], writes=[bdst])
        sch.barrier()
        sch.emit()

    def stage_experts():
        with ExitStack() as es:
            def sb(name, shape, dt):
                return es.enter_context(nc.sbuf_tensor("e_" + name, shape, dt))
            xg = [sb("xg%d" % i, [128, D], BF16) for i in range(2)]
            xgT = sb("xgT", [128, KC, CAP], BF16)
            actT = sb("actT", [128, KC, CAP], BF16)
            NUPB, NDNB = 4, 2
            Wu = [sb("Wu%d" % i, [128, KC, 2, 256], BF16) for i in range(NUPB)]
            Wd = [sb("Wd%d" % i, [128, KC, 512], BF16) for i in range(NDNB)]
            bup = sb("bup", [128, 2, 32], F32)
            b_bup = [Buf(), Buf()]
            d_bup = [sch.dsem(), sch.dsem()]
            bdn = [sb("bdn%d" % i, [128, D], F32) for i in range(2)]
            identb = sb("identb", [128, 128], BF16)
            HN = CAP // 2
            tg_ = [sb("tg%d" % i, [128, HN], F32) for i in range(2)]
            ts_ = [sb("ts%d" % i, [128, HN], F32) for i in range(2)]
            tl_ = [sb("tl%d" % i, [128, HN], F32) for i in range(2)]
            yst = [sb("yst%d" % i, [128, 512], F32) for i in range(3)]
            b_xg, b_Wu, b_Wd = [Buf(), Buf()], [Buf() for _ in range(NUPB)], [Buf() for _ in range(NDNB)]
            b_xgT, b_actT, b_c = Buf(), Buf(), Buf()
            b_bdn = [Buf(), Buf()]
            b_tg, b_ts, b_tl = [Buf(), Buf()], [Buf(), Buf()], [Buf(), Buf()]
            b_yst = [Buf() for _ in range(3)]
            d_xg = [sch.dsem(), sch.dsem()]
            d_Wu = [sch.dsem() for _ in range(NUPB)]
            d_Wd = [sch.dsem() for _ in range(NDNB)]
            d_bdn = [sch.dsem(), sch.dsem()]
            d_yst = [sch.dsem() for _ in range(3)]
            d_c = sch.dsem()
            sch.dma("sp", lambda e: e.dma_start(out=identb[:], in_=cst["identb"]), d_c, writes=[b_c])
            if ep:
                xgi = sb("xgi", [128, NE * CAPB], I32)
                sch.dma("sp", lambda e: e.dma_start(out=xgi[:], in_=xg_idx), d_c, writes=[b_c])
            upieces, dpieces = [], []
            for ex in range(NE):
                wex_ = ex // 8 if ep else ex
                for pc in range(8):
                    upieces.append((wex_, pc))
                for nt in range(4):
                    dpieces.append((wex_, nt))
            PDU, PDD = NUPB, NDNB

            def issue_u(i):
                if i >= len(upieces):
                    return
                wex_, pc = upieces[i]
                bi = i % NUPB
                for hl in range(2):
                    src = w_up[wex_, :, hl * D + pc * 256:hl * D + (pc + 1) * 256].rearrange("(k p) n -> p k n", p=128)
                    sch.dma("pool", lambda e, bi=bi, hl=hl, src=src: e.dma_start(out=Wu[bi][:, :, hl, :], in_=src), d_Wu[bi], writes=[b_Wu[bi]])

            def issue_d(i):
                if i >= len(dpieces):
                    return
                wex_, nt = dpieces[i]
                bi = i % NDNB
                src = w_down[wex_, :, nt * 512:(nt + 1) * 512].rearrange("(k p) n -> p k n", p=128)
                sch.dma("pool", lambda e, bi=bi, src=src: e.dma_start(out=Wd[bi][:], in_=src), d_Wd[bi], writes=[b_Wd[bi]])
            for i in range(PDU):
                issue_u(i)
            for i in range(PDD):
                issue_d(i)
            uidx = 0
            didx = 0
            nps = 0
            nxg = 0
            nys = 0
            nt_ = 0
            for ex in range(NE):
                eb = ex % 2
                if ep:
                    wex = ex // 8
                    rbase = (ex % 8) * (NE // 8) * CAP + wex * CAP
                else:
                    wex = ex
                    rbase = ex * CAP
                sch.dma("sp", lambda e, eb=eb, wex=wex: e.dma_start(out=bdn[eb][:], in_=b_down[wex:wex + 1, :].partition_broadcast(128)),
                        d_bdn[eb], writes=[b_bdn[eb]])
                sch.dma("sp", lambda e, eb=eb, wex=wex: e.dma_start(out=bup[:, eb, :], in_=b_up_fm[:, wex * 32:(wex + 1) * 32]),
                        d_bup[eb], writes=[b_bup[eb]])
                for blk in range(CAPB):
                    xi = nxg % 2
                    nxg += 1
                    r0 = rbase + blk * 128
                    for q in range(NQX):
                        if ep:
                            jcol = ex * CAPB + blk
                            sch.dma("pool", lambda e, xi=xi, jcol=jcol, q=q: e.indirect_dma_start(
                                out=xg[xi][:, q * WX:(q + 1) * WX], out_offset=None, in_=XGr[q][:, :],
                                in_offset=bass.IndirectOffsetOnAxis(ap=xgi[:, jcol:jcol + 1], axis=0)),
                                d_xg[xi], reads=[bXGr, b_c], writes=[b_xg[xi]])
                        else:
                            sch.dma("sp", lambda e, xi=xi, r0=r0, q=q: e.dma_start(out=xg[xi][:, q * WX:(q + 1) * WX], in_=XGr[q][r0:r0 + 128, :]),
                                    d_xg[xi], reads=[bXGr], writes=[b_xg[xi]])
                    for q4 in range(4):
                        pi = 4 + nps % 2
                        nps += 1
                        pv = ps[pi].bitcast(BF16)
                        for j in range(4):
                            k_ = q4 * 4 + j
                            sch.op("pe", lambda e, pv=pv, j=j, k_=k_, xi=xi: e.transpose(pv[:, j * 128:(j + 1) * 128],
                                                                                      xg[xi][:, k_ * 128:(k_ + 1) * 128], identb[:]),
                                   reads=[b_xg[xi], b_c], writes=[psb[pi]])
                        sch.op("act", lambda e, pv=pv, q4=q4, blk=blk: e.activation(
                            out=xgT[:, q4 * 4:(q4 + 1) * 4, blk * 128:(blk + 1) * 128],
                            in_=pv[:, 0:512].rearrange("p (a b) -> p a b", a=4), func=AF.Copy), reads=[psb[pi]], writes=[b_xgT])
                for pc in range(8):
                    bi = uidx % NUPB
                    for jj in range(2):
                        j = pc * 2 + jj
                        for hf in range(2):
                            pg = (2 * nt_) % 4
                            pl = (2 * nt_ + 1) % 4
                            ti = nt_ % 2
                            nt_ += 1
                            for k_ in range(KC):
                                sch.op("pe", mm(ps[pg][:, 0:HN], Wu[bi][:, k_, 0, jj * 128:(jj + 1) * 128], xgT[:, k_, hf * HN:(hf + 1) * HN],
                                                k_ == 0, k_ == KC - 1), reads=[b_Wu[bi], b_xgT], writes=[psb[pg]])
                            for k_ in range(KC):
                                sch.op("pe", mm(ps[pl][:, 0:HN], Wu[bi][:, k_, 1, jj * 128:(jj + 1) * 128], xgT[:, k_, hf * HN:(hf + 1) * HN],
                                                k_ == 0, k_ == KC - 1), reads=[b_Wu[bi], b_xgT], writes=[psb[pl]])
                            bg = bup[:, eb, j:j + 1]
                            bl = bup[:, eb, 16 + j:17 + j]
                            sch.op("dve", lambda e, ti=ti, pg=pg, bg=bg: e.tensor_scalar(out=tg_[ti][:], in0=ps[pg][:, 0:HN], scalar1=bg, scalar2=7.0,
                                                                                     op0=ALU.add, op1=ALU.min),
                                   reads=[psb[pg], b_bup[eb]], writes=[b_tg[ti]], same=False)
                            sch.op("act", lambda e, ti=ti: e.activation(out=ts_[ti][:], in_=tg_[ti][:], func=AF.Sigmoid, scale=1.702),
                                   reads=[b_tg[ti]], writes=[b_ts[ti]])
                            sch.op("dve", lambda e, ti=ti, pl=pl, bl=bl: e.tensor_scalar(out=tl_[ti][:], in0=ps[pl][:, 0:HN], scalar1=bl, scalar2=7.0,
                                                                                     op0=ALU.add, op1=ALU.min),
                                   reads=[psb[pl], b_bup[eb]], writes=[b_tl[ti]], same=False)
                            sch.op("dve", lambda e, ti=ti: e.tensor_scalar(out=tl_[ti][:], in0=tl_[ti][:], scalar1=-7.0, scalar2=1.0,
                                                                          op0=ALU.max, op1=ALU.add), reads=[b_tl[ti]], writes=[b_tl[ti]], same=False)
                            sch.op("dve", lambda e, ti=ti: e.tensor_tensor(out=tg_[ti][:], in0=tg_[ti][:], in1=ts_[ti][:], op=ALU.mult),
                                   reads=[b_tg[ti], b_ts[ti]], writes=[b_tg[ti]], same=False)
                            sch.op("dve", lambda e, ti=ti, j=j, hf=hf: e.tensor_tensor(out=actT[:, j, hf * HN:(hf + 1) * HN], in0=tg_[ti][:], in1=tl_[ti][:],
                                                                                      op=ALU.mult),
                                   reads=[b_tg[ti], b_tl[ti]], writes=[b_actT], same=False)
                    issue_u(uidx + PDU)
                    uidx += 1
                for nt in range(4):
                    bi = didx % NDNB
                    for blk in range(CAPB):
                        pi = 6 + nps % 2
                        nps += 1
                        for k_ in range(KC):
                            sch.op("pe", mm(ps[pi][:, :], actT[:, k_, blk * 128:(blk + 1) * 128], Wd[bi][:, k_, :], k_ == 0, k_ == KC - 1),
                                   reads=[b_Wd[bi], b_actT], writes=[psb[pi]])
                        yi = nys % 3
                        nys += 1
                        sch.op("dve", lambda e, yi=yi, pi=pi, eb=eb, nt=nt: e.tensor_tensor(out=yst[yi][:], in0=ps[pi][:, :],
                                                                                       in1=bdn[eb][:, nt * 512:(nt + 1) * 512], op=ALU.add),
                               reads=[psb[pi], b_bdn[eb]], writes=[b_yst[yi]])
                        r0 = rbase + blk * 128
                        wst = min(WY, 512)
                        for hh in range(512 // wst):
                            c0 = nt * 512 + hh * wst
                            qy, cq = c0 // WY, c0 % WY
                            sch.dma("sp", lambda e, yi=yi, r0=r0, qy=qy, cq=cq, hh=hh, wst=wst: e.dma_start(
                                out=Y[qy][r0:r0 + 128, cq:cq + wst], in_=yst[yi][:, hh * wst:(hh + 1) * wst]),
                                d_yst[yi], reads=[b_yst[yi]], writes=[bY])
                    issue_d(didx + PDD)
                    didx += 1
            if debug:
                dx = dscr("dbg_xgT", [128, KC, CAP], BF16)
                da = dscr("dbg_actT", [128, KC, CAP], BF16)
                sch.dma("sp", lambda e: e.dma_start(out=dx, in_=xgT[:]), d_c, reads=[b_xgT], writes=[Buf()])
                sch.dma("sp", lambda e: e.dma_start(out=da, in_=actT[:]), d_c, reads=[b_actT], writes=[Buf()])
            sch.barrier()
            sch.emit()

    def stage_combine():
        with ExitStack() as es:
            def sb(name, shape, dt):
                return es.enter_context(nc.sbuf_tensor("c_" + name, shape, dt))
            yk = [[sb("yk%d_%d" % (i, k_), [128, D], F32) for k_ in range(4)] for i in range(2)]
            h1 = [sb("h1%d" % i, [128, D], F32) for i in range(2)]
            hp = sb("hp", [128, D], F32)
            sq = sb("sq", [128, D], BF16)
            ot = [sb("ot%d" % i, [128, D], F32) for i in range(2)]
            gbc = sb("gbc", [128, D], F32)
            bbc = sb("bbc", [128, D], F32)
            st = sb("st", [128, 8], F32)
            ri = [sb("ri%d" % i, [128, 8], F32) for i in range(2)]
            rw = [[sb("rw%d_%d" % (i, k_), [128, 1], I32) for k_ in range(4)] for i in range(2)]
            b_yk, b_h1, b_ot, b_ri, b_rw = [[Buf(), Buf()] for _ in range(5)]
            b_c, b_hp, b_sq, b_st = Buf(), Buf(), Buf(), Buf()
            d_c = sch.dsem()
            d_yk, d_h1, d_ot, d_ri, d_rw = [[sch.dsem(), sch.dsem()] for _ in range(5)]
            bout = Buf()
            sch.dma("sp", lambda e: e.dma_start(out=gbc[:], in_=ln_gb[2:3, :].partition_broadcast(128)), d_c, writes=[b_c])
            sch.dma("sp", lambda e: e.dma_start(out=bbc[:], in_=ln_gb[3:4, :].partition_broadcast(128)), d_c, writes=[b_c])
            for c in range(NT):
                tok0 = c * 128
                cb = c % 2
                sch.dma("sp", lambda e, cb=cb, tok0=tok0: e.dma_start(out=ri[cb][:], in_=RI[tok0:tok0 + 128, :]), d_ri[cb],
                        reads=[bRI], writes=[b_ri[cb]])
                for k_ in range(4):
                    sch.dma("sp", lambda e, cb=cb, tok0=tok0, k_=k_: e.dma_start(out=rw[cb][k_][:], in_=ROWI[k_][tok0:tok0 + 128, :]), d_rw[cb],
                            reads=[bRI], writes=[b_rw[cb]])
                sch.dma("sp", lambda e, cb=cb, tok0=tok0: e.dma_start(out=h1[cb][:], in_=H1[tok0:tok0 + 128, :]), d_h1[cb],
                        reads=[bH1], writes=[b_h1[cb]])
                for k_ in range(4):
                    for q in range(NQY):
                        sch.dma("pool", lambda e, cb=cb, k_=k_, q=q: e.indirect_dma_start(
                            out=yk[cb][k_][:, q * WY:(q + 1) * WY], out_offset=None, in_=Yr[q][:, :],
                            in_offset=bass.IndirectOffsetOnAxis(ap=rw[cb][k_][:, 0:1], axis=0)),
                            d_yk[cb], reads=[bYr, b_rw[cb]], writes=[b_yk[cb]])
                sch.op("act", lambda e, cb=cb: e.activation(out=hp[:], in_=h1[cb][:], func=AF.Copy, scale=DN_ALPHA), reads=[b_h1[cb]], writes=[b_hp])
                for k_ in range(4):
                    sch.op("dve", lambda e, cb=cb, k_=k_: e.scalar_tensor_tensor(out=hp[:], in0=yk[cb][k_][:], scalar=ri[cb][:, 4 + k_:5 + k_],
                                                                              in1=hp[:], op0=ALU.mult, op1=ALU.add),
                           reads=[b_yk[cb], b_ri[cb], b_hp], writes=[b_hp])
                layer_norm_tile(sb, hp, b_hp, ot[cb], b_ot[cb], gbc, bbc, b_c, sq, b_sq, st, b_st)
                sch.dma("sp", lambda e, cb=cb, tok0=tok0: e.dma_start(out=out[tok0:tok0 + 128, :], in_=ot[cb][:]), d_ot[cb],
                        reads=[b_ot[cb]], writes=[bout])
            sch.barrier()
            sch.emit()

    if upto >= 6:
        if ep:
            all_gather(XG, XGr, bXG, bXGr)
        stage_experts()
    if upto >= 7:
        if ep:
            all_gather(Y, Yr, bY, bYr)
        stage_combine()

    sch.barrier()
    sch.emit()
    return nc


def prep_inputs(inp, b, S, consts=None, ep=False):
    m = {}
    m["x"] = np.ascontiguousarray(inp["x"][b, :S])
    m["w_in"] = inp["w_in"][0]
    b_in = inp["b_in"][0]
    b_fm = np.zeros((128, NPF + 1), np.float32)
    for (kind, col0, ncols, dl, pfc) in fm_jobs():
        b_fm[:ncols, pfc] = b_in[col0:col0 + ncols]
    cols = list(range(1792, 2048)) + list(range(2304, 2560))
    for gi in range(3):
        c0 = 2584 + gi * 1536 + 1024
        cols += list(range(c0, c0 + 512))
    m["b_fm"] = b_fm
    m["b_tm"] = np.ascontiguousarray(b_in[cols][None, :])
    m.update(consts if consts is not None else host_consts(S))
    for k in ("cmp_k_w1", "cmp_v_w1", "cmp_k_w2", "cmp_v_w2"):
        m[k] = inp[k][0]
    m["posT_k"] = np.ascontiguousarray(inp["cmp_pos_k"][0].T)
    m["posT_v"] = np.ascontiguousarray(inp["cmp_pos_v"][0].T)
    m["w_br_nsa"] = inp["w_br_nsa"][0]
    m["w_br_dil"] = inp["w_br_dil"][0]
    m["w_out"] = inp["w_out"][0]
    m["ln_gb"] = np.ascontiguousarray(np.stack([inp["ln1_g"][0], inp["ln1_b"][0], inp["ln2_g"][0], inp["ln2_b"][0]], 0))
    m["w_router"] = inp["w_router"][0]
    m["b_router"] = inp["b_router"]
    esl = slice(b * (NE // 8), (b + 1) * (NE // 8)) if ep else slice(0, NE)
    nel = NE // 8 if ep else NE
    m["w_up"] = inp["w_up"][0][esl]
    m["w_down"] = inp["w_down"][0][esl]
    m["b_up_fm"] = np.ascontiguousarray(inp["b_up"][0][esl].reshape(nel, 32, 128).transpose(2, 0, 1).reshape(128, nel * 32))
    m["b_down"] = inp["b_down"][0][esl]
    if ep:
        cap = CAPB * 128
        nrow = NE * cap
        e_ = np.arange(NE)
        m["ebase_y"] = np.tile(((e_ // 4) * nrow + b * 4 * cap + (e_ % 4) * cap).astype(np.float32)[None, :], (128, 1))
        ex = np.arange(NE)
        le, sc = ex // 8, ex % 8
        blk = np.arange(CAPB)
        base = (sc[:, None] * nrow + (b * 4 + le[:, None]) * cap + blk[None, :] * 128).reshape(-1)
        m["xg_idx"] = np.ascontiguousarray((base[None, :] + np.arange(128)[:, None]).astype(np.int32))
    return m


def kernel(**inputs):
    S = 4096
    inp = {k: np.asarray(v) for k, v in inputs.items()}
    consts = host_consts(S)
    nc = build(S, ep=False)
    in_maps = [prep_inputs(inp, b, S, consts, ep=False) for b in range(8)]
    res = run_bass_kernel_spmd(nc, in_maps, core_ids=list(range(8)))
    return np.stack([np.asarray(r["out"], dtype=np.float32) for r in res.results], 0)
```

```python
from contextlib import ExitStack
import numpy as np
import ml_dtypes
import concourse.bass as bass
import concourse.mybir as mybir
from concourse.bass_utils import run_bass_kernel_spmd

F32 = mybir.dt.float32
BF16 = mybir.dt.bfloat16
I32 = mybir.dt.int32
U32 = mybir.dt.uint32
AF = mybir.ActivationFunctionType
ALU = mybir.AluOpType
AX = mybir.AxisListType

D = 2048
KC = 16
IN_W = 11288
NEGB = -30000.0
SCALE = 128 ** -0.5
DILS = (1, 4, 16)
DN_ALPHA = 2.0 ** 0.25
LN_EPS = 1e-5
NE = 32
CAPB = 5
DBG_BR = "csw"
SAME_ENG_SYNC = True


class Buf:
    __slots__ = ("name", "w", "r")

    def __init__(self, name=""):
        self.name = name
        self.w = None
        self.r = {}


class DSem:
    __slots__ = ("key", "sem", "cnt")


class Sched:
    ENG = ("pe", "act", "dve", "pool", "sp")

    def __init__(self, nc):
        self.nc = nc
        self.prog = {e: [] for e in self.ENG}
        self.cnt = {e: 0 for e in self.ENG}
        self.sems = {}
        self.seen = {e: {} for e in self.ENG}
        self.dsems = []
        for e in ("pe", "act", "dve", "pool"):
            self.sems[e] = nc.alloc_semaphore("prog_" + e)

    def dsem(self):
        d = DSem()
        d.key = "d%d" % len(self.dsems)
        d.sem = self.nc.alloc_semaphore("dma_" + d.key)
        d.cnt = 0
        self.sems[d.key] = d.sem
        self.dsems.append(d)
        return d

    def _wait(self, eng, k, i):
        seen = self.seen[eng]
        if k == eng and (eng == "pe" or not SAME_ENG_SYNC):
            return
        if i > 0 and seen.get(k, 0) < i:
            seen[k] = i
            sem = self.sems[k]
            self.prog[eng].append(lambda e, sem=sem, i=i: e.wait_ge(sem, i))

    def _waits(self, eng, reads, writes, same=True):
        need = {}

        def add(dep):
            if dep is not None and (same or dep[0] != eng) and need.get(dep[0], 0) < dep[1]:
                need[dep[0]] = dep[1]
        for b in reads:
            add(b.w)
        for b in writes:
            add(b.w)
            for k, i in b.r.items():
                add((k, i))
        for k, i in need.items():
            self._wait(eng, k, i)

    def op(self, eng, fn, reads=(), writes=(), same=True):
        self._waits(eng, reads, writes, same)
        self.cnt[eng] += 1
        idx = self.cnt[eng]
        sem = self.sems[eng]
        self.prog[eng].append(lambda e, fn=fn, sem=sem: fn(e).then_inc(sem, 1))
        for b in reads:
            if b.r.get(eng, 0) < idx:
                b.r[eng] = idx
        for b in writes:
            b.w = (eng, idx)
            b.r = {}

    def dma(self, q, fn, ds, reads=(), writes=()):
        self._waits(q, reads, writes)
        ds.cnt += 16
        idx = ds.cnt
        sem = ds.sem
        self.prog[q].append(lambda e, fn=fn, sem=sem: fn(e).then_inc(sem, 16))
        for b in reads:
            if b.r.get(ds.key, 0) < idx:
                b.r[ds.key] = idx
        for b in writes:
            b.w = (ds.key, idx)
            b.r = {}

    def barrier(self):
        for e in self.ENG:
            for k in ("pe", "act", "dve", "pool"):
                self._wait(e, k, self.cnt[k])
            for d in self.dsems:
                self._wait(e, d.key, d.cnt)

    def emit(self):
        nc = self.nc
        prog = self.prog
        with nc.Block() as block:
            @block.tensor
            def _(e):
                for f in prog["pe"]:
                    f(e)

            @block.scalar
            def _(e):
                for f in prog["act"]:
                    f(e)

            @block.vector
            def _(e):
                for f in prog["dve"]:
                    f(e)

            @block.gpsimd
            def _(e):
                for f in prog["pool"]:
                    f(e)

            @block.sync
            def _(e):
                for f in prog["sp"]:
                    f(e)
        self.prog = {e: [] for e in self.ENG}


def fm_jobs():
    jobs = []
    for hd in range(8):
        jobs.append(("rope", hd * 128, 128, 1, hd))
    for g in range(2):
        jobs.append(("rope", 1024 + g * 128, 128, 1, 8 + g))
    for g in range(2):
        jobs.append(("plain", 1280 + g * 128, 128, 1, 10 + g))
    for g in range(2):
        jobs.append(("rope", 1536 + g * 128, 128, 1, 12 + g))
    for g in range(2):
        jobs.append(("rope", 2048 + g * 128, 128, 1, 14 + g))
    for gi in range(3):
        base = 2584 + gi * 1536
        for hd in range(4):
            jobs.append(("rope", base + hd * 128, 128, DILS[gi], 16 + gi * 4 + hd))
        for hd in range(4):
            jobs.append(("rope", base + 512 + hd * 128, 128, DILS[gi], 28 + gi * 4 + hd))
    for i in range(16):
        jobs.append(("sig", 7192 + i * 128, 128, 1, 40 + i))
    for i in range(16):
        jobs.append(("sig", 9240 + i * 128, 128, 1, 56 + i))
    jobs.append(("gl", 2560, 24, 1, 72))
    return jobs


NPF = 72


def host_consts(S):
    c = {}
    c["identb"] = np.eye(128, dtype=np.float32).astype(ml_dtypes.bfloat16)
    c["identf"] = np.eye(128, dtype=np.float32)
    pos = np.arange(S, dtype=np.float32)
    inv = (np.float32(10000.0) ** (-np.arange(0, 128, 2, dtype=np.float32) / np.float32(128))).astype(np.float32)
    ang = (pos[:, None] * inv[None, :]).astype(np.float32)
    cos = np.cos(ang).astype(np.float32).T
    sin = np.sin(ang).astype(np.float32).T
    c["cosT"] = np.ascontiguousarray(np.concatenate([cos, cos], 0))
    c["sinT"] = np.ascontiguousarray(np.concatenate([sin, -sin], 0))
    NSLC = S // 64
    NCMP = (S - 32) // 16 + 1
    bf = ml_dtypes.bfloat16
    sidx = np.arange(S)
    c["Eall"] = (sidx[None, :] // 64 == np.arange(NSLC)[:, None]).astype(np.float32).astype(bf)
    sl = np.arange(128)[:, None]
    tl = np.arange(512)[None, :]

    def band(lo, hi, dlt):
        v = tl - dlt - sl
        return np.where((v >= lo) & (v <= hi), 0.0, NEGB).astype(np.float32)
    tiles = [band(0, 1 << 30, 128 * k) for k in range(4)]
    tiles += [band(0, 511, -512 + 128 * k) for k in range(8)]
    tiles += [band(0, 128, -128 + 128 * k) for k in range(5)]
    c["BT"] = np.ascontiguousarray(np.stack(tiles, 1)).astype(bf)
    cidx = np.arange(256)
    cm = np.where((16 * cidx[:, None] + 31 <= sidx[None, :]) & (cidx[:, None] < NCMP), 0.0, NEGB).astype(np.float32)
    c["CMB"] = np.ascontiguousarray(cm.reshape(2, 128, S).transpose(1, 0, 2)).astype(bf)
    j = np.arange(NSLC)[None, :]
    cur = (sidx // 64)[:, None]
    forced = (j == 0) | (j == cur) | (j == cur - 1)
    future = j > cur
    c["KEEP"] = np.where(forced | future, 0.0, 1.0).astype(np.float32)
    c["ADD"] = np.where(forced, 1e9, np.where(future, -1e30, 0.0)).astype(np.float32)
    cs = cidx[:, None] * 16
    ov = (cs < (j + 1) * 64) & (cs + 32 > j * 64) & (cidx[:, None] < NCMP)
    c["ovl"] = ov.astype(np.float32).astype(bf)
    p_ = np.arange(128)
    c["ustr"] = (p_[:, None] < p_[None, :]).astype(np.float32).astype(bf)
    c["ebase"] = np.tile((np.arange(NE, dtype=np.float32) * (CAPB * 128))[None, :], (128, 1)).astype(np.float32)
    return c


CONST_SPECS = {
    "identb": ([128, 128], BF16), "identf": ([128, 128], F32),
    "cosT": ([128, None], F32), "sinT": ([128, None], F32),
    "Eall": ([-64, None], BF16), "BT": ([128, 17, 512], BF16), "CMB": ([128, 2, None], BF16),
    "KEEP": ([None, -64], F32), "ADD": ([None, -64], F32), "ovl": ([256, -64], BF16),
    "ustr": ([128, 128], BF16), "ebase": ([128, NE], F32),
}


def build(S, upto=99, debug=False, ep=False):
    nc = bass.Bass("TRN2", target_bir_lowering=False)
    okind = "ExternalOutput" if debug else "Internal"

    def din(name, shape, dt=F32):
        return nc.dram_tensor(name, shape, dt, kind="ExternalInput").ap()

    def dscr(name, shape, dt):
        return nc.dram_tensor(name, shape, dt, kind=okind).ap()

    x = din("x", [S, D])
    w_in = din("w_in", [D, IN_W])
    b_fm = din("b_fm", [128, NPF + 1])
    b_tm = din("b_tm", [1, 2048])
    cst = {}
    for k, (shp, dt) in CONST_SPECS.items():
        cst[k] = din(k, [S if s is None else (S // 64 if s == -64 else s) for s in shp], dt)
    PF = dscr("PF", [NPF * 128, S], BF16)
    G = dscr("G", [24, S], F32)
    PTn = dscr("PTn", [S, 512], BF16)
    PTd = [dscr("PTd%d" % g, [S, 512], BF16) for g in range(3)]
    out = nc.dram_tensor("out", [S, D], F32, kind="ExternalOutput").ap()

    sch = Sched(nc)
    ps = [nc.alloc_psum_tensor("ps%d" % i, [128, 512], F32).ap() for i in range(8)]
    psb = [Buf("ps%d" % i) for i in range(8)]
    bPF, bG, bPTn = Buf("PF"), Buf("G"), Buf("PTn")
    bPTd = [Buf("PTd%d" % g) for g in range(3)]

    def stage1():
        with ExitStack() as es:
            def sb(name, shape, dt):
                return es.enter_context(nc.sbuf_tensor("s1_" + name, shape, dt))
            HT = min(S, 2048)
            NH = S // HT
            xT = sb("xT", [128, KC, HT], BF16)
            xb = [sb("xb%d" % i, [128, D], BF16) for i in range(2)]
            identb = sb("identb", [128, 128], BF16)
            cosT = sb("cosT", [128, HT], F32)
            sinT = sb("sinT", [128, HT], F32)
            bfm = sb("bfm", [128, NPF + 1], F32)
            btm = sb("btm", [128, 2048], F32)
            wt = [sb("wt%d" % i, [128, KC, 512], BF16) for i in range(2)]
            stg = [sb("stg%d" % i, [128, HT], BF16) for i in range(2)]
            stgG = sb("stgG", [24, HT], F32)
            stgT = [sb("stgT%d" % i, [128, 512], BF16) for i in range(2)]
            tA = [sb("tA%d" % i, [128, 512], F32) for i in range(2)]
            t1 = [sb("t1%d" % i, [128, 512], F32) for i in range(2)]
            t2 = [sb("t2%d" % i, [128, 512], F32) for i in range(2)]
            b_xT = [Buf() for _ in range(HT // 128)]
            b_xb = [Buf(), Buf()]
            b_c, b_cos, b_sin = Buf(), Buf(), Buf()
            b_wt = [Buf(), Buf()]
            b_stg = [Buf(), Buf()]
            b_stgG = Buf()
            b_stgT = [Buf(), Buf()]
            b_tA, b_t1, b_t2 = [Buf(), Buf()], [Buf(), Buf()], [Buf(), Buf()]
            d_c = sch.dsem()
            d_xb = [sch.dsem(), sch.dsem()]
            d_wt = [sch.dsem(), sch.dsem()]
            d_stg = [sch.dsem(), sch.dsem()]
            d_stgT = [sch.dsem(), sch.dsem()]
            d_tab = sch.dsem()
            sch.dma("sp", lambda e: e.dma_start(out=identb[:], in_=cst["identb"]), d_c, writes=[b_c])
            sch.dma("sp", lambda e: e.dma_start(out=bfm[:], in_=b_fm), d_c, writes=[b_c])
            sch.dma("sp", lambda e: e.dma_start(out=btm[:], in_=b_tm.partition_broadcast(128)), d_c, writes=[b_c])
            jobs = fm_jobs()
            cnt = {"ps": 0, "wt": 0, "stg": 0, "stgT": 0, "t": 0, "ev": 0}
            for h in range(NH):
                h0 = h * HT
                sch.dma("sp", lambda e, h0=h0: e.dma_start(out=cosT[:], in_=cst["cosT"][:, h0:h0 + HT]), d_tab, writes=[b_cos])
                sch.dma("sp", lambda e, h0=h0: e.dma_start(out=sinT[:], in_=cst["sinT"][:, h0:h0 + HT]), d_tab, writes=[b_sin])
                for c in range(HT // 128):
                    i = c % 2
                    tok0 = h0 + c * 128
                    sch.dma("pool", lambda e, i=i, tok0=tok0: e.dma_start(out=xb[i][:], in_=x[tok0:tok0 + 128, :]),
                            d_xb[i], writes=[b_xb[i]])
                    for q4 in range(4):
                        pi = cnt["ps"] % 4
                        cnt["ps"] += 1
                        pv = ps[pi].bitcast(BF16)
                        for j in range(4):
                            kc = q4 * 4 + j
                            sch.op("pe", lambda e, pv=pv, j=j, kc=kc, i=i: e.transpose(
                                pv[:, j * 128:(j + 1) * 128], xb[i][:, kc * 128:(kc + 1) * 128], identb[:]),
                                reads=[b_xb[i], b_c], writes=[psb[pi]])
                        src = pv[:, 0:512].rearrange("p (a b) -> p a b", a=4)
                        dst = xT[:, q4 * 4:(q4 + 1) * 4, c * 128:(c + 1) * 128]
                        if (c * 4 + q4) % 2 == 0:
                            sch.op("act", lambda e, src=src, dst=dst: e.activation(out=dst, in_=src, func=AF.Copy),
                                   reads=[psb[pi]], writes=[b_xT[c]])
                        else:
                            sch.op("dve", lambda e, src=src, dst=dst: e.tensor_copy(out=dst, in_=src),
                                   reads=[psb[pi]], writes=[b_xT[c]])
                ngrp = (len(jobs) + 3) // 4
                for gi in range(ngrp):
                    grp = jobs[gi * 4:(gi + 1) * 4]
                    wi = cnt["wt"] % 2
                    cnt["wt"] += 1
                    j0 = 0
                    while j0 < len(grp):
                        j1 = j0 + 1
                        while j1 < len(grp) and grp[j1][1] == grp[j1 - 1][1] + grp[j1 - 1][2] and grp[j1 - 1][2] == 128:
                            j1 += 1
                        c0 = grp[j0][1]
                        ncol = sum(g_[2] for g_ in grp[j0:j1])
                        src = w_in[:, c0:c0 + ncol].rearrange("(kc p) n -> p kc n", p=128)
                        dst = wt[wi][:, :, j0 * 128:j0 * 128 + ncol]
                        sch.dma("pool", lambda e, src=src, dst=dst: e.dma_start(out=dst, in_=src), d_wt[wi], writes=[b_wt[wi]])
                        j0 = j1
                    for jj, (kind, col0, ncols, dl, pfc) in enumerate(grp):
                        si = cnt["stg"] % 2
                        if kind != "gl":
                            cnt["stg"] += 1
                        for tt in range(HT // 512):
                            pi = 4 + cnt["ps"] % 4
                            cnt["ps"] += 1
                            for kc in range(KC):
                                sch.op("pe", lambda e, pi=pi, wi=wi, jj=jj, ncols=ncols, kc=kc, tt=tt: e.matmul(
                                    ps[pi][0:ncols, :], wt[wi][:, kc, jj * 128:jj * 128 + ncols], xT[:, kc, tt * 512:(tt + 1) * 512],
                                    start=(kc == 0), stop=(kc == KC - 1)),
                                    reads=[b_wt[wi]] + b_xT[tt * 4:(tt + 1) * 4], writes=[psb[pi]])
                            bias = bfm[0:ncols, pfc:pfc + 1]
                            tsl = slice(tt * 512, (tt + 1) * 512)
                            if kind == "plain":
                                sch.op("act", lambda e, pi=pi, si=si, bias=bias, tsl=tsl: e.activation(
                                    out=stg[si][:, tsl], in_=ps[pi][:], func=AF.Identity, bias=bias),
                                    reads=[psb[pi], b_c], writes=[b_stg[si]])
                            elif kind == "sig":
                                sch.op("act", lambda e, pi=pi, si=si, bias=bias, tsl=tsl: e.activation(
                                    out=stg[si][:, tsl], in_=ps[pi][:], func=AF.Sigmoid, bias=bias),
                                    reads=[psb[pi], b_c], writes=[b_stg[si]])
                            elif kind == "gl":
                                sch.op("act", lambda e, pi=pi, bias=bias, tsl=tsl: e.activation(
                                    out=stgG[:, tsl], in_=ps[pi][0:24, :], func=AF.Sigmoid, bias=bias),
                                    reads=[psb[pi], b_c], writes=[b_stgG])
                            else:
                                ti = cnt["t"] % 2
                                cnt["t"] += 1
                                sch.op("act", lambda e, pi=pi, ti=ti, bias=bias: e.activation(
                                    out=tA[ti][:], in_=ps[pi][:], func=AF.Identity, bias=bias),
                                    reads=[psb[pi], b_c], writes=[b_tA[ti]])
                                sch.op("dve", lambda e, ti=ti, tsl=tsl: e.tensor_tensor(
                                    out=t1[ti][:], in0=tA[ti][:], in1=cosT[:, tsl], op=ALU.mult),
                                    reads=[b_tA[ti], b_cos], writes=[b_t1[ti]])
                                sch.op("pool", lambda e, ti=ti, tsl=tsl: e.tensor_tensor(
                                    out=t2[ti][0:64, :], in0=tA[ti][64:128, :], in1=sinT[64:128, tsl], op=ALU.mult),
                                    reads=[b_tA[ti], b_sin], writes=[b_t2[ti]])
                                sch.op("pool", lambda e, ti=ti, tsl=tsl: e.tensor_tensor(
                                    out=t2[ti][64:128, :], in0=tA[ti][0:64, :], in1=sinT[0:64, tsl], op=ALU.mult),
                                    reads=[b_tA[ti], b_sin], writes=[b_t2[ti]])
                                if dl == 1:
                                    o_ap = stg[si][:, tsl]
                                    i0 = t1[ti][:]
                                    i1 = t2[ti][:]
                                else:
                                    npos = 512 // dl
                                    il0 = tt * npos
                                    o_ap = stg[si][:].rearrange("p (r i) -> p r i", r=dl)[:, :, il0:il0 + npos]
                                    i0 = t1[ti][:].rearrange("p (i r) -> p r i", r=dl)
                                    i1 = t2[ti][:].rearrange("p (i r) -> p r i", r=dl)
                                sch.op("dve", lambda e, o_ap=o_ap, i0=i0, i1=i1: e.tensor_tensor(
                                    out=o_ap, in0=i0, in1=i1, op=ALU.add),
                                    reads=[b_t1[ti], b_t2[ti]], writes=[b_stg[si]])
                        if kind == "gl":
                            sch.dma("sp", lambda e, h0=h0: e.dma_start(out=G[:, h0:h0 + HT], in_=stgG[:]), d_stg[0],
                                    reads=[b_stgG], writes=[bG])
                        elif dl == 1:
                            sch.dma("sp", lambda e, pfc=pfc, si=si, h0=h0: e.dma_start(
                                out=PF[pfc * 128:(pfc + 1) * 128, h0:h0 + HT], in_=stg[si][:]), d_stg[si],
                                reads=[b_stg[si]], writes=[bPF])
                        else:
                            L = S // dl
                            hl = HT // dl
                            dst = PF[pfc * 128:(pfc + 1) * 128, :].rearrange("p (r i) -> p r i", r=dl)[:, :, h * hl:(h + 1) * hl]
                            src = stg[si][:].rearrange("p (r i) -> p r i", r=dl)
                            sch.dma("sp", lambda e, dst=dst, src=src: e.dma_start(out=dst, in_=src), d_stg[si],
                                    reads=[b_stg[si]], writes=[bPF])
                tmj = [(None, 1, PTn, bPTn)] + [(2584 + gi * 1536 + 1024, DILS[gi], PTd[gi], bPTd[gi]) for gi in range(3)]
                for ti_, (col0, dl, dst_t, dst_b) in enumerate(tmj):
                    wi = cnt["wt"] % 2
                    cnt["wt"] += 1
                    if col0 is None:
                        for half, c0 in enumerate((1792, 2304)):
                            src = w_in[:, c0:c0 + 256].rearrange("(kc p) n -> p kc n", p=128)
                            dst = wt[wi][:, :, half * 256:(half + 1) * 256]
                            sch.dma("pool", lambda e, src=src, dst=dst: e.dma_start(out=dst, in_=src), d_wt[wi], writes=[b_wt[wi]])
                    else:
                        src = w_in[:, col0:col0 + 512].rearrange("(kc p) n -> p kc n", p=128)
                        sch.dma("pool", lambda e, src=src, wi=wi: e.dma_start(out=wt[wi][:], in_=src), d_wt[wi], writes=[b_wt[wi]])
                    hl = HT // dl
                    npos = min(128, hl)
                    for r in range(dl):
                        for j in range(hl // npos):
                            pi = 4 + cnt["ps"] % 4
                            cnt["ps"] += 1
                            t_lo = r + dl * npos * j
                            xbufs = b_xT[(t_lo // 128):((t_lo + dl * (npos - 1)) // 128) + 1]
                            for kc in range(KC):
                                lhsT = xT[:, kc, t_lo:t_lo + dl * (npos - 1) + 1:dl]
                                sch.op("pe", lambda e, pi=pi, wi=wi, kc=kc, lhsT=lhsT, npos=npos: e.matmul(
                                    ps[pi][0:npos, :], lhsT, wt[wi][:, kc, :], start=(kc == 0), stop=(kc == KC - 1)),
                                    reads=[b_wt[wi]] + xbufs, writes=[psb[pi]])
                            si = cnt["stgT"] % 2
                            cnt["stgT"] += 1
                            bsl = btm[0:npos, ti_ * 512:(ti_ + 1) * 512]
                            sch.op("dve", lambda e, pi=pi, si=si, bsl=bsl, npos=npos: e.tensor_tensor(
                                out=stgT[si][0:npos, :], in0=ps[pi][0:npos, :], in1=bsl, op=ALU.add),
                                reads=[psb[pi], b_c], writes=[b_stgT[si]])
                            row0 = r * (S // dl) + h * hl + j * npos
                            sch.dma("sp", lambda e, dst_t=dst_t, row0=row0, npos=npos, si=si: e.dma_start(
                                out=dst_t[row0:row0 + npos, :], in_=stgT[si][0:npos, :]), d_stgT[si],
                                reads=[b_stgT[si]], writes=[dst_b])
            sch.barrier()
            sch.emit()

    if upto >= 1:
        stage1()

    NCMP = (S - 32) // 16 + 1
    NSLC = S // 64
    NT = S // 128
    QN = min(512, S)
    NQT = S // QN
    ON = dscr("ON", [1024, S], BF16)
    OD = dscr("OD", [512, S], BF16)
    bON, bOD = Buf("ON"), Buf("OD")
    cmp_w1 = [din("cmp_k_w1", [4096, 256]), din("cmp_v_w1", [4096, 256])]
    cmp_w2 = [din("cmp_k_w2", [256, 128]), din("cmp_v_w2", [256, 128])]
    posT = [din("posT_k", [128, 32]), din("posT_v", [128, 32])]

    class ACtx:
        pass

    def make_actx(sb, tag):
        a = ACtx()
        a.pT = [sb(tag + "pT%d" % i, [128, 512], BF16) for i in range(4)]
        a.b_pT = [Buf() for _ in range(4)]
        a.rd = [sb(tag + "rd%d" % i, [128, 512], F32) for i in range(2)]
        a.b_rd = [Buf(), Buf()]
        a.tmp = [sb(tag + "tmp%d" % i, [128, 512], F32) for i in range(2)]
        a.b_tmp = [Buf(), Buf()]
        a.n = 0
        a.k = 0
        a.e = 0
        return a

    def mm(out_ap, l_ap, r_ap, st, sp_):
        return lambda e: e.matmul(out_ap, l_ap, r_ap, start=st, stop=sp_)

    def attn_tile(a, qT, qbufs, N, chunks, ones_ap, cbuf):
        ni = 2 + a.n % 2
        di = 4 + a.n % 2
        a.n += 1
        nch = len(chunks)

        SBK = (0, 1, 7)

        def emit_s(i):
            ch = chunks[i]
            si = SBK[(a.k + i) % 3]
            kn = ch["kn"]
            nx = len(ch["extra"])
            sch.op("pe", mm(ps[si][0:kn, 0:N], ch["kT"], qT, True, nx == 0),
                   reads=list(qbufs) + list(ch["kvbufs"]), writes=[psb[si]])
            for xi, (l_ap, r_ap, bufs) in enumerate(ch["extra"]):
                sch.op("pe", mm(ps[si][0:kn, 0:N], l_ap, r_ap, False, xi == nx - 1), reads=list(bufs), writes=[psb[si]])
            pi = (a.k + i) % 4
            sch.op("act", lambda e, o=a.pT[pi][0:kn, 0:N], i_=ps[si][0:kn, 0:N]: e.activation(out=o, in_=i_, func=AF.Exp, scale=SCALE),
                   reads=[psb[si]], writes=[a.b_pT[pi]])
        emit_s(0)
        if nch > 1:
            emit_s(1)
        for i in range(nch):
            if i + 2 < nch:
                emit_s(i + 2)
            ch = chunks[i]
            kn = ch["kn"]
            pi = (a.k + i) % 4
            sch.op("pe", mm(ps[ni][:, 0:N], ch["v"], a.pT[pi][0:kn, 0:N], i == 0, i == nch - 1),
                   reads=[a.b_pT[pi]] + list(ch["kvbufs"]), writes=[psb[ni]])
            sch.op("pe", mm(ps[di][:, 0:N], ones_ap[0:kn, :], a.pT[pi][0:kn, 0:N], i == 0, i == nch - 1),
                   reads=[a.b_pT[pi], cbuf], writes=[psb[di]])
        a.k += nch
        return ni, di

    def attn_epilogue(a, ni, di, N, gate_ap, gbufs, acc_ap, acc_buf, first):
        ri = a.e % 2
        a.e += 1
        rd, brd = a.rd[ri], a.b_rd[ri]
        sch.op("dve", lambda e: e.tensor_scalar_max(out=rd[:, 0:N], in0=ps[di][:, 0:N], scalar1=1e-30), reads=[psb[di]], writes=[brd])
        wide = N >= 256
        sch.op("dve", lambda e: e.reciprocal(out=rd[:, 0:N], in_=rd[:, 0:N]), reads=[brd], writes=[brd], same=not wide)
        if gate_ap is not None:
            sch.op("dve", lambda e: e.tensor_tensor(out=rd[:, 0:N], in0=rd[:, 0:N], in1=gate_ap, op=ALU.mult),
                   reads=[brd] + list(gbufs), writes=[brd], same=not wide)
        if first:
            sch.op("dve", lambda e: e.tensor_tensor(out=acc_ap, in0=ps[ni][:, 0:N], in1=rd[:, 0:N], op=ALU.mult),
                   reads=[psb[ni], brd], writes=[acc_buf], same=not wide)
        else:
            tm, btm_ = a.tmp[ri], a.b_tmp[ri]
            sch.op("dve", lambda e: e.tensor_tensor(out=tm[:, 0:N], in0=ps[ni][:, 0:N], in1=rd[:, 0:N], op=ALU.mult),
                   reads=[psb[ni], brd], writes=[btm_], same=not wide)
            sch.op("pool", lambda e: e.tensor_tensor(out=acc_ap, in0=acc_ap, in1=tm[:, 0:N], op=ALU.add),
                   reads=[btm_, acc_buf], writes=[acc_buf])

    def stage_nsa(g):
        tg = "n%d_" % g
        with ExitStack() as es:
            def sb(name, shape, dt):
                return es.enter_context(nc.sbuf_tensor(tg + name, shape, dt))
            QT = sb("QT", [128, 4, S], BF16)
            KsT = sb("KsT", [128, S], BF16)
            KwT = sb("KwT", [128, S], BF16)
            Vs = sb("Vs", [128, NT, 128], BF16)
            Vw = sb("Vw", [128, NT, 128], BF16)
            kcT = sb("kcT", [128, 256], BF16)
            vc = sb("vc", [128, 2, 128], BF16)
            MbT = sb("MbT", [NSLC, S], BF16)
            Eall = sb("Eall", [NSLC, S], BF16)
            BT = sb("BT", [128, 12, 512], BF16)
            KEEP = sb("KEEP", [128, NT, NSLC], F32)
            ADD = sb("ADD", [128, NT, NSLC], F32)
            ovl = sb("ovl", [128, 2, NSLC], BF16)
            identb = sb("identb", [128, 128], BF16)
            ones = sb("ones", [128, 128], BF16)
            zer = sb("zer", [128, 256], BF16)
            b_q, b_kv, b_c, b_kc, b_MbT = Buf(), Buf(), Buf(), Buf(), Buf()
            d_l = sch.dsem()
            ld = lambda o, i, bufs: sch.dma("sp", lambda e: e.dma_start(out=o, in_=i), d_l, writes=bufs)
            ld(QT[:], PF[4 * g * 128:(4 * g + 4) * 128, :].rearrange("(r p) s -> p r s", p=128), [b_q])
            sch._waits("sp", [bPF, bPTn], [])
            ld(KsT[:], PF[(12 + g) * 128:(13 + g) * 128, :], [b_kv])
            ld(KwT[:], PF[(14 + g) * 128:(15 + g) * 128, :], [b_kv])
            ld(Vs[:], PTn[:, g * 128:(g + 1) * 128].rearrange("(c p) d -> p c d", p=128), [b_kv])
            ld(Vw[:], PTn[:, 256 + g * 128:256 + (g + 1) * 128].rearrange("(c p) d -> p c d", p=128), [b_kv])
            ld(Eall[:], cst["Eall"], [b_c])
            ld(BT[:], cst["BT"][:, 0:12, :], [b_c])
            ld(KEEP[:], cst["KEEP"].rearrange("(c p) j -> p c j", p=128), [b_c])
            ld(ADD[:], cst["ADD"].rearrange("(c p) j -> p c j", p=128), [b_c])
            ld(ovl[:], cst["ovl"].rearrange("(c p) j -> p c j", p=128), [b_c])
            ld(identb[:], cst["identb"], [b_c])
            sch.op("dve", lambda e: e.memset(ones[:], 1.0), writes=[b_c])
            sch.op("dve", lambda e: e.memset(zer[:], 0.0), writes=[b_c])
            sch.op("dve", lambda e: e.memset(kcT[:], 0.0), writes=[b_kc])
            sch.op("dve", lambda e: e.memset(vc[:], 0.0), writes=[b_kc])
            cch = [(0, min(128, NCMP))] + ([(1, NCMP - 128)] if NCMP > 128 else [])
            with ExitStack() as es2:
                def sb2(name, shape, dt):
                    return es2.enter_context(nc.sbuf_tensor(tg + "c_" + name, shape, dt))
                src = sb2("src", [128, S], BF16)
                W1 = sb2("W1", [128, 32, 256], BF16)
                W2 = sb2("W2", [128, 2, 128], BF16)
                pT_ = sb2("posT", [128, 32], BF16)
                cvec = sb2("cvec", [128, 2], F32)
                xh = sb2("xh", [128, 256], F32)
                x2 = sb2("x2", [128, 256], F32)
                th = sb2("th", [128, 256], F32)
                gl = sb2("gl", [128, 2, 256], BF16)
                b_src, b_W, b_cv, b_xh, b_x2, b_th, b_gl = [Buf() for _ in range(7)]
                d_c2 = sch.dsem()
                for which in range(2):
                    pfc = (8 if which == 0 else 10) + g
                    sch.dma("sp", lambda e, pfc=pfc: e.dma_start(out=src[:], in_=PF[pfc * 128:(pfc + 1) * 128, :]), d_c2,
                            reads=[bPF], writes=[b_src])
                    sch.dma("pool", lambda e, which=which: e.dma_start(
                        out=W1[:], in_=cmp_w1[which].rearrange("(l d) h -> d l h", d=128)), d_c2, writes=[b_W])
                    sch.dma("pool", lambda e, which=which: e.dma_start(
                        out=W2[:], in_=cmp_w2[which].rearrange("(c p) d -> p c d", p=128)), d_c2, writes=[b_W])
                    sch.dma("pool", lambda e, which=which: e.dma_start(out=pT_[:], in_=posT[which]), d_c2, writes=[b_W])
                    for hc in range(2):
                        for l in range(32):
                            sch.op("pe", mm(ps[7][:, 0:1], W1[:, l, hc * 128:(hc + 1) * 128], pT_[:, l:l + 1], l == 0, l == 31),
                                   reads=[b_W], writes=[psb[7]])
                        sch.op("act", lambda e, hc=hc: e.activation(out=cvec[:, hc:hc + 1], in_=ps[7][:, 0:1], func=AF.Copy),
                               reads=[psb[7]], writes=[b_cv])
                        for l in range(32):
                            sch.op("pe", mm(ps[6][:, 0:NCMP], W1[:, l, hc * 128:(hc + 1) * 128],
                                            src[:, l:l + 16 * (NCMP - 1) + 1:16], l == 0, l == 31),
                                   reads=[b_W, b_src], writes=[psb[6]])
                        sch.op("act", lambda e, hc=hc: e.activation(out=xh[:, 0:NCMP], in_=ps[6][:, 0:NCMP], func=AF.Identity,
                                                                   bias=cvec[:, hc:hc + 1]),
                               reads=[psb[6], b_cv], writes=[b_xh])
                        sch.op("dve", lambda e: e.tensor_tensor(out=x2[:, 0:NCMP], in0=xh[:, 0:NCMP], in1=xh[:, 0:NCMP], op=ALU.mult),
                               reads=[b_xh], writes=[b_x2])
                        sch.op("dve", lambda e: e.tensor_scalar(out=x2[:, 0:NCMP], in0=x2[:, 0:NCMP], scalar1=0.044715, scalar2=1.0,
                                                                op0=ALU.mult, op1=ALU.add), reads=[b_x2], writes=[b_x2])
                        sch.op("dve", lambda e: e.tensor_tensor(out=x2[:, 0:NCMP], in0=x2[:, 0:NCMP], in1=xh[:, 0:NCMP], op=ALU.mult),
                               reads=[b_x2, b_xh], writes=[b_x2])
                        sch.op("act", lambda e: e.activation(out=th[:, 0:NCMP], in_=x2[:, 0:NCMP], func=AF.Tanh, scale=0.7978845608028654),
                               reads=[b_x2], writes=[b_th])
                        sch.op("dve", lambda e: e.tensor_scalar(out=th[:, 0:NCMP], in0=th[:, 0:NCMP], scalar1=1.0, scalar2=0.5,
                                                                op0=ALU.add, op1=ALU.mult), reads=[b_th], writes=[b_th])
                        sch.op("dve", lambda e, hc=hc: e.tensor_tensor(out=gl[:, hc, 0:NCMP], in0=th[:, 0:NCMP], in1=xh[:, 0:NCMP], op=ALU.mult),
                               reads=[b_th, b_xh], writes=[b_gl])
                    if which == 0:
                        for hc in range(2):
                            sch.op("pe", mm(ps[7][:, 0:NCMP], W2[:, hc, :], gl[:, hc, 0:NCMP], hc == 0, hc == 1),
                                   reads=[b_W, b_gl], writes=[psb[7]])
                        sch.op("act", lambda e: e.activation(out=kcT[:, 0:NCMP], in_=ps[7][:, 0:NCMP], func=AF.Copy),
                               reads=[psb[7]], writes=[b_kc])
                    else:
                        for cc, kn in cch:
                            for hc in range(2):
                                sch.op("pe", mm(ps[7][0:kn, 0:128], gl[:, hc, cc * 128:cc * 128 + kn], W2[:, hc, :], hc == 0, hc == 1),
                                       reads=[b_W, b_gl], writes=[psb[7]])
                            sch.op("act", lambda e, cc=cc, kn=kn: e.activation(out=vc[0:kn, cc, :], in_=ps[7][0:kn, 0:128], func=AF.Copy),
                                   reads=[psb[7]], writes=[b_kc])
                if debug:
                    dkc = dscr("dbg_kc%d" % g, [128, 256], BF16)
                    dvc = dscr("dbg_vc%d" % g, [128, 2, 128], BF16)
                    sch.dma("sp", lambda e: e.dma_start(out=dkc, in_=kcT[:]), d_c2, reads=[b_kc], writes=[Buf()])
                    sch.dma("sp", lambda e: e.dma_start(out=dvc, in_=vc[:]), d_c2, reads=[b_kc], writes=[Buf()])
                sch.barrier()
                sch.emit()
            with ExitStack() as es3:
                def sb3(name, shape, dt):
                    return es3.enter_context(nc.sbuf_tensor(tg + "a_" + name, shape, dt))
                a = make_actx(sb3, "")
                Grep = sb3("Grep", [128, 12, QN], F32)
                CMBt = sb3("CMBt", [128, 2, QN], BF16)
                pc = [sb3("pc%d" % i, [128, QN], BF16) for i in range(2)]
                pn = [sb3("pn%d" % i, [128, QN], BF16) for i in range(2)]
                oacc = [sb3("oacc%d" % i, [128, QN], F32) for i in range(4)]
                ost = [sb3("ost%d" % i, [128, QN], BF16) for i in range(2)]
                impm = sb3("impm", [128, NSLC], F32)
                impt = sb3("impt", [128, NSLC], F32)
                v8 = sb3("v8", [128, 16], F32)
                Mb = sb3("Mb", [128, NSLC], BF16)
                b_G, b_CMB = Buf(), Buf()
                b_pc, b_pn = [Buf(), Buf()], [Buf(), Buf()]
                b_oacc = [Buf() for _ in range(4)]
                b_ost = [Buf(), Buf()]
                b_impm, b_impt, b_v8, b_Mb = Buf(), Buf(), Buf(), Buf()
                d_G, d_CMB = sch.dsem(), sch.dsem()
                d_ost = [sch.dsem(), sch.dsem()]
                n_ost = 0
                for qt in range(NQT):
                    T0 = qt * QN
                    N = QN
                    sch.dma("sp", lambda e, T0=T0: e.dma_start(out=Grep[:], in_=G[g * 12:(g + 1) * 12, T0:T0 + QN].partition_broadcast(128)),
                            d_G, reads=[bG], writes=[b_G])
                    sch.dma("sp", lambda e, T0=T0: e.dma_start(out=CMBt[:], in_=cst["CMB"][:, :, T0:T0 + QN]), d_CMB, writes=[b_CMB])
                    ntc = N // 128
                    sch.op("pe", mm(ps[6][:, 0:ntc * NSLC], zer[:, 0:128], zer[:, 0:ntc * NSLC], True, False), reads=[b_c], writes=[psb[6]])
                    for r in range(4):
                        qT = QT[:, r, T0:T0 + N]
                        for cc, kn in cch:
                            si = a.k % 2
                            a.k += 1
                            sch.op("pe", mm(ps[si][0:kn, 0:N], kcT[:, cc * 128:cc * 128 + kn], qT, True, False),
                                   reads=[b_q, b_kc], writes=[psb[si]])
                            sch.op("pe", mm(ps[si][0:kn, 0:N], identb[0:kn, 0:kn], CMBt[0:kn, cc, :], False, True),
                                   reads=[b_c, b_CMB], writes=[psb[si]])
                            sch.op("act", lambda e, cc=cc, kn=kn, si=si: e.activation(out=pc[cc][0:kn, 0:N], in_=ps[si][0:kn, 0:N],
                                                                                   func=AF.Exp, scale=SCALE),
                                   reads=[psb[si]], writes=[b_pc[cc]])
                        ni = 2 + a.n % 2
                        di = 4 + a.n % 2
                        a.n += 1
                        for ci, (cc, kn) in enumerate(cch):
                            sch.op("pe", mm(ps[di][:, 0:N], ones[0:kn, :], pc[cc][0:kn, 0:N], ci == 0, ci == len(cch) - 1),
                                   reads=[b_c, b_pc[cc]], writes=[psb[di]])
                        ri = a.e % 2
                        a.e += 1
                        rd, brd = a.rd[ri], a.b_rd[ri]
                        sch.op("dve", lambda e, rd=rd, di=di: e.tensor_scalar_max(out=rd[:, 0:N], in0=ps[di][:, 0:N], scalar1=1e-30),
                               reads=[psb[di]], writes=[brd])
                        sch.op("dve", lambda e, rd=rd: e.reciprocal(out=rd[:, 0:N], in_=rd[:, 0:N]), reads=[brd], writes=[brd])
                        for cc, kn in cch:
                            sch.op("pool", lambda e, cc=cc, kn=kn, rd=rd: e.tensor_tensor(out=pn[cc][0:kn, 0:N], in0=pc[cc][0:kn, 0:N],
                                                                                       in1=rd[0:kn, 0:N], op=ALU.mult),
                                   reads=[b_pc[cc], brd], writes=[b_pn[cc]])
                        for ci, (cc, kn) in enumerate(cch):
                            sch.op("pe", mm(ps[ni][:, 0:N], vc[0:kn, cc, :], pn[cc][0:kn, 0:N], ci == 0, ci == len(cch) - 1),
                                   reads=[b_kc, b_pn[cc]], writes=[psb[ni]])
                        sch.op("dve", lambda e, r=r, ni=ni: e.tensor_tensor(out=oacc[r][:, 0:N], in0=ps[ni][:, 0:N], in1=Grep[:, r * 3, :],
                                                                          op=ALU.mult),
                               reads=[psb[ni], b_G], writes=[b_oacc[r]])
                        for tc in range(ntc):
                            for cc, kn in cch:
                                sch.op("pe", mm(ps[6][:, tc * NSLC:(tc + 1) * NSLC], pn[cc][0:kn, tc * 128:(tc + 1) * 128],
                                                ovl[0:kn, cc, :], False, False),
                                       reads=[b_pn[cc], b_c], writes=[psb[6]])
                    for tc in range(ntc):
                        chn = T0 // 128 + tc
                        sch.op("dve", lambda e, tc=tc, chn=chn: e.tensor_tensor(out=impm[:], in0=ps[6][:, tc * NSLC:(tc + 1) * NSLC],
                                                                               in1=KEEP[:, chn, :], op=ALU.mult),
                               reads=[psb[6], b_c], writes=[b_impm])
                        sch.op("dve", lambda e, chn=chn: e.tensor_tensor(out=impm[:], in0=impm[:], in1=ADD[:, chn, :], op=ALU.add),
                               reads=[b_impm, b_c], writes=[b_impm])
                        sch.op("dve", lambda e: e.max(out=v8[:, 0:8], in_=impm[:]), reads=[b_impm], writes=[b_v8])
                        sch.op("dve", lambda e: e.match_replace(out=impt[:], in_to_replace=v8[:, 0:8], in_values=impm[:], imm_value=-3.0e38),
                               reads=[b_impm, b_v8], writes=[b_impt])
                        sch.op("dve", lambda e: e.max(out=v8[:, 8:16], in_=impt[:]), reads=[b_impt], writes=[b_v8])
                        sch.op("dve", lambda e: e.tensor_scalar(out=Mb[:], in0=impm[:], scalar1=v8[:, 15:16], scalar2=NEGB,
                                                                op0=ALU.is_lt, op1=ALU.mult),
                               reads=[b_impm, b_v8], writes=[b_Mb])
                        pv7 = ps[7].bitcast(BF16)
                        sch.op("pe", lambda e, pv7=pv7: e.transpose(pv7[0:NSLC, 0:128], Mb[:, 0:NSLC], identb[:]),
                               reads=[b_Mb, b_c], writes=[psb[7]])
                        sch.op("act", lambda e, pv7=pv7, tc=tc, T0=T0: e.activation(out=MbT[:, T0 + tc * 128:T0 + (tc + 1) * 128],
                                                                           in_=pv7[0:NSLC, 0:128], func=AF.Copy),
                               reads=[psb[7]], writes=[b_MbT])
                    for r in range(4):
                        qT = QT[:, r, T0:T0 + N]
                        chunks = []
                        for sc in range((T0 + N) // 128):
                            extra = [(Eall[:, sc * 128:(sc + 1) * 128], MbT[:, T0:T0 + N], [b_c, b_MbT])]
                            if sc * 128 + 127 > T0:
                                dk = (sc * 128 - T0) // 128
                                extra.append((identb[:], BT[:, dk, 0:N], [b_c]))
                            chunks.append(dict(kT=KsT[:, sc * 128:(sc + 1) * 128], kn=128, v=Vs[:, sc, :], kvbufs=[b_kv], extra=extra))
                        ni, di = attn_tile(a, qT, [b_q], N, chunks, ones, b_c)
                        if "c" not in DBG_BR:
                            sch.op("dve", lambda e, r=r: e.memset(oacc[r][:, 0:N], 0.0), writes=[b_oacc[r]])
                        if "s" in DBG_BR:
                            attn_epilogue(a, ni, di, N, Grep[:, r * 3 + 1, :], [b_G], oacc[r][:, 0:N], b_oacc[r], False)
                        chunks = []
                        for k in range(8):
                            s0 = T0 - 512 + 128 * k
                            if s0 < 0 or s0 >= S or s0 >= T0 + N:
                                continue
                            sc = s0 // 128
                            chunks.append(dict(kT=KwT[:, sc * 128:(sc + 1) * 128], kn=128, v=Vw[:, sc, :], kvbufs=[b_kv],
                                               extra=[(identb[:], BT[:, 4 + k, 0:N], [b_c])]))
                        ni, di = attn_tile(a, qT, [b_q], N, chunks, ones, b_c)
                        if "w" in DBG_BR:
                            attn_epilogue(a, ni, di, N, Grep[:, r * 3 + 2, :], [b_G], oacc[r][:, 0:N], b_oacc[r], False)
                        oi = n_ost % 2
                        n_ost += 1
                        sch.op("act", lambda e, oi=oi, r=r: e.activation(out=ost[oi][:, 0:N], in_=oacc[r][:, 0:N], func=AF.Copy),
                               reads=[b_oacc[r]], writes=[b_ost[oi]])
                        hd = 4 * g + r
                        sch.dma("sp", lambda e, oi=oi, hd=hd, T0=T0: e.dma_start(out=ON[hd * 128:(hd + 1) * 128, T0:T0 + N], in_=ost[oi][:, 0:N]),
                                d_ost[oi], reads=[b_ost[oi]], writes=[bON])
                if debug:
                    dmb = dscr("dbg_mbt%d" % g, [NSLC, S], BF16)
                    sch.dma("sp", lambda e: e.dma_start(out=dmb, in_=MbT[:]), d_G, reads=[b_MbT], writes=[Buf()])
                sch.barrier()
                sch.emit()

    if upto >= 2:
        stage_nsa(0)
        stage_nsa(1)

    def stage_dil(hd):
        tg = "d%d_" % hd
        with ExitStack() as es:
            def sb(name, shape, dt):
                return es.enter_context(nc.sbuf_tensor(tg + name, shape, dt))
            QT = sb("QT", [128, 3, S], BF16)
            KT = sb("KT", [128, 3, S], BF16)
            kns = [min(128, S // dl) for dl in DILS]
            V = [sb("V%d" % gi, [kns[gi], S // kns[gi], 128], BF16) for gi in range(3)]
            BT = sb("BT", [128, 5, 512], BF16)
            ones = sb("ones", [128, 128], BF16)
            anum = sb("anum", [128, S], F32)
            aden = sb("aden", [128, S], F32)
            ost = [sb("ost%d" % i, [128, 512], BF16) for i in range(2)]
            rdf = [sb("rdf%d" % i, [128, 512], F32) for i in range(2)]
            a = make_actx(sb, "")
            b_q, b_kv, b_c, b_acc = Buf(), Buf(), Buf(), Buf()
            b_ost, b_rdf = [Buf(), Buf()], [Buf(), Buf()]
            d_l = sch.dsem()
            d_ost = [sch.dsem(), sch.dsem()]
            ld = lambda o, i, bufs: sch.dma("sp", lambda e: e.dma_start(out=o, in_=i), d_l, writes=bufs)
            for gi in range(3):
                cq = 16 + gi * 4 + hd
                ck = 28 + gi * 4 + hd
                ld(QT[:, gi, :], PF[cq * 128:(cq + 1) * 128, :], [b_q])
                ld(KT[:, gi, :], PF[ck * 128:(ck + 1) * 128, :], [b_kv])
                ld(V[gi][:], PTd[gi][:, hd * 128:(hd + 1) * 128].rearrange("(c p) d -> p c d", p=kns[gi]), [b_kv])
            ld(BT[:], cst["BT"][:, 12:17, :], [b_c])
            sch.op("dve", lambda e: e.memset(ones[:], 1.0), writes=[b_c])
            for gi in range(3):
                dl = DILS[gi]
                L = S // dl
                kn = kns[gi]
                N_ = min(512, L)
                for rho in range(dl):
                    for qi in range(L // N_):
                        I0 = qi * N_
                        qT = QT[:, gi, rho * L + I0:rho * L + I0 + N_]
                        chunks = []
                        s0 = max(0, I0 - 128)
                        while s0 < I0 + N_:
                            bi = (s0 - I0 + 128) // 128
                            chunks.append(dict(kT=KT[:, gi, rho * L + s0:rho * L + s0 + kn], kn=kn,
                                               v=V[gi][:, (rho * L + s0) // kn, :], kvbufs=[b_kv],
                                               extra=[(ones[0:kn, 0:kn] if False else identb_g[0:kn, 0:kn], BT[0:kn, bi, 0:N_], [b_c, b_idg])]))
                            s0 += kn
                        ni, di = attn_tile(a, qT, [b_q], N_, chunks, ones, b_c)
                        c0 = rho + dl * I0
                        c1 = rho + dl * (I0 + N_ - 1) + 1
                        if gi == 0:
                            sch.op("act", lambda e, ni=ni, c0=c0, c1=c1, dl=dl, N_=N_: e.activation(
                                out=anum[:, c0:c1:dl], in_=ps[ni][:, 0:N_], func=AF.Copy), reads=[psb[ni]], writes=[b_acc])
                            sch.op("dve", lambda e, di=di, c0=c0, c1=c1, dl=dl, N_=N_: e.tensor_copy(
                                out=aden[:, c0:c1:dl], in_=ps[di][:, 0:N_]), reads=[psb[di]], writes=[b_acc])
                        else:
                            sch.op("dve", lambda e, ni=ni, c0=c0, c1=c1, dl=dl, N_=N_: e.tensor_tensor(
                                out=anum[:, c0:c1:dl], in0=anum[:, c0:c1:dl], in1=ps[ni][:, 0:N_], op=ALU.add),
                                reads=[psb[ni], b_acc], writes=[b_acc])
                            sch.op("dve", lambda e, di=di, c0=c0, c1=c1, dl=dl, N_=N_: e.tensor_tensor(
                                out=aden[:, c0:c1:dl], in0=aden[:, c0:c1:dl], in1=ps[di][:, 0:N_], op=ALU.add),
                                reads=[psb[di], b_acc], writes=[b_acc])
            for qt in range(S // QN):
                T0 = qt * QN
                oi = qt % 2
                sch.op("dve", lambda e, oi=oi, T0=T0: e.reciprocal(out=rdf[oi][:, 0:QN], in_=aden[:, T0:T0 + QN]),
                       reads=[b_acc], writes=[b_rdf[oi]])
                sch.op("dve", lambda e, oi=oi, T0=T0: e.tensor_tensor(out=ost[oi][:, 0:QN], in0=anum[:, T0:T0 + QN], in1=rdf[oi][:, 0:QN],
                                                                      op=ALU.mult), reads=[b_acc, b_rdf[oi]], writes=[b_ost[oi]])
                sch.dma("sp", lambda e, oi=oi, T0=T0: e.dma_start(out=OD[hd * 128:(hd + 1) * 128, T0:T0 + QN], in_=ost[oi][:, 0:QN]),
                        d_ost[oi], reads=[b_ost[oi]], writes=[bOD])
            sch.barrier()
            sch.emit()

    if upto >= 3:
        identb_g = nc.alloc_sbuf_tensor("identb_g", [128, 128], BF16).ap()
        b_idg = Buf()
        d_idg = sch.dsem()
        sch.dma("sp", lambda e: e.dma_start(out=identb_g, in_=cst["identb"]), d_idg, writes=[b_idg])
        for hd in range(4):
            stage_dil(hd)


    CAP = CAPB * 128
    NROW = NE * CAP
    w_br_nsa = din("w_br_nsa", [1024, D])
    w_br_dil = din("w_br_dil", [512, D])
    w_out = din("w_out", [D, D])
    ln_gb = din("ln_gb", [4, D])
    w_router = din("w_router", [D, NE])
    b_router = din("b_router", [1, NE])
    MT = dscr("MT", [D, S], BF16)
    H1 = dscr("H1", [S, D], F32)
    NQX, NQY = (4, 8) if ep else (1, 1)
    WX, WY = D // NQX, D // NQY
    XG = [dscr("XG%d" % q, [NROW, WX], BF16) for q in range(NQX)]
    RI = dscr("RI", [S, 8], F32)
    ROWI = [dscr("ROWI%d" % k_, [S, 1], I32) for k_ in range(4)]
    bMT, bH1, bXG, bRI = Buf(), Buf(), Buf(), Buf()

    def stage_merge():
        with ExitStack() as es:
            def sb(name, shape, dt):
                return es.enter_context(nc.sbuf_tensor("m_" + name, shape, dt))
            Wa = sb("Wa", [128, 8, D], BF16)
            Wb = sb("Wb", [128, 4, D], BF16)
            ONt = [sb("ONt%d" % i, [128, 8, QN], BF16) for i in range(2)]
            ODt = [sb("ODt%d" % i, [128, 4, QN], BF16) for i in range(2)]
            ga = sb("ga", [128, 16, QN], BF16)
            gb = sb("gb", [128, 16, QN], BF16)
            mst = [sb("mst%d" % i, [128, 16, QN], BF16) for i in range(2)]
            m1 = [sb("m1%d" % i, [128, QN], F32) for i in range(2)]
            m2 = [sb("m2%d" % i, [128, QN], F32) for i in range(2)]
            b_W, b_g = Buf(), Buf()
            b_in_, b_mst, b_m1, b_m2 = [Buf(), Buf()], [Buf(), Buf()], [Buf(), Buf()], [Buf(), Buf()]
            d_W, d_g = sch.dsem(), sch.dsem()
            d_in_, d_mst = [sch.dsem(), sch.dsem()], [sch.dsem(), sch.dsem()]
            sch.dma("pool", lambda e: e.dma_start(out=Wa[:], in_=w_br_nsa.rearrange("(f p) n -> p f n", p=128)), d_W, writes=[b_W])
            sch.dma("pool", lambda e: e.dma_start(out=Wb[:], in_=w_br_dil.rearrange("(f p) n -> p f n", p=128)), d_W, writes=[b_W])
            k = 0
            for qt in range(NQT):
                T0 = qt * QN
                bi = qt % 2
                sch.dma("sp", lambda e, bi=bi, T0=T0: e.dma_start(out=ONt[bi][:], in_=ON[:, T0:T0 + QN].rearrange("(f p) t -> p f t", p=128)),
                        d_in_[bi], writes=[b_in_[bi]])
                sch.dma("sp", lambda e, bi=bi, T0=T0: e.dma_start(out=ODt[bi][:], in_=OD[:, T0:T0 + QN].rearrange("(f p) t -> p f t", p=128)),
                        d_in_[bi], writes=[b_in_[bi]])
                sch.dma("sp", lambda e, T0=T0: e.dma_start(out=ga[:], in_=PF[40 * 128:56 * 128, T0:T0 + QN].rearrange("(c p) t -> p c t", p=128)),
                        d_g, writes=[b_g])
                sch.dma("sp", lambda e, T0=T0: e.dma_start(out=gb[:], in_=PF[56 * 128:72 * 128, T0:T0 + QN].rearrange("(c p) t -> p c t", p=128)),
                        d_g, writes=[b_g])
                for n in range(16):
                    pa = (2 * k) % 8
                    pb = (2 * k + 1) % 8
                    ti = k % 2
                    k += 1
                    for f in range(8):
                        sch.op("pe", mm(ps[pa][:, 0:QN], Wa[:, f, n * 128:(n + 1) * 128], ONt[bi][:, f, :], f == 0, f == 7),
                               reads=[b_W, b_in_[bi]], writes=[psb[pa]])
                    for f in range(4):
                        sch.op("pe", mm(ps[pb][:, 0:QN], Wb[:, f, n * 128:(n + 1) * 128], ODt[bi][:, f, :], f == 0, f == 3),
                               reads=[b_W, b_in_[bi]], writes=[psb[pb]])
                    sch.op("dve", lambda e, pa=pa, ti=ti, n=n: e.tensor_tensor(out=m1[ti][:], in0=ps[pa][:, 0:QN], in1=ga[:, n, :], op=ALU.mult),
                           reads=[psb[pa], b_g], writes=[b_m1[ti]])
                    sch.op("dve", lambda e, pb=pb, ti=ti, n=n: e.tensor_tensor(out=m2[ti][:], in0=ps[pb][:, 0:QN], in1=gb[:, n, :], op=ALU.mult),
                           reads=[psb[pb], b_g], writes=[b_m2[ti]])
                    sch.op("pool", lambda e, ti=ti, n=n, bi=bi: e.tensor_tensor(out=mst[bi][:, n, :], in0=m1[ti][:], in1=m2[ti][:], op=ALU.add),
                           reads=[b_m1[ti], b_m2[ti]], writes=[b_mst[bi]])
                sch.dma("sp", lambda e, bi=bi, T0=T0: e.dma_start(out=MT[:, T0:T0 + QN].rearrange("(c p) t -> p c t", p=128), in_=mst[bi][:]),
                        d_mst[bi], reads=[b_mst[bi]], writes=[bMT])
            sch.barrier()
            sch.emit()

    def layer_norm_tile(sbt, hp, b_hp, outt, b_out, gbc, bbc, b_gb, sq, b_sq, st, b_st):
        sch.op("dve", lambda e: e.tensor_reduce(out=st[:, 0:1], in_=hp[:], axis=AX.X, op=ALU.add), reads=[b_hp], writes=[b_st])
        sch.op("dve", lambda e: e.tensor_scalar(out=st[:, 1:2], in0=st[:, 0:1], scalar1=-1.0 / D, scalar2=None, op0=ALU.mult),
               reads=[b_st], writes=[b_st])
        sch.op("act", lambda e: e.activation(out=hp[:], in_=hp[:], func=AF.Identity, bias=st[:, 1:2]), reads=[b_hp, b_st], writes=[b_hp])
        sch.op("act", lambda e: e.activation(out=sq[:], in_=hp[:], func=AF.Square, accum_out=st[:, 2:3]), reads=[b_hp], writes=[b_sq, b_st], same=False)
        sch.op("dve", lambda e: e.tensor_scalar(out=st[:, 3:4], in0=st[:, 2:3], scalar1=1.0 / D, scalar2=LN_EPS, op0=ALU.mult, op1=ALU.add),
               reads=[b_st], writes=[b_st])
        sch.op("act", lambda e: e.activation(out=st[:, 4:5], in_=st[:, 3:4], func=AF.Sqrt), reads=[b_st], writes=[b_st])
        sch.op("dve", lambda e: e.reciprocal(out=st[:, 5:6], in_=st[:, 4:5]), reads=[b_st], writes=[b_st])
        sch.op("dve", lambda e: e.scalar_tensor_tensor(out=outt[:], in0=hp[:], scalar=st[:, 5:6], in1=gbc[:], op0=ALU.mult, op1=ALU.mult),
               reads=[b_hp, b_st, b_gb], writes=[b_out])
        sch.op("pool", lambda e: e.tensor_tensor(out=outt[:], in0=outt[:], in1=bbc[:], op=ALU.add), reads=[b_out, b_gb], writes=[b_out])

    ebase_y_in = din("ebase_y", [128, NE]) if ep else None

    def stage_out():
        with ExitStack() as es:
            def sb(name, shape, dt):
                return es.enter_context(nc.sbuf_tensor("o_" + name, shape, dt))
            Wo = sb("Wo", [128, KC, D], BF16)
            mTt = sb("mTt", [128, KC, QN], BF16)
            xc = [sb("xc%d" % i, [128, D], F32) for i in range(2)]
            hp = sb("hp", [128, D], F32)
            sq = sb("sq", [128, D], BF16)
            h1 = [sb("h1%d" % i, [128, D], F32) for i in range(2)]
            h1b = [sb("h1b%d" % i, [128, D], BF16) for i in range(2)]
            h1T = sb("h1T", [128, KC, 128], F32)
            gbc = sb("gbc", [128, D], F32)
            bbc = sb("bbc", [128, D], F32)
            Wr = sb("Wr", [128, KC, NE], F32)
            brb = sb("brb", [128, NE], F32)
            identf = sb("identf", [128, 128], F32)
            ones = sb("ones", [128, 128], BF16)
            ustr = sb("ustr", [128, 128], BF16)
            ebase = sb("ebase", [128, NE], F32)
            base = sb("base", [128, NE], F32)
            zt = sb("zt", [128, 4096], BF16)
            st = sb("st", [128, 8], F32)
            rt = {n_: sb(n_, [128, NE], F32) for n_ in ("lg", "m4", "ex", "gd", "slot", "okm", "rowf", "oh", "t1", "t2")}
            mb16 = sb("mb16", [128, NE], BF16)
            oh4 = sb("oh4", [128, 4, NE], F32)
            t14 = sb("t14", [128, 4, NE], F32)
            v8 = sb("v8", [128, 8], F32)
            sm = sb("sm", [128, 8], F32)
            ri = [sb("ri%d" % i, [128, 8], F32) for i in range(2)]
            idxf = sb("idxf", [128, 4], F32)
            idxi = [[sb("idxi%d_%d" % (i, k_), [128, 1], I32) for k_ in range(4)] for i in range(2)]
            rowi = [sb("rowi%d" % i, [128, 4], I32) for i in range(2)]
            b_W, b_mT, b_c, b_hp, b_sq, b_st, b_h1T, b_r, b_base = [Buf() for _ in range(9)]
            b_xc, b_h1, b_h1b, b_ri, b_idx, b_rowi = [[Buf(), Buf()] for _ in range(6)]
            d_W, d_mT, d_c = sch.dsem(), sch.dsem(), sch.dsem()
            d_xc, d_h1, d_sc, d_ri = [[sch.dsem(), sch.dsem()] for _ in range(4)]
            d_z = sch.dsem()
            sch.dma("pool", lambda e: e.dma_start(out=Wo[:], in_=w_out.rearrange("(k p) n -> p k n", p=128)), d_W, writes=[b_W])
            cl = lambda o, i: sch.dma("sp", lambda e: e.dma_start(out=o, in_=i), d_c, writes=[b_c])
            cl(gbc[:], ln_gb[0:1, :].partition_broadcast(128))
            cl(bbc[:], ln_gb[1:2, :].partition_broadcast(128))
            cl(Wr[:], w_router.rearrange("(k p) n -> p k n", p=128))
            cl(brb[:], b_router.partition_broadcast(128))
            cl(identf[:], cst["identf"])
            cl(ustr[:], cst["ustr"])
            cl(ebase[:], cst["ebase"])
            if ep:
                ebasey = sb("ebasey", [128, NE], F32)
                rowfy = sb("rowfy", [128, NE], F32)
                riy = sb("riy", [128, 4], F32)
                cl(ebasey[:], ebase_y_in)
            sch.op("dve", lambda e: e.memset(ones[:], 1.0), writes=[b_c])
            sch.op("dve", lambda e: e.memset(base[:], 0.0), writes=[b_base])
            sch.op("dve", lambda e: e.memset(zt[:], 0.0), writes=[b_c])
            nbz = 4096 // WX
            for q in range(NQX):
                for bz in range(NROW // (128 * nbz)):
                    dst = XG[q][bz * 128 * nbz:(bz + 1) * 128 * nbz, :].rearrange("(b p) f -> p b f", p=128)
                    sch.dma("sp", lambda e, dst=dst: e.dma_start(out=dst, in_=zt[:].rearrange("p (b f) -> p b f", b=nbz)),
                            d_z, reads=[b_c], writes=[bXG])
            npp = 0
            bc_reg = {}
            sch.prog["pool"].append(lambda e: bc_reg.__setitem__("r", e.to_reg(NROW - 1)))
            for c in range(NT):
                tok0 = c * 128
                cb = c % 2
                if c % (QN // 128) == 0:
                    sch.dma("sp", lambda e, tok0=tok0: e.dma_start(out=mTt[:], in_=MT[:, tok0:tok0 + QN].rearrange("(k p) t -> p k t", p=128)),
                            d_mT, reads=[bMT], writes=[b_mT])
                cl_ = c % (QN // 128)
                sch.dma("sp", lambda e, cb=cb, tok0=tok0: e.dma_start(out=xc[cb][:], in_=x[tok0:tok0 + 128, :]), d_xc[cb], writes=[b_xc[cb]])
                for nt in range(4):
                    pi = npp % 4
                    npp += 1
                    for k_ in range(KC):
                        sch.op("pe", mm(ps[pi][:, :], mTt[:, k_, cl_ * 128:(cl_ + 1) * 128], Wo[:, k_, nt * 512:(nt + 1) * 512], k_ == 0, k_ == KC - 1),
                               reads=[b_W, b_mT], writes=[psb[pi]])
                    sch.op("dve", lambda e, pi=pi, cb=cb, nt=nt: e.scalar_tensor_tensor(
                        out=hp[:, nt * 512:(nt + 1) * 512], in0=xc[cb][:, nt * 512:(nt + 1) * 512], scalar=DN_ALPHA, in1=ps[pi][:, :],
                        op0=ALU.mult, op1=ALU.add), reads=[psb[pi], b_xc[cb]], writes=[b_hp], same=False)
                layer_norm_tile(sb, hp, b_hp, h1[cb], b_h1[cb], gbc, bbc, b_c, sq, b_sq, st, b_st)
                sch.dma("sp", lambda e, cb=cb, tok0=tok0: e.dma_start(out=H1[tok0:tok0 + 128, :], in_=h1[cb][:]), d_h1[cb],
                        reads=[b_h1[cb]], writes=[bH1])
                sch.op("act", lambda e, cb=cb: e.activation(out=h1b[cb][:], in_=h1[cb][:], func=AF.Copy), reads=[b_h1[cb]], writes=[b_h1b[cb]])
                for q4 in range(4):
                    pi = 4 + q4 % 2
                    for j in range(4):
                        k_ = q4 * 4 + j
                        sch.op("pe", lambda e, pi=pi, j=j, k_=k_, cb=cb: e.transpose(ps[pi][:, j * 128:(j + 1) * 128],
                                                                                  h1[cb][:, k_ * 128:(k_ + 1) * 128], identf[:]),
                               reads=[b_h1[cb], b_c], writes=[psb[pi]])
                    sch.op("act", lambda e, pi=pi, q4=q4: e.activation(out=h1T[:, q4 * 4:(q4 + 1) * 4, :],
                                                                     in_=ps[pi][:, :].rearrange("p (a b) -> p a b", a=4), func=AF.Copy),
                           reads=[psb[pi]], writes=[b_h1T])
                for k_ in range(KC):
                    sch.op("pe", mm(ps[6][:, 0:NE], h1T[:, k_, :], Wr[:, k_, :], k_ == 0, k_ == KC - 1), reads=[b_h1T, b_c], writes=[psb[6]])
                R_ = rt
                dv = lambda fn, rd_=(), wr_=(): sch.op("dve", fn, reads=[b_r, b_c] + list(rd_), writes=[b_r] + list(wr_))
                dv(lambda e: e.tensor_tensor(out=R_["lg"][:], in0=ps[6][:, 0:NE], in1=brb[:], op=ALU.add), rd_=[psb[6]])
                dv(lambda e: e.max(out=v8[:], in_=R_["lg"][:]))
                dv(lambda e: e.tensor_scalar(out=R_["m4"][:], in0=R_["lg"][:], scalar1=v8[:, 3:4], scalar2=None, op0=ALU.is_ge))
                dv(lambda e: e.tensor_scalar(out=sm[:, 0:1], in0=v8[:, 0:1], scalar1=-1.0, scalar2=None, op0=ALU.mult))
                sch.op("act", lambda e: e.activation(out=R_["ex"][:], in_=R_["lg"][:], func=AF.Exp, bias=sm[:, 0:1]), reads=[b_r], writes=[b_r])
                dv(lambda e: e.tensor_tensor(out=R_["ex"][:], in0=R_["ex"][:], in1=R_["m4"][:], op=ALU.mult))
                dv(lambda e: e.tensor_reduce(out=sm[:, 1:2], in_=R_["ex"][:], axis=AX.X, op=ALU.add))
                dv(lambda e: e.reciprocal(out=sm[:, 2:3], in_=sm[:, 1:2]))
                dv(lambda e: e.tensor_scalar(out=R_["gd"][:], in0=R_["ex"][:], scalar1=sm[:, 2:3], scalar2=None, op0=ALU.mult))
                dv(lambda e: e.tensor_copy(out=mb16[:], in_=R_["m4"][:]))
                sch.op("pe", mm(ps[7][:, 0:NE], ustr[:], mb16[:], True, True), reads=[b_r, b_c], writes=[psb[7]])
                sch.op("pe", mm(ps[7][:, NE:2 * NE], ones[:], mb16[:], True, True), reads=[b_r, b_c], writes=[psb[7]])
                dv(lambda e: e.tensor_tensor(out=R_["slot"][:], in0=ps[7][:, 0:NE], in1=base[:], op=ALU.add), rd_=[psb[7], b_base])
                dv(lambda e: e.tensor_tensor(out=base[:], in0=ps[7][:, NE:2 * NE], in1=base[:], op=ALU.add), rd_=[psb[7], b_base], wr_=[b_base])
                dv(lambda e: e.tensor_scalar(out=R_["okm"][:], in0=R_["slot"][:], scalar1=float(CAP), scalar2=None, op0=ALU.is_lt))
                dv(lambda e: e.tensor_tensor(out=R_["okm"][:], in0=R_["okm"][:], in1=R_["m4"][:], op=ALU.mult))
                dv(lambda e: e.tensor_tensor(out=R_["rowf"][:], in0=R_["slot"][:], in1=ebase[:], op=ALU.add))
                if ep:
                    dv(lambda e: e.tensor_tensor(out=rowfy[:], in0=R_["slot"][:], in1=ebasey[:], op=ALU.add))
                bc_k = lambda ap: ap.unsqueeze(1).to_broadcast([128, 4, NE])
                dv(lambda e: e.tensor_tensor(out=oh4[:], in0=bc_k(R_["lg"][:]), in1=v8[:, 0:4].unsqueeze(2).to_broadcast([128, 4, NE]),
                                             op=ALU.is_equal))
                dv(lambda e: e.tensor_tensor(out=oh4[:], in0=oh4[:], in1=bc_k(R_["okm"][:]), op=ALU.mult))
                dv(lambda e: e.tensor_reduce(out=sm[:, 4:8], in_=oh4[:], axis=AX.X, op=ALU.add))
                dv(lambda e: e.tensor_tensor(out=t14[:], in0=oh4[:], in1=bc_k(R_["rowf"][:]), op=ALU.mult))
                dv(lambda e, cb=cb: e.tensor_reduce(out=ri[cb][:, 0:4], in_=t14[:], axis=AX.X, op=ALU.add), rd_=[b_ri[cb]], wr_=[b_ri[cb]])
                if ep:
                    dv(lambda e: e.tensor_tensor(out=t14[:], in0=oh4[:], in1=bc_k(rowfy[:]), op=ALU.mult))
                    dv(lambda e: e.tensor_reduce(out=riy[:, 0:4], in_=t14[:], axis=AX.X, op=ALU.add))
                dv(lambda e: e.tensor_tensor(out=t14[:], in0=oh4[:], in1=bc_k(R_["gd"][:]), op=ALU.mult))
                dv(lambda e, cb=cb: e.tensor_reduce(out=ri[cb][:, 4:8], in_=t14[:], axis=AX.X, op=ALU.add), rd_=[b_ri[cb]], wr_=[b_ri[cb]])
                dv(lambda e: e.tensor_scalar(out=idxf[:], in0=sm[:, 4:8], scalar1=-1.0e6, scalar2=1.0e6, op0=ALU.mult, op1=ALU.add))
                dv(lambda e, cb=cb: e.tensor_tensor(out=idxf[:], in0=idxf[:], in1=ri[cb][:, 0:4], op=ALU.add), rd_=[b_ri[cb]])
                for k_ in range(4):
                    dv(lambda e, k_=k_, cb=cb: e.tensor_copy(out=idxi[cb][k_][:], in_=idxf[:, k_:k_ + 1]), rd_=[b_idx[cb]], wr_=[b_idx[cb]])
                if ep:
                    dv(lambda e, cb=cb: e.tensor_copy(out=rowi[cb][:], in_=riy[:, 0:4]), rd_=[b_ri[cb], b_rowi[cb]], wr_=[b_rowi[cb]])
                else:
                    dv(lambda e, cb=cb: e.tensor_copy(out=rowi[cb][:], in_=ri[cb][:, 0:4]), rd_=[b_ri[cb], b_rowi[cb]], wr_=[b_rowi[cb]])
                for k_ in range(4):
                    for q in range(NQX):
                        sch.dma("pool", lambda e, k_=k_, cb=cb, q=q: e.indirect_dma_start(
                            out=XG[q][:, :], out_offset=bass.IndirectOffsetOnAxis(ap=idxi[cb][k_][:, 0:1], axis=0),
                            in_=h1b[cb][:, q * WX:(q + 1) * WX], in_offset=None, bounds_check=bc_reg["r"], oob_is_err=False),
                            d_sc[cb], reads=[b_h1b[cb], b_idx[cb], bXG], writes=[Buf()])
                sch.dma("sp", lambda e, cb=cb, tok0=tok0: e.dma_start(out=RI[tok0:tok0 + 128, :], in_=ri[cb][:]), d_ri[cb],
                        reads=[b_ri[cb]], writes=[bRI])
                for k_ in range(4):
                    sch.dma("sp", lambda e, cb=cb, tok0=tok0, k_=k_: e.dma_start(out=ROWI[k_][tok0:tok0 + 128, :], in_=rowi[cb][:, k_:k_ + 1]), d_ri[cb],
                            reads=[b_rowi[cb]], writes=[bRI])
            sch.barrier()
            sch.emit()

    if upto >= 4:
        stage_merge()
    if upto >= 5:
        stage_out()


    NEL = NE // 8 if ep else NE
    w_up = din("w_up", [NEL, D, 2 * D])
    w_down = din("w_down", [NEL, D, D])
    b_up_fm = din("b_up_fm", [128, NEL * 32])
    b_down = din("b_down", [NEL, D])
    Y = [dscr("Y%d" % q, [NROW, WY], F32) for q in range(NQY)]
    bY = Buf()
    if ep:
        XGr = [nc.dram_tensor("XGall%d" % q, [8 * NROW, WX], BF16, kind="Internal").ap() for q in range(NQX)]
        Yr = [nc.dram_tensor("Yall%d" % q, [8 * NROW, WY], F32, kind="Internal").ap() for q in range(NQY)]
        bXGr, bYr = Buf(), Buf()
        d_cc = sch.dsem()
        xg_idx = din("xg_idx", [128, NE * CAPB], I32)
    else:
        XGr, Yr, bXGr, bYr = XG, Y, bXG, bY

    def all_gather(srcs, dsts, bsrc, bdst):
        for src, dst in zip(srcs, dsts):
            sch.dma("pool", lambda e, src=src, dst=dst: e.collective_compute(
                "AllGather", ALU.bypass, replica_groups=[list(range(8))], ins=[src[:, :]], outs=[dst[:, :]]),
                d_cc, reads=[bsrc], writes=[bdst])
        sch.barrier()
        sch.emit()

    def stage_experts():
        with ExitStack() as es:
            def sb(name, shape, dt):
                return es.enter_context(nc.sbuf_tensor("e_" + name, shape, dt))
            xg = [sb("xg%d" % i, [128, D], BF16) for i in range(2)]
            xgT = sb("xgT", [128, KC, CAP], BF16)
            actT = sb("actT", [128, KC, CAP], BF16)
            NUPB, NDNB = 4, 2
            Wu = [sb("Wu%d" % i, [128, KC, 2, 256], BF16) for i in range(NUPB)]
            Wd = [sb("Wd%d" % i, [128, KC, 512], BF16) for i in range(NDNB)]
            bup = sb("bup", [128, 2, 32], F32)
            b_bup = [Buf(), Buf()]
            d_bup = [sch.dsem(), sch.dsem()]
            bdn = [sb("bdn%d" % i, [128, D], F32) for i in range(2)]
            identb = sb("identb", [128, 128], BF16)
            HN = CAP // 2
            tg_ = [sb("tg%d" % i, [128, HN], F32) for i in range(2)]
            ts_ = [sb("ts%d" % i, [128, HN], F32) for i in range(2)]
            tl_ = [sb("tl%d" % i, [128, HN], F32) for i in range(2)]
            yst = [sb("yst%d" % i, [128, 512], F32) for i in range(3)]
            b_xg, b_Wu, b_Wd = [Buf(), Buf()], [Buf() for _ in range(NUPB)], [Buf() for _ in range(NDNB)]
            b_xgT, b_actT, b_c = Buf(), Buf(), Buf()
            b_bdn = [Buf(), Buf()]
            b_tg, b_ts, b_tl = [Buf(), Buf()], [Buf(), Buf()], [Buf(), Buf()]
            b_yst = [Buf() for _ in range(3)]
            d_xg = [sch.dsem(), sch.dsem()]
            d_Wu = [sch.dsem() for _ in range(NUPB)]
            d_Wd = [sch.dsem() for _ in range(NDNB)]
            d_bdn = [sch.dsem(), sch.dsem()]
            d_yst = [sch.dsem() for _ in range(3)]
            d_c = sch.dsem()
            sch.dma("sp", lambda e: e.dma_start(out=identb[:], in_=cst["identb"]), d_c, writes=[b_c])
            if ep:
                xgi = sb("xgi", [128, NE * CAPB], I32)
                sch.dma("sp", lambda e: e.dma_start(out=xgi[:], in_=xg_idx), d_c, writes=[b_c])
            upieces, dpieces = [], []
            for ex in range(NE):
                wex_ = ex // 8 if ep else ex
                for pc in range(8):
                    upieces.append((wex_, pc))
                for nt in range(4):
                    dpieces.append((wex_, nt))
            PDU, PDD = NUPB, NDNB

            def issue_u(i):
                if i >= len(upieces):
                    return
                wex_, pc = upieces[i]
                bi = i % NUPB
                for hl in range(2):
                    src = w_up[wex_, :, hl * D + pc * 256:hl * D + (pc + 1) * 256].rearrange("(k p) n -> p k n", p=128)
                    sch.dma("pool", lambda e, bi=bi, hl=hl, src=src: e.dma_start(out=Wu[bi][:, :, hl, :], in_=src), d_Wu[bi], writes=[b_Wu[bi]])

            def issue_d(i):
                if i >= len(dpieces):
                    return
                wex_, nt = dpieces[i]
                bi = i % NDNB
                src = w_down[wex_, :, nt * 512:(nt + 1) * 512].rearrange("(k p) n -> p k n", p=128)
                sch.dma("pool", lambda e, bi=bi, src=src: e.dma_start(out=Wd[bi][:], in_=src), d_Wd[bi], writes=[b_Wd[bi]])
            for i in range(PDU):
                issue_u(i)
            for i in range(PDD):
                issue_d(i)
            uidx = 0
            didx = 0
            nps = 0
            nxg = 0
            nys = 0
            nt_ = 0
            for ex in range(NE):
                eb = ex % 2
                if ep:
                    wex = ex // 8
                    rbase = (ex % 8) * (NE // 8) * CAP + wex * CAP
                else:
                    wex = ex
                    rbase = ex * CAP
                sch.dma("sp", lambda e, eb=eb, wex=wex: e.dma_start(out=bdn[eb][:], in_=b_down[wex:wex + 1, :].partition_broadcast(128)),
                        d_bdn[eb], writes=[b_bdn[eb]])
                sch.dma("sp", lambda e, eb=eb, wex=wex: e.dma_start(out=bup[:, eb, :], in_=b_up_fm[:, wex * 32:(wex + 1) * 32]),
                        d_bup[eb], writes=[b_bup[eb]])
                for blk in range(CAPB):
                    xi = nxg % 2
                    nxg += 1
                    r0 = rbase + blk * 128
                    for q in range(NQX):
                        if ep:
                            jcol = ex * CAPB + blk
                            sch.dma("pool", lambda e, xi=xi, jcol=jcol, q=q: e.indirect_dma_start(
                                out=xg[xi][:, q * WX:(q + 1) * WX], out_offset=None, in_=XGr[q][:, :],
                                in_offset=bass.IndirectOffsetOnAxis(ap=xgi[:, jcol:jcol + 1], axis=0)),
                                d_xg[xi], reads=[bXGr, b_c], writes=[b_xg[xi]])
                        else:
                            sch.dma("sp", lambda e, xi=xi, r0=r0, q=q: e.dma_start(out=xg[xi][:, q * WX:(q + 1) * WX], in_=XGr[q][r0:r0 + 128, :]),
                                    d_xg[xi], reads=[bXGr], writes=[b_xg[xi]])
                    for q4 in range(4):
                        pi = 4 + nps % 2
                        nps += 1
                        pv = ps[pi].bitcast(BF16)
                        for j in range(4):
                            k_ = q4 * 4 + j
                            sch.op("pe", lambda e, pv=pv, j=j, k_=k_, xi=xi: e.transpose(pv[:, j * 128:(j + 1) * 128],
                                                                                      xg[xi][:, k_ * 128:(k_ + 1) * 128], identb[:]),
                                   reads=[b_xg[xi], b_c], writes=[psb[pi]])
                        sch.op("act", lambda e, pv=pv, q4=q4, blk=blk: e.activation(
                            out=xgT[:, q4 * 4:(q4 + 1) * 4, blk * 128:(blk + 1) * 128],
                            in_=pv[:, 0:512].rearrange("p (a b) -> p a b", a=4), func=AF.Copy), reads=[psb[pi]], writes=[b_xgT])
                for pc in range(8):
                    bi = uidx % NUPB
                    for jj in range(2):
                        j = pc * 2 + jj
                        for hf in range(2):
                            pg = (2 * nt_) % 4
                            pl = (2 * nt_ + 1) % 4
                            ti = nt_ % 2
                            nt_ += 1
                            for k_ in range(KC):
                                sch.op("pe", mm(ps[pg][:, 0:HN], Wu[bi][:, k_, 0, jj * 128:(jj + 1) * 128], xgT[:, k_, hf * HN:(hf + 1) * HN],
                                                k_ == 0, k_ == KC - 1), reads=[b_Wu[bi], b_xgT], writes=[psb[pg]])
                            for k_ in range(KC):
                                sch.op("pe", mm(ps[pl][:, 0:HN], Wu[bi][:, k_, 1, jj * 128:(jj + 1) * 128], xgT[:, k_, hf * HN:(hf + 1) * HN],
                                                k_ == 0, k_ == KC - 1), reads=[b_Wu[bi], b_xgT], writes=[psb[pl]])
                            bg = bup[:, eb, j:j + 1]
                            bl = bup[:, eb, 16 + j:17 + j]
                            sch.op("dve", lambda e, ti=ti, pg=pg, bg=bg: e.tensor_scalar(out=tg_[ti][:], in0=ps[pg][:, 0:HN], scalar1=bg, scalar2=7.0,
                                                                                     op0=ALU.add, op1=ALU.min),
                                   reads=[psb[pg], b_bup[eb]], writes=[b_tg[ti]], same=False)
                            sch.op("act", lambda e, ti=ti: e.activation(out=ts_[ti][:], in_=tg_[ti][:], func=AF.Sigmoid, scale=1.702),
                                   reads=[b_tg[ti]], writes=[b_ts[ti]])
                            sch.op("dve", lambda e, ti=ti, pl=pl, bl=bl: e.tensor_scalar(out=tl_[ti][:], in0=ps[pl][:, 0:HN], scalar1=bl, scalar2=7.0,
                                                                                     op0=ALU.add, op1=ALU.min),
                                   reads=[psb[pl], b_bup[eb]], writes=[b_tl[ti]], same=False)
                            sch.op("dve", lambda e, ti=ti: e.tensor_scalar(out=tl_[ti][:], in0=tl_[ti][:], scalar1=-7.0, scalar2=1.0,
                                                                          op0=ALU.max, op1=ALU.add), reads=[b_tl[ti]], writes=[b_tl[ti]], same=False)
                            sch.op("dve", lambda e, ti=ti: e.tensor_tensor(out=tg_[ti][:], in0=tg_[ti][:], in1=ts_[ti][:], op=ALU.mult),
                                   reads=[b_tg[ti], b_ts[ti]], writes=[b_tg[ti]], same=False)
                            sch.op("dve", lambda e, ti=ti, j=j, hf=hf: e.tensor_tensor(out=actT[:, j, hf * HN:(hf + 1) * HN], in0=tg_[ti][:], in1=tl_[ti][:],
                                                                                      op=ALU.mult),
                                   reads=[b_tg[ti], b_tl[ti]], writes=[b_actT], same=False)
                    issue_u(uidx + PDU)
                    uidx += 1
                for nt in range(4):
                    bi = didx % NDNB
                    for blk in range(CAPB):
                        pi = 6 + nps % 2
                        nps += 1
                        for k_ in range(KC):
                            sch.op("pe", mm(ps[pi][:, :], actT[:, k_, blk * 128:(blk + 1) * 128], Wd[bi][:, k_, :], k_ == 0, k_ == KC - 1),
                                   reads=[b_Wd[bi], b_actT], writes=[psb[pi]])
                        yi = nys % 3
                        nys += 1
                        sch.op("dve", lambda e, yi=yi, pi=pi, eb=eb, nt=nt: e.tensor_tensor(out=yst[yi][:], in0=ps[pi][:, :],
                                                                                       in1=bdn[eb][:, nt * 512:(nt + 1) * 512], op=ALU.add),
                               reads=[psb[pi], b_bdn[eb]], writes=[b_yst[yi]])
                        r0 = rbase + blk * 128
                        wst = min(WY, 512)
                        for hh in range(512 // wst):
                            c0 = nt * 512 + hh * wst
                            qy, cq = c0 // WY, c0 % WY
                            sch.dma("sp", lambda e, yi=yi, r0=r0, qy=qy, cq=cq, hh=hh, wst=wst: e.dma_start(
                                out=Y[qy][r0:r0 + 128, cq:cq + wst], in_=yst[yi][:, hh * wst:(hh + 1) * wst]),
                                d_yst[yi], reads=[b_yst[yi]], writes=[bY])
                    issue_d(didx + PDD)
                    didx += 1
            if debug:
                dx = dscr("dbg_xgT", [128, KC, CAP], BF16)
                da = dscr("dbg_actT", [128, KC, CAP], BF16)
                sch.dma("sp", lambda e: e.dma_start(out=dx, in_=xgT[:]), d_c, reads=[b_xgT], writes=[Buf()])
                sch.dma("sp", lambda e: e.dma_start(out=da, in_=actT[:]), d_c, reads=[b_actT], writes=[Buf()])
            sch.barrier()
            sch.emit()

    def stage_combine():
        with ExitStack() as es:
            def sb(name, shape, dt):
                return es.enter_context(nc.sbuf_tensor("c_" + name, shape, dt))
            yk = [[sb("yk%d_%d" % (i, k_), [128, D], F32) for k_ in range(4)] for i in range(2)]
            h1 = [sb("h1%d" % i, [128, D], F32) for i in range(2)]
            hp = sb("hp", [128, D], F32)
            sq = sb("sq", [128, D], BF16)
            ot = [sb("ot%d" % i, [128, D], F32) for i in range(2)]
            gbc = sb("gbc", [128, D], F32)
            bbc = sb("bbc", [128, D], F32)
            st = sb("st", [128, 8], F32)
            ri = [sb("ri%d" % i, [128, 8], F32) for i in range(2)]
            rw = [[sb("rw%d_%d" % (i, k_), [128, 1], I32) for k_ in range(4)] for i in range(2)]
            b_yk, b_h1, b_ot, b_ri, b_rw = [[Buf(), Buf()] for _ in range(5)]
            b_c, b_hp, b_sq, b_st = Buf(), Buf(), Buf(), Buf()
            d_c = sch.dsem()
            d_yk, d_h1, d_ot, d_ri, d_rw = [[sch.dsem(), sch.dsem()] for _ in range(5)]
            bout = Buf()
            sch.dma("sp", lambda e: e.dma_start(out=gbc[:], in_=ln_gb[2:3, :].partition_broadcast(128)), d_c, writes=[b_c])
            sch.dma("sp", lambda e: e.dma_start(out=bbc[:], in_=ln_gb[3:4, :].partition_broadcast(128)), d_c, writes=[b_c])
            for c in range(NT):
                tok0 = c * 128
                cb = c % 2
                sch.dma("sp", lambda e, cb=cb, tok0=tok0: e.dma_start(out=ri[cb][:], in_=RI[tok0:tok0 + 128, :]), d_ri[cb],
                        reads=[bRI], writes=[b_ri[cb]])
                for k_ in range(4):
                    sch.dma("sp", lambda e, cb=cb, tok0=tok0, k_=k_: e.dma_start(out=rw[cb][k_][:], in_=ROWI[k_][tok0:tok0 + 128, :]), d_rw[cb],
                            reads=[bRI], writes=[b_rw[cb]])
                sch.dma("sp", lambda e, cb=cb, tok0=tok0: e.dma_start(out=h1[cb][:], in_=H1[tok0:tok0 + 128, :]), d_h1[cb],
                        reads=[bH1], writes=[b_h1[cb]])
                for k_ in range(4):
                    for q in range(NQY):
                        sch.dma("pool", lambda e, cb=cb, k_=k_, q=q: e.indirect_dma_start(
                            out=yk[cb][k_][:, q * WY:(q + 1) * WY], out_offset=None, in_=Yr[q][:, :],
                            in_offset=bass.IndirectOffsetOnAxis(ap=rw[cb][k_][:, 0:1], axis=0)),
                            d_yk[cb], reads=[bYr, b_rw[cb]], writes=[b_yk[cb]])
                sch.op("act", lambda e, cb=cb: e.activation(out=hp[:], in_=h1[cb][:], func=AF.Copy, scale=DN_ALPHA), reads=[b_h1[cb]], writes=[b_hp])
                for k_ in range(4):
                    sch.op("dve", lambda e, cb=cb, k_=k_: e.scalar_tensor_tensor(out=hp[:], in0=yk[cb][k_][:], scalar=ri[cb][:, 4 + k_:5 + k_],
                                                                              in1=hp[:], op0=ALU.mult, op1=ALU.add),
                           reads=[b_yk[cb], b_ri[cb], b_hp], writes=[b_hp], same=False)
                layer_norm_tile(sb, hp, b_hp, ot[cb], b_ot[cb], gbc, bbc, b_c, sq, b_sq, st, b_st)
                sch.dma("sp", lambda e, cb=cb, tok0=tok0: e.dma_start(out=out[tok0:tok0 + 128, :], in_=ot[cb][:]), d_ot[cb],
                        reads=[b_ot[cb]], writes=[bout])
            sch.barrier()
            sch.emit()

    if upto >= 6:
        if ep:
            all_gather(XG, XGr, bXG, bXGr)
        stage_experts()
    if upto >= 7:
        if ep:
            all_gather(Y, Yr, bY, bYr)
        stage_combine()

    sch.barrier()
    sch.emit()
    return nc


def prep_inputs(inp, b, S, consts=None, ep=False):
    m = {}
    m["x"] = np.ascontiguousarray(inp["x"][b, :S])
    m["w_in"] = inp["w_in"][0]
    b_in = inp["b_in"][0]
    b_fm = np.zeros((128, NPF + 1), np.float32)
    for (kind, col0, ncols, dl, pfc) in fm_jobs():
        b_fm[:ncols, pfc] = b_in[col0:col0 + ncols]
    cols = list(range(1792, 2048)) + list(range(2304, 2560))
    for gi in range(3):
        c0 = 2584 + gi * 1536 + 1024
        cols += list(range(c0, c0 + 512))
    m["b_fm"] = b_fm
    m["b_tm"] = np.ascontiguousarray(b_in[cols][None, :])
    m.update(consts if consts is not None else host_consts(S))
    for k in ("cmp_k_w1", "cmp_v_w1", "cmp_k_w2", "cmp_v_w2"):
        m[k] = inp[k][0]
    m["posT_k"] = np.ascontiguousarray(inp["cmp_pos_k"][0].T)
    m["posT_v"] = np.ascontiguousarray(inp["cmp_pos_v"][0].T)
    m["w_br_nsa"] = inp["w_br_nsa"][0]
    m["w_br_dil"] = inp["w_br_dil"][0]
    m["w_out"] = inp["w_out"][0]
    m["ln_gb"] = np.ascontiguousarray(np.stack([inp["ln1_g"][0], inp["ln1_b"][0], inp["ln2_g"][0], inp["ln2_b"][0]], 0))
    m["w_router"] = inp["w_router"][0]
    m["b_router"] = inp["b_router"]
    esl = slice(b * (NE // 8), (b + 1) * (NE // 8)) if ep else slice(0, NE)
    nel = NE // 8 if ep else NE
    m["w_up"] = inp["w_up"][0][esl]
    m["w_down"] = inp["w_down"][0][esl]
    m["b_up_fm"] = np.ascontiguousarray(inp["b_up"][0][esl].reshape(nel, 32, 128).transpose(2, 0, 1).reshape(128, nel * 32))
    m["b_down"] = inp["b_down"][0][esl]
    if ep:
        cap = CAPB * 128
        nrow = NE * cap
        e_ = np.arange(NE)
        m["ebase_y"] = np.tile(((e_ // 4) * nrow + b * 4 * cap + (e_ % 4) * cap).astype(np.float32)[None, :], (128, 1))
        ex = np.arange(NE)
        le, sc = ex // 8, ex % 8
        blk = np.arange(CAPB)
        base = (sc[:, None] * nrow + (b * 4 + le[:, None]) * cap + blk[None, :] * 128).reshape(-1)
        m["xg_idx"] = np.ascontiguousarray((base[None, :] + np.arange(128)[:, None]).astype(np.int32))
    return m


def kernel(**inputs):
    S = 4096
    inp = {k: np.asarray(v) for k, v in inputs.items()}
    consts = host_consts(S)
    nc = build(S, ep=False)
    in_maps = [prep_inputs(inp, b, S, consts, ep=False) for b in range(8)]
    res = run_bass_kernel_spmd(nc, in_maps, core_ids=list(range(8)))
    return np.stack([np.asarray(r["out"], dtype=np.float32) for r in res.results], 0)
```

```python
from contextlib import ExitStack
import numpy as np
import ml_dtypes
import concourse.bass as bass
import concourse.mybir as mybir
from concourse.bass_utils import run_bass_kernel_spmd

F32 = mybir.dt.float32
BF16 = mybir.dt.bfloat16
I32 = mybir.dt.int32
U32 = mybir.dt.uint32
AF = mybir.ActivationFunctionType
ALU = mybir.AluOpType
AX = mybir.AxisListType

D = 2048
KC = 16
IN_W = 11288
NEGB = -30000.0
SCALE = 128 ** -0.5
DILS = (1, 4, 16)
DN_ALPHA = 2.0 ** 0.25
LN_EPS = 1e-5
NE = 32
CAPB = 5
DBG_BR = "csw"
SAME_ENG_SYNC = True


class Buf:
    __slots__ = ("name", "w", "r")

    def __init__(self, name=""):
        self.name = name
        self.w = None
        self.r = {}


class DSem:
    __slots__ = ("key", "sem", "cnt")


class Sched:
    ENG = ("pe", "act", "dve", "pool", "sp")

    def __init__(self, nc):
        self.nc = nc
        self.prog = {e: [] for e in self.ENG}
        self.cnt = {e: 0 for e in self.ENG}
        self.sems = {}
        self.seen = {e: {} for e in self.ENG}
        self.dsems = []
        for e in ("pe", "act", "dve", "pool"):
            self.sems[e] = nc.alloc_semaphore("prog_" + e)

    def dsem(self):
        d = DSem()
        d.key = "d%d" % len(self.dsems)
        d.sem = self.nc.alloc_semaphore("dma_" + d.key)
        d.cnt = 0
        self.sems[d.key] = d.sem
        self.dsems.append(d)
        return d

    def _wait(self, eng, k, i):
        seen = self.seen[eng]
        if k == eng and (eng == "pe" or not SAME_ENG_SYNC):
            return
        if i > 0 and seen.get(k, 0) < i:
            seen[k] = i
            sem = self.sems[k]
            self.prog[eng].append(lambda e, sem=sem, i=i: e.wait_ge(sem, i))

    def _waits(self, eng, reads, writes, same=True):
        need = {}

        def add(dep):
            if dep is not None and (same or dep[0] != eng) and need.get(dep[0], 0) < dep[1]:
                need[dep[0]] = dep[1]
        for b in reads:
            add(b.w)
        for b in writes:
            add(b.w)
            for k, i in b.r.items():
                add((k, i))
        for k, i in need.items():
            self._wait(eng, k, i)

    def op(self, eng, fn, reads=(), writes=(), same=True):
        self._waits(eng, reads, writes, same)
        self.cnt[eng] += 1
        idx = self.cnt[eng]
        sem = self.sems[eng]
        self.prog[eng].append(lambda e, fn=fn, sem=sem: fn(e).then_inc(sem, 1))
        for b in reads:
            if b.r.get(eng, 0) < idx:
                b.r[eng] = idx
        for b in writes:
            b.w = (eng, idx)
            b.r = {}

    def dma(self, q, fn, ds, reads=(), writes=()):
        self._waits(q, reads, writes)
        ds.cnt += 16
        idx = ds.cnt
        sem = ds.sem
        self.prog[q].append(lambda e, fn=fn, sem=sem: fn(e).then_inc(sem, 16))
        for b in reads:
            if b.r.get(ds.key, 0) < idx:
                b.r[ds.key] = idx
        for b in writes:
            b.w = (ds.key, idx)
            b.r = {}

    def barrier(self):
        for e in self.ENG:
            for k in ("pe", "act", "dve", "pool"):
                self._wait(e, k, self.cnt[k])
            for d in self.dsems:
                self._wait(e, d.key, d.cnt)

    def emit(self):
        nc = self.nc
        prog = self.prog
        with nc.Block() as block:
            @block.tensor
            def _(e):
                for f in prog["pe"]:
                    f(e)

            @block.scalar
            def _(e):
                for f in prog["act"]:
                    f(e)

            @block.vector
            def _(e):
                for f in prog["dve"]:
                    f(e)

            @block.gpsimd
            def _(e):
                for f in prog["pool"]:
                    f(e)

            @block.sync
            def _(e):
                for f in prog["sp"]:
                    f(e)
        self.prog = {e: [] for e in self.ENG}


def fm_jobs():
    jobs = []
    for hd in range(8):
        jobs.append(("rope", hd * 128, 128, 1, hd))
    for g in range(2):
        jobs.append(("rope", 1024 + g * 128, 128, 1, 8 + g))
    for g in range(2):
        jobs.append(("plain", 1280 + g * 128, 128, 1, 10 + g))
    for g in range(2):
        jobs.append(("rope", 1536 + g * 128, 128, 1, 12 + g))
    for g in range(2):
        jobs.append(("rope", 2048 + g * 128, 128, 1, 14 + g))
    for gi in range(3):
        base = 2584 + gi * 1536
        for hd in range(4):
            jobs.append(("rope", base + hd * 128, 128, DILS[gi], 16 + gi * 4 + hd))
        for hd in range(4):
            jobs.append(("rope", base + 512 + hd * 128, 128, DILS[gi], 28 + gi * 4 + hd))
    for i in range(16):
        jobs.append(("sig", 7192 + i * 128, 128, 1, 40 + i))
    for i in range(16):
        jobs.append(("sig", 9240 + i * 128, 128, 1, 56 + i))
    jobs.append(("gl", 2560, 24, 1, 72))
    return jobs


NPF = 72


def host_consts(S):
    c = {}
    c["identb"] = np.eye(128, dtype=np.float32).astype(ml_dtypes.bfloat16)
    c["identf"] = np.eye(128, dtype=np.float32)
    pos = np.arange(S, dtype=np.float32)
    inv = (np.float32(10000.0) ** (-np.arange(0, 128, 2, dtype=np.float32) / np.float32(128))).astype(np.float32)
    ang = (pos[:, None] * inv[None, :]).astype(np.float32)
    cos = np.cos(ang).astype(np.float32).T
    sin = np.sin(ang).astype(np.float32).T
    c["cosT"] = np.ascontiguousarray(np.concatenate([cos, cos], 0))
    c["sinT"] = np.ascontiguousarray(np.concatenate([sin, -sin], 0))
    NSLC = S // 64
    NCMP = (S - 32) // 16 + 1
    bf = ml_dtypes.bfloat16
    sidx = np.arange(S)
    c["Eall"] = (sidx[None, :] // 64 == np.arange(NSLC)[:, None]).astype(np.float32).astype(bf)
    sl = np.arange(128)[:, None]
    tl = np.arange(512)[None, :]

    def band(lo, hi, dlt):
        v = tl - dlt - sl
        return np.where((v >= lo) & (v <= hi), 0.0, NEGB).astype(np.float32)
    tiles = [band(0, 1 << 30, 128 * k) for k in range(4)]
    tiles += [band(0, 511, -512 + 128 * k) for k in range(8)]
    tiles += [band(0, 128, -128 + 128 * k) for k in range(5)]
    c["BT"] = np.ascontiguousarray(np.stack(tiles, 1)).astype(bf)
    cidx = np.arange(256)
    cm = np.where((16 * cidx[:, None] + 31 <= sidx[None, :]) & (cidx[:, None] < NCMP), 0.0, NEGB).astype(np.float32)
    c["CMB"] = np.ascontiguousarray(cm.reshape(2, 128, S).transpose(1, 0, 2)).astype(bf)
    j = np.arange(NSLC)[None, :]
    cur = (sidx // 64)[:, None]
    forced = (j == 0) | (j == cur) | (j == cur - 1)
    future = j > cur
    c["KEEP"] = np.where(forced | future, 0.0, 1.0).astype(np.float32)
    c["ADD"] = np.where(forced, 1e9, np.where(future, -1e30, 0.0)).astype(np.float32)
    cs = cidx[:, None] * 16
    ov = (cs < (j + 1) * 64) & (cs + 32 > j * 64) & (cidx[:, None] < NCMP)
    c["ovl"] = ov.astype(np.float32).astype(bf)
    p_ = np.arange(128)
    c["ustr"] = (p_[:, None] < p_[None, :]).astype(np.float32).astype(bf)
    c["ebase"] = np.tile((np.arange(NE, dtype=np.float32) * (CAPB * 128))[None, :], (128, 1)).astype(np.float32)
    return c


CONST_SPECS = {
    "identb": ([128, 128], BF16), "identf": ([128, 128], F32),
    "cosT": ([128, None], F32), "sinT": ([128, None], F32),
    "Eall": ([-64, None], BF16), "BT": ([128, 17, 512], BF16), "CMB": ([128, 2, None], BF16),
    "KEEP": ([None, -64], F32), "ADD": ([None, -64], F32), "ovl": ([256, -64], BF16),
    "ustr": ([128, 128], BF16), "ebase": ([128, NE], F32),
}


def build(S, upto=99, debug=False, ep=False):
    nc = bass.Bass("TRN2", target_bir_lowering=False)
    okind = "ExternalOutput" if debug else "Internal"

    def din(name, shape, dt=F32):
        return nc.dram_tensor(name, shape, dt, kind="ExternalInput").ap()

    def dscr(name, shape, dt):
        return nc.dram_tensor(name, shape, dt, kind=okind).ap()

    x = din("x", [S, D])
    w_in = din("w_in", [D, IN_W])
    b_fm = din("b_fm", [128, NPF + 1])
    b_tm = din("b_tm", [1, 2048])
    cst = {}
    for k, (shp, dt) in CONST_SPECS.items():
        cst[k] = din(k, [S if s is None else (S // 64 if s == -64 else s) for s in shp], dt)
    PF = dscr("PF", [NPF * 128, S], BF16)
    G = dscr("G", [24, S], F32)
    PTn = dscr("PTn", [S, 512], BF16)
    PTd = [dscr("PTd%d" % g, [S, 512], BF16) for g in range(3)]
    out = nc.dram_tensor("out", [S, D], F32, kind="ExternalOutput").ap()

    sch = Sched(nc)
    ps = [nc.alloc_psum_tensor("ps%d" % i, [128, 512], F32).ap() for i in range(8)]
    psb = [Buf("ps%d" % i) for i in range(8)]
    bPF, bG, bPTn = Buf("PF"), Buf("G"), Buf("PTn")
    bPTd = [Buf("PTd%d" % g) for g in range(3)]

    def stage1():
        with ExitStack() as es:
            def sb(name, shape, dt):
                return es.enter_context(nc.sbuf_tensor("s1_" + name, shape, dt))
            HT = min(S, 2048)
            NH = S // HT
            xT = sb("xT", [128, KC, HT], BF16)
            xb = [sb("xb%d" % i, [128, D], BF16) for i in range(2)]
            identb = sb("identb", [128, 128], BF16)
            cosT = sb("cosT", [128, HT], F32)
            sinT = sb("sinT", [128, HT], F32)
            bfm = sb("bfm", [128, NPF + 1], F32)
            btm = sb("btm", [128, 2048], F32)
            wt = [sb("wt%d" % i, [128, KC, 512], BF16) for i in range(2)]
            stg = [sb("stg%d" % i, [128, HT], BF16) for i in range(2)]
            stgG = sb("stgG", [24, HT], F32)
            stgT = [sb("stgT%d" % i, [128, 512], BF16) for i in range(2)]
            tA = [sb("tA%d" % i, [128, 512], F32) for i in range(2)]
            t1 = [sb("t1%d" % i, [128, 512], F32) for i in range(2)]
            t2 = [sb("t2%d" % i, [128, 512], F32) for i in range(2)]
            b_xT = [Buf() for _ in range(HT // 128)]
            b_xb = [Buf(), Buf()]
            b_c, b_cos, b_sin = Buf(), Buf(), Buf()
            b_wt = [Buf(), Buf()]
            b_stg = [Buf(), Buf()]
            b_stgG = Buf()
            b_stgT = [Buf(), Buf()]
            b_tA, b_t1, b_t2 = [Buf(), Buf()], [Buf(), Buf()], [Buf(), Buf()]
            d_c = sch.dsem()
            d_xb = [sch.dsem(), sch.dsem()]
            d_wt = [sch.dsem(), sch.dsem()]
            d_stg = [sch.dsem(), sch.dsem()]
            d_stgT = [sch.dsem(), sch.dsem()]
            d_tab = sch.dsem()
            sch.dma("sp", lambda e: e.dma_start(out=identb[:], in_=cst["identb"]), d_c, writes=[b_c])
            sch.dma("sp", lambda e: e.dma_start(out=bfm[:], in_=b_fm), d_c, writes=[b_c])
            sch.dma("sp", lambda e: e.dma_start(out=btm[:], in_=b_tm.partition_broadcast(128)), d_c, writes=[b_c])
            jobs = fm_jobs()
            cnt = {"ps": 0, "wt": 0, "stg": 0, "stgT": 0, "t": 0, "ev": 0}
            for h in range(NH):
                h0 = h * HT
                sch.dma("sp", lambda e, h0=h0: e.dma_start(out=cosT[:], in_=cst["cosT"][:, h0:h0 + HT]), d_tab, writes=[b_cos])
                sch.dma("sp", lambda e, h0=h0: e.dma_start(out=sinT[:], in_=cst["sinT"][:, h0:h0 + HT]), d_tab, writes=[b_sin])
                for c in range(HT // 128):
                    i = c % 2
                    tok0 = h0 + c * 128
                    sch.dma("pool", lambda e, i=i, tok0=tok0: e.dma_start(out=xb[i][:], in_=x[tok0:tok0 + 128, :]),
                            d_xb[i], writes=[b_xb[i]])
                    for q4 in range(4):
                        pi = cnt["ps"] % 4
                        cnt["ps"] += 1
                        pv = ps[pi].bitcast(BF16)
                        for j in range(4):
                            kc = q4 * 4 + j
                            sch.op("pe", lambda e, pv=pv, j=j, kc=kc, i=i: e.transpose(
                                pv[:, j * 128:(j + 1) * 128], xb[i][:, kc * 128:(kc + 1) * 128], identb[:]),
                                reads=[b_xb[i], b_c], writes=[psb[pi]])
                        src = pv[:, 0:512].rearrange("p (a b) -> p a b", a=4)
                        dst = xT[:, q4 * 4:(q4 + 1) * 4, c * 128:(c + 1) * 128]
                        if (c * 4 + q4) % 2 == 0:
                            sch.op("act", lambda e, src=src, dst=dst: e.activation(out=dst, in_=src, func=AF.Copy),
                                   reads=[psb[pi]], writes=[b_xT[c]])
                        else:
                            sch.op("dve", lambda e, src=src, dst=dst: e.tensor_copy(out=dst, in_=src),
                                   reads=[psb[pi]], writes=[b_xT[c]])
                ngrp = (len(jobs) + 3) // 4
                for gi in range(ngrp):
                    grp = jobs[gi * 4:(gi + 1) * 4]
                    wi = cnt["wt"] % 2
                    cnt["wt"] += 1
                    j0 = 0
                    while j0 < len(grp):
                        j1 = j0 + 1
                        while j1 < len(grp) and grp[j1][1] == grp[j1 - 1][1] + grp[j1 - 1][2] and grp[j1 - 1][2] == 128:
                            j1 += 1
                        c0 = grp[j0][1]
                        ncol = sum(g_[2] for g_ in grp[j0:j1])
                        src = w_in[:, c0:c0 + ncol].rearrange("(kc p) n -> p kc n", p=128)
                        dst = wt[wi][:, :, j0 * 128:j0 * 128 + ncol]
                        sch.dma("pool", lambda e, src=src, dst=dst: e.dma_start(out=dst, in_=src), d_wt[wi], writes=[b_wt[wi]])
                        j0 = j1
                    for jj, (kind, col0, ncols, dl, pfc) in enumerate(grp):
                        si = cnt["stg"] % 2
                        if kind != "gl":
                            cnt["stg"] += 1
                        for tt in range(HT // 512):
                            pi = 4 + cnt["ps"] % 4
                            cnt["ps"] += 1
                            for kc in range(KC):
                                sch.op("pe", lambda e, pi=pi, wi=wi, jj=jj, ncols=ncols, kc=kc, tt=tt: e.matmul(
                                    ps[pi][0:ncols, :], wt[wi][:, kc, jj * 128:jj * 128 + ncols], xT[:, kc, tt * 512:(tt + 1) * 512],
                                    start=(kc == 0), stop=(kc == KC - 1)),
                                    reads=[b_wt[wi]] + b_xT[tt * 4:(tt + 1) * 4], writes=[psb[pi]])
                            bias = bfm[0:ncols, pfc:pfc + 1]
                            tsl = slice(tt * 512, (tt + 1) * 512)
                            if kind == "plain":
                                sch.op("act", lambda e, pi=pi, si=si, bias=bias, tsl=tsl: e.activation(
                                    out=stg[si][:, tsl], in_=ps[pi][:], func=AF.Identity, bias=bias),
                                    reads=[psb[pi], b_c], writes=[b_stg[si]])
                            elif kind == "sig":
                                sch.op("act", lambda e, pi=pi, si=si, bias=bias, tsl=tsl: e.activation(
                                    out=stg[si][:, tsl], in_=ps[pi][:], func=AF.Sigmoid, bias=bias),
                                    reads=[psb[pi], b_c], writes=[b_stg[si]])
                            elif kind == "gl":
                                sch.op("act", lambda e, pi=pi, bias=bias, tsl=tsl: e.activation(
                                    out=stgG[:, tsl], in_=ps[pi][0:24, :], func=AF.Sigmoid, bias=bias),
                                    reads=[psb[pi], b_c], writes=[b_stgG])
                            else:
                                ti = cnt["t"] % 2
                                cnt["t"] += 1
                                sch.op("act", lambda e, pi=pi, ti=ti, bias=bias: e.activation(
                                    out=tA[ti][:], in_=ps[pi][:], func=AF.Identity, bias=bias),
                                    reads=[psb[pi], b_c], writes=[b_tA[ti]])
                                sch.op("dve", lambda e, ti=ti, tsl=tsl: e.tensor_tensor(
                                    out=t1[ti][:], in0=tA[ti][:], in1=cosT[:, tsl], op=ALU.mult),
                                    reads=[b_tA[ti], b_cos], writes=[b_t1[ti]])
                                sch.op("pool", lambda e, ti=ti, tsl=tsl: e.tensor_tensor(
                                    out=t2[ti][0:64, :], in0=tA[ti][64:128, :], in1=sinT[64:128, tsl], op=ALU.mult),
                                    reads=[b_tA[ti], b_sin], writes=[b_t2[ti]])
                                sch.op("pool", lambda e, ti=ti, tsl=tsl: e.tensor_tensor(
                                    out=t2[ti][64:128, :], in0=tA[ti][0:64, :], in1=sinT[0:64, tsl], op=ALU.mult),
                                    reads=[b_tA[ti], b_sin], writes=[b_t2[ti]])
                                if dl == 1:
                                    o_ap = stg[si][:, tsl]
                                    i0 = t1[ti][:]
                                    i1 = t2[ti][:]
                                else:
                                    npos = 512 // dl
                                    il0 = tt * npos
                                    o_ap = stg[si][:].rearrange("p (r i) -> p r i", r=dl)[:, :, il0:il0 + npos]
                                    i0 = t1[ti][:].rearrange("p (i r) -> p r i", r=dl)
                                    i1 = t2[ti][:].rearrange("p (i r) -> p r i", r=dl)
                                sch.op("dve", lambda e, o_ap=o_ap, i0=i0, i1=i1: e.tensor_tensor(
                                    out=o_ap, in0=i0, in1=i1, op=ALU.add),
                                    reads=[b_t1[ti], b_t2[ti]], writes=[b_stg[si]])
                        if kind == "gl":
                            sch.dma("sp", lambda e, h0=h0: e.dma_start(out=G[:, h0:h0 + HT], in_=stgG[:]), d_stg[0],
                                    reads=[b_stgG], writes=[bG])
                        elif dl == 1:
                            sch.dma("sp", lambda e, pfc=pfc, si=si, h0=h0: e.dma_start(
                                out=PF[pfc * 128:(pfc + 1) * 128, h0:h0 + HT], in_=stg[si][:]), d_stg[si],
                                reads=[b_stg[si]], writes=[bPF])
                        else:
                            L = S // dl
                            hl = HT // dl
                            dst = PF[pfc * 128:(pfc + 1) * 128, :].rearrange("p (r i) -> p r i", r=dl)[:, :, h * hl:(h + 1) * hl]
                            src = stg[si][:].rearrange("p (r i) -> p r i", r=dl)
                            sch.dma("sp", lambda e, dst=dst, src=src: e.dma_start(out=dst, in_=src), d_stg[si],
                                    reads=[b_stg[si]], writes=[bPF])
                tmj = [(None, 1, PTn, bPTn)] + [(2584 + gi * 1536 + 1024, DILS[gi], PTd[gi], bPTd[gi]) for gi in range(3)]
                for ti_, (col0, dl, dst_t, dst_b) in enumerate(tmj):
                    wi = cnt["wt"] % 2
                    cnt["wt"] += 1
                    if col0 is None:
                        for half, c0 in enumerate((1792, 2304)):
                            src = w_in[:, c0:c0 + 256].rearrange("(kc p) n -> p kc n", p=128)
                            dst = wt[wi][:, :, half * 256:(half + 1) * 256]
                            sch.dma("pool", lambda e, src=src, dst=dst: e.dma_start(out=dst, in_=src), d_wt[wi], writes=[b_wt[wi]])
                    else:
                        src = w_in[:, col0:col0 + 512].rearrange("(kc p) n -> p kc n", p=128)
                        sch.dma("pool", lambda e, src=src, wi=wi: e.dma_start(out=wt[wi][:], in_=src), d_wt[wi], writes=[b_wt[wi]])
                    hl = HT // dl
                    npos = min(128, hl)
                    for r in range(dl):
                        for j in range(hl // npos):
                            pi = 4 + cnt["ps"] % 4
                            cnt["ps"] += 1
                            t_lo = r + dl * npos * j
                            xbufs = b_xT[(t_lo // 128):((t_lo + dl * (npos - 1)) // 128) + 1]
                            for kc in range(KC):
                                lhsT = xT[:, kc, t_lo:t_lo + dl * (npos - 1) + 1:dl]
                                sch.op("pe", lambda e, pi=pi, wi=wi, kc=kc, lhsT=lhsT, npos=npos: e.matmul(
                                    ps[pi][0:npos, :], lhsT, wt[wi][:, kc, :], start=(kc == 0), stop=(kc == KC - 1)),
                                    reads=[b_wt[wi]] + xbufs, writes=[psb[pi]])
                            si = cnt["stgT"] % 2
                            cnt["stgT"] += 1
                            bsl = btm[0:npos, ti_ * 512:(ti_ + 1) * 512]
                            sch.op("dve", lambda e, pi=pi, si=si, bsl=bsl, npos=npos: e.tensor_tensor(
                                out=stgT[si][0:npos, :], in0=ps[pi][0:npos, :], in1=bsl, op=ALU.add),
                                reads=[psb[pi], b_c], writes=[b_stgT[si]])
                            row0 = r * (S // dl) + h * hl + j * npos
                            sch.dma("sp", lambda e, dst_t=dst_t, row0=row0, npos=npos, si=si: e.dma_start(
                                out=dst_t[row0:row0 + npos, :], in_=stgT[si][0:npos, :]), d_stgT[si],
                                reads=[b_stgT[si]], writes=[dst_b])
            sch.barrier()
            sch.emit()

    if upto >= 1:
        stage1()

    NCMP = (S - 32) // 16 + 1
    NSLC = S // 64
    NT = S // 128
    QN = min(512, S)
    NQT = S // QN
    ON = dscr("ON", [1024, S], BF16)
    OD = dscr("OD", [512, S], BF16)
    bON, bOD = Buf("ON"), Buf("OD")
    cmp_w1 = [din("cmp_k_w1", [4096, 256]), din("cmp_v_w1", [4096, 256])]
    cmp_w2 = [din("cmp_k_w2", [256, 128]), din("cmp_v_w2", [256, 128])]
    posT = [din("posT_k", [128, 32]), din("posT_v", [128, 32])]

    class ACtx:
        pass

    def make_actx(sb, tag):
        a = ACtx()
        a.pT = [sb(tag + "pT%d" % i, [128, 512], BF16) for i in range(3)]
        a.b_pT = [Buf() for _ in range(3)]
        a.rd = [sb(tag + "rd%d" % i, [128, 512], F32) for i in range(2)]
        a.b_rd = [Buf(), Buf()]
        a.tmp = [sb(tag + "tmp%d" % i, [128, 512], F32) for i in range(2)]
        a.b_tmp = [Buf(), Buf()]
        a.n = 0
        a.k = 0
        a.e = 0
        return a

    def mm(out_ap, l_ap, r_ap, st, sp_):
        return lambda e: e.matmul(out_ap, l_ap, r_ap, start=st, stop=sp_)

    def attn_tile(a, qT, qbufs, N, chunks, ones_ap, cbuf):
        ni = 2 + a.n % 2
        di = 4 + a.n % 2
        a.n += 1
        nch = len(chunks)

        def emit_s(i):
            ch = chunks[i]
            si = (a.k + i) % 2
            kn = ch["kn"]
            nx = len(ch["extra"])
            sch.op("pe", mm(ps[si][0:kn, 0:N], ch["kT"], qT, True, nx == 0),
                   reads=list(qbufs) + list(ch["kvbufs"]), writes=[psb[si]])
            for xi, (l_ap, r_ap, bufs) in enumerate(ch["extra"]):
                sch.op("pe", mm(ps[si][0:kn, 0:N], l_ap, r_ap, False, xi == nx - 1), reads=list(bufs), writes=[psb[si]])
            pi = (a.k + i) % 3
            sch.op("act", lambda e, o=a.pT[pi][0:kn, 0:N], i_=ps[si][0:kn, 0:N]: e.activation(out=o, in_=i_, func=AF.Exp, scale=SCALE),
                   reads=[psb[si]], writes=[a.b_pT[pi]])
        emit_s(0)
        for i in range(nch):
            if i + 1 < nch:
                emit_s(i + 1)
            ch = chunks[i]
            kn = ch["kn"]
            pi = (a.k + i) % 3
            sch.op("pe", mm(ps[ni][:, 0:N], ch["v"], a.pT[pi][0:kn, 0:N], i == 0, i == nch - 1),
                   reads=[a.b_pT[pi]] + list(ch["kvbufs"]), writes=[psb[ni]])
            sch.op("pe", mm(ps[di][:, 0:N], ones_ap[0:kn, :], a.pT[pi][0:kn, 0:N], i == 0, i == nch - 1),
                   reads=[a.b_pT[pi], cbuf], writes=[psb[di]])
        a.k += nch
        return ni, di

    def attn_epilogue(a, ni, di, N, gate_ap, gbufs, acc_ap, acc_buf, first):
        ri = a.e % 2
        a.e += 1
        rd, brd = a.rd[ri], a.b_rd[ri]
        sch.op("dve", lambda e: e.tensor_scalar_max(out=rd[:, 0:N], in0=ps[di][:, 0:N], scalar1=1e-30), reads=[psb[di]], writes=[brd])
        wide = N >= 256
        sch.op("dve", lambda e: e.reciprocal(out=rd[:, 0:N], in_=rd[:, 0:N]), reads=[brd], writes=[brd], same=not wide)
        if gate_ap is not None:
            sch.op("dve", lambda e: e.tensor_tensor(out=rd[:, 0:N], in0=rd[:, 0:N], in1=gate_ap, op=ALU.mult),
                   reads=[brd] + list(gbufs), writes=[brd], same=not wide)
        if first:
            sch.op("dve", lambda e: e.tensor_tensor(out=acc_ap, in0=ps[ni][:, 0:N], in1=rd[:, 0:N], op=ALU.mult),
                   reads=[psb[ni], brd], writes=[acc_buf], same=not wide)
        else:
            tm, btm_ = a.tmp[ri], a.b_tmp[ri]
            sch.op("dve", lambda e: e.tensor_tensor(out=tm[:, 0:N], in0=ps[ni][:, 0:N], in1=rd[:, 0:N], op=ALU.mult),
                   reads=[psb[ni], brd], writes=[btm_], same=not wide)
            sch.op("pool", lambda e: e.tensor_tensor(out=acc_ap, in0=acc_ap, in1=tm[:, 0:N], op=ALU.add),
                   reads=[btm_, acc_buf], writes=[acc_buf])

    def stage_nsa(g):
        tg = "n%d_" % g
        with ExitStack() as es:
            def sb(name, shape, dt):
                return es.enter_context(nc.sbuf_tensor(tg + name, shape, dt))
            QT = sb("QT", [128, 4, S], BF16)
            KsT = sb("KsT", [128, S], BF16)
            KwT = sb("KwT", [128, S], BF16)
            Vs = sb("Vs", [128, NT, 128], BF16)
            Vw = sb("Vw", [128, NT, 128], BF16)
            kcT = sb("kcT", [128, 256], BF16)
            vc = sb("vc", [128, 2, 128], BF16)
            MbT = sb("MbT", [NSLC, S], BF16)
            Eall = sb("Eall", [NSLC, S], BF16)
            BT = sb("BT", [128, 12, 512], BF16)
            KEEP = sb("KEEP", [128, NT, NSLC], F32)
            ADD = sb("ADD", [128, NT, NSLC], F32)
            ovl = sb("ovl", [128, 2, NSLC], BF16)
            identb = sb("identb", [128, 128], BF16)
            ones = sb("ones", [128, 128], BF16)
            zer = sb("zer", [128, 256], BF16)
            b_q, b_kv, b_c, b_kc, b_MbT = Buf(), Buf(), Buf(), Buf(), Buf()
            d_l = sch.dsem()
            ld = lambda o, i, bufs: sch.dma("sp", lambda e: e.dma_start(out=o, in_=i), d_l, writes=bufs)
            ld(QT[:], PF[4 * g * 128:(4 * g + 4) * 128, :].rearrange("(r p) s -> p r s", p=128), [b_q])
            sch._waits("sp", [bPF, bPTn], [])
            ld(KsT[:], PF[(12 + g) * 128:(13 + g) * 128, :], [b_kv])
            ld(KwT[:], PF[(14 + g) * 128:(15 + g) * 128, :], [b_kv])
            ld(Vs[:], PTn[:, g * 128:(g + 1) * 128].rearrange("(c p) d -> p c d", p=128), [b_kv])
            ld(Vw[:], PTn[:, 256 + g * 128:256 + (g + 1) * 128].rearrange("(c p) d -> p c d", p=128), [b_kv])
            ld(Eall[:], cst["Eall"], [b_c])
            ld(BT[:], cst["BT"][:, 0:12, :], [b_c])
            ld(KEEP[:], cst["KEEP"].rearrange("(c p) j -> p c j", p=128), [b_c])
            ld(ADD[:], cst["ADD"].rearrange("(c p) j -> p c j", p=128), [b_c])
            ld(ovl[:], cst["ovl"].rearrange("(c p) j -> p c j", p=128), [b_c])
            ld(identb[:], cst["identb"], [b_c])
            sch.op("dve", lambda e: e.memset(ones[:], 1.0), writes=[b_c])
            sch.op("dve", lambda e: e.memset(zer[:], 0.0), writes=[b_c])
            sch.op("dve", lambda e: e.memset(kcT[:], 0.0), writes=[b_kc])
            sch.op("dve", lambda e: e.memset(vc[:], 0.0), writes=[b_kc])
            cch = [(0, min(128, NCMP))] + ([(1, NCMP - 128)] if NCMP > 128 else [])
            with ExitStack() as es2:
                def sb2(name, shape, dt):
                    return es2.enter_context(nc.sbuf_tensor(tg + "c_" + name, shape, dt))
                src = sb2("src", [128, S], BF16)
                W1 = sb2("W1", [128, 32, 256], BF16)
                W2 = sb2("W2", [128, 2, 128], BF16)
                pT_ = sb2("posT", [128, 32], BF16)
                cvec = sb2("cvec", [128, 2], F32)
                xh = sb2("xh", [128, 256], F32)
                x2 = sb2("x2", [128, 256], F32)
                th = sb2("th", [128, 256], F32)
                gl = sb2("gl", [128, 2, 256], BF16)
                b_src, b_W, b_cv, b_xh, b_x2, b_th, b_gl = [Buf() for _ in range(7)]
                d_c2 = sch.dsem()
                for which in range(2):
                    pfc = (8 if which == 0 else 10) + g
                    sch.dma("sp", lambda e, pfc=pfc: e.dma_start(out=src[:], in_=PF[pfc * 128:(pfc + 1) * 128, :]), d_c2,
                            reads=[bPF], writes=[b_src])
                    sch.dma("pool", lambda e, which=which: e.dma_start(
                        out=W1[:], in_=cmp_w1[which].rearrange("(l d) h -> d l h", d=128)), d_c2, writes=[b_W])
                    sch.dma("pool", lambda e, which=which: e.dma_start(
                        out=W2[:], in_=cmp_w2[which].rearrange("(c p) d -> p c d", p=128)), d_c2, writes=[b_W])
                    sch.dma("pool", lambda e, which=which: e.dma_start(out=pT_[:], in_=posT[which]), d_c2, writes=[b_W])
                    for hc in range(2):
                        for l in range(32):
                            sch.op("pe", mm(ps[7][:, 0:1], W1[:, l, hc * 128:(hc + 1) * 128], pT_[:, l:l + 1], l == 0, l == 31),
                                   reads=[b_W], writes=[psb[7]])
                        sch.op("act", lambda e, hc=hc: e.activation(out=cvec[:, hc:hc + 1], in_=ps[7][:, 0:1], func=AF.Copy),
                               reads=[psb[7]], writes=[b_cv])
                        for l in range(32):
                            sch.op("pe", mm(ps[6][:, 0:NCMP], W1[:, l, hc * 128:(hc + 1) * 128],
                                            src[:, l:l + 16 * (NCMP - 1) + 1:16], l == 0, l == 31),
                                   reads=[b_W, b_src], writes=[psb[6]])
                        sch.op("act", lambda e, hc=hc: e.activation(out=xh[:, 0:NCMP], in_=ps[6][:, 0:NCMP], func=AF.Identity,
                                                                   bias=cvec[:, hc:hc + 1]),
                               reads=[psb[6], b_cv], writes=[b_xh])
                        sch.op("dve", lambda e: e.tensor_tensor(out=x2[:, 0:NCMP], in0=xh[:, 0:NCMP], in1=xh[:, 0:NCMP], op=ALU.mult),
                               reads=[b_xh], writes=[b_x2])
                        sch.op("dve", lambda e: e.tensor_scalar(out=x2[:, 0:NCMP], in0=x2[:, 0:NCMP], scalar1=0.044715, scalar2=1.0,
                                                                op0=ALU.mult, op1=ALU.add), reads=[b_x2], writes=[b_x2])
                        sch.op("dve", lambda e: e.tensor_tensor(out=x2[:, 0:NCMP], in0=x2[:, 0:NCMP], in1=xh[:, 0:NCMP], op=ALU.mult),
                               reads=[b_x2, b_xh], writes=[b_x2])
                        sch.op("act", lambda e: e.activation(out=th[:, 0:NCMP], in_=x2[:, 0:NCMP], func=AF.Tanh, scale=0.7978845608028654),
                               reads=[b_x2], writes=[b_th])
                        sch.op("dve", lambda e: e.tensor_scalar(out=th[:, 0:NCMP], in0=th[:, 0:NCMP], scalar1=1.0, scalar2=0.5,
                                                                op0=ALU.add, op1=ALU.mult), reads=[b_th], writes=[b_th])
                        sch.op("dve", lambda e, hc=hc: e.tensor_tensor(out=gl[:, hc, 0:NCMP], in0=th[:, 0:NCMP], in1=xh[:, 0:NCMP], op=ALU.mult),
                               reads=[b_th, b_xh], writes=[b_gl])
                    if which == 0:
                        for hc in range(2):
                            sch.op("pe", mm(ps[7][:, 0:NCMP], W2[:, hc, :], gl[:, hc, 0:NCMP], hc == 0, hc == 1),
                                   reads=[b_W, b_gl], writes=[psb[7]])
                        sch.op("act", lambda e: e.activation(out=kcT[:, 0:NCMP], in_=ps[7][:, 0:NCMP], func=AF.Copy),
                               reads=[psb[7]], writes=[b_kc])
                    else:
                        for cc, kn in cch:
                            for hc in range(2):
                                sch.op("pe", mm(ps[7][0:kn, 0:128], gl[:, hc, cc * 128:cc * 128 + kn], W2[:, hc, :], hc == 0, hc == 1),
                                       reads=[b_W, b_gl], writes=[psb[7]])
                            sch.op("act", lambda e, cc=cc, kn=kn: e.activation(out=vc[0:kn, cc, :], in_=ps[7][0:kn, 0:128], func=AF.Copy),
                                   reads=[psb[7]], writes=[b_kc])
                if debug:
                    dkc = dscr("dbg_kc%d" % g, [128, 256], BF16)
                    dvc = dscr("dbg_vc%d" % g, [128, 2, 128], BF16)
                    sch.dma("sp", lambda e: e.dma_start(out=dkc, in_=kcT[:]), d_c2, reads=[b_kc], writes=[Buf()])
                    sch.dma("sp", lambda e: e.dma_start(out=dvc, in_=vc[:]), d_c2, reads=[b_kc], writes=[Buf()])
                sch.barrier()
                sch.emit()
            with ExitStack() as es3:
                def sb3(name, shape, dt):
                    return es3.enter_context(nc.sbuf_tensor(tg + "a_" + name, shape, dt))
                a = make_actx(sb3, "")
                Grep = sb3("Grep", [128, 12, QN], F32)
                CMBt = sb3("CMBt", [128, 2, QN], BF16)
                pc = [sb3("pc%d" % i, [128, QN], BF16) for i in range(2)]
                pn = [sb3("pn%d" % i, [128, QN], BF16) for i in range(2)]
                oacc = [sb3("oacc%d" % i, [128, QN], F32) for i in range(4)]
                ost = [sb3("ost%d" % i, [128, QN], BF16) for i in range(2)]
                impm = sb3("impm", [128, NSLC], F32)
                impt = sb3("impt", [128, NSLC], F32)
                v8 = sb3("v8", [128, 16], F32)
                Mb = sb3("Mb", [128, NSLC], BF16)
                b_G, b_CMB = Buf(), Buf()
                b_pc, b_pn = [Buf(), Buf()], [Buf(), Buf()]
                b_oacc = [Buf() for _ in range(4)]
                b_ost = [Buf(), Buf()]
                b_impm, b_impt, b_v8, b_Mb = Buf(), Buf(), Buf(), Buf()
                d_G, d_CMB = sch.dsem(), sch.dsem()
                d_ost = [sch.dsem(), sch.dsem()]
                n_ost = 0
                for qt in range(NQT):
                    T0 = qt * QN
                    N = QN
                    sch.dma("sp", lambda e, T0=T0: e.dma_start(out=Grep[:], in_=G[g * 12:(g + 1) * 12, T0:T0 + QN].partition_broadcast(128)),
                            d_G, reads=[bG], writes=[b_G])
                    sch.dma("sp", lambda e, T0=T0: e.dma_start(out=CMBt[:], in_=cst["CMB"][:, :, T0:T0 + QN]), d_CMB, writes=[b_CMB])
                    ntc = N // 128
                    sch.op("pe", mm(ps[6][:, 0:ntc * NSLC], zer[:, 0:128], zer[:, 0:ntc * NSLC], True, False), reads=[b_c], writes=[psb[6]])
                    for r in range(4):
                        qT = QT[:, r, T0:T0 + N]
                        for cc, kn in cch:
                            si = a.k % 2
                            a.k += 1
                            sch.op("pe", mm(ps[si][0:kn, 0:N], kcT[:, cc * 128:cc * 128 + kn], qT, True, False),
                                   reads=[b_q, b_kc], writes=[psb[si]])
                            sch.op("pe", mm(ps[si][0:kn, 0:N], identb[0:kn, 0:kn], CMBt[0:kn, cc, :], False, True),
                                   reads=[b_c, b_CMB], writes=[psb[si]])
                            sch.op("act", lambda e, cc=cc, kn=kn, si=si: e.activation(out=pc[cc][0:kn, 0:N], in_=ps[si][0:kn, 0:N],
                                                                                   func=AF.Exp, scale=SCALE),
                                   reads=[psb[si]], writes=[b_pc[cc]])
                        ni = 2 + a.n % 2
                        di = 4 + a.n % 2
                        a.n += 1
                        for ci, (cc, kn) in enumerate(cch):
                            sch.op("pe", mm(ps[di][:, 0:N], ones[0:kn, :], pc[cc][0:kn, 0:N], ci == 0, ci == len(cch) - 1),
                                   reads=[b_c, b_pc[cc]], writes=[psb[di]])
                        ri = a.e % 2
                        a.e += 1
                        rd, brd = a.rd[ri], a.b_rd[ri]
                        sch.op("dve", lambda e, rd=rd, di=di: e.tensor_scalar_max(out=rd[:, 0:N], in0=ps[di][:, 0:N], scalar1=1e-30),
                               reads=[psb[di]], writes=[brd])
                        sch.op("dve", lambda e, rd=rd: e.reciprocal(out=rd[:, 0:N], in_=rd[:, 0:N]), reads=[brd], writes=[brd])
                        for cc, kn in cch:
                            sch.op("pool", lambda e, cc=cc, kn=kn, rd=rd: e.tensor_tensor(out=pn[cc][0:kn, 0:N], in0=pc[cc][0:kn, 0:N],
                                                                                       in1=rd[0:kn, 0:N], op=ALU.mult),
                                   reads=[b_pc[cc], brd], writes=[b_pn[cc]])
                        for ci, (cc, kn) in enumerate(cch):
                            sch.op("pe", mm(ps[ni][:, 0:N], vc[0:kn, cc, :], pn[cc][0:kn, 0:N], ci == 0, ci == len(cch) - 1),
                                   reads=[b_kc, b_pn[cc]], writes=[psb[ni]])
                        sch.op("dve", lambda e, r=r, ni=ni: e.tensor_tensor(out=oacc[r][:, 0:N], in0=ps[ni][:, 0:N], in1=Grep[:, r * 3, :],
                                                                          op=ALU.mult),
                               reads=[psb[ni], b_G], writes=[b_oacc[r]])
                        for tc in range(ntc):
                            for cc, kn in cch:
                                sch.op("pe", mm(ps[6][:, tc * NSLC:(tc + 1) * NSLC], pn[cc][0:kn, tc * 128:(tc + 1) * 128],
                                                ovl[0:kn, cc, :], False, False),
                                       reads=[b_pn[cc], b_c], writes=[psb[6]])
                    for tc in range(ntc):
                        chn = T0 // 128 + tc
                        sch.op("dve", lambda e, tc=tc, chn=chn: e.tensor_tensor(out=impm[:], in0=ps[6][:, tc * NSLC:(tc + 1) * NSLC],
                                                                               in1=KEEP[:, chn, :], op=ALU.mult),
                               reads=[psb[6], b_c], writes=[b_impm])
                        sch.op("dve", lambda e, chn=chn: e.tensor_tensor(out=impm[:], in0=impm[:], in1=ADD[:, chn, :], op=ALU.add),
                               reads=[b_impm, b_c], writes=[b_impm])
                        sch.op("dve", lambda e: e.max(out=v8[:, 0:8], in_=impm[:]), reads=[b_impm], writes=[b_v8])
                        sch.op("dve", lambda e: e.match_replace(out=impt[:], in_to_replace=v8[:, 0:8], in_values=impm[:], imm_value=-3.0e38),
                               reads=[b_impm, b_v8], writes=[b_impt])
                        sch.op("dve", lambda e: e.max(out=v8[:, 8:16], in_=impt[:]), reads=[b_impt], writes=[b_v8])
                        sch.op("dve", lambda e: e.tensor_scalar(out=Mb[:], in0=impm[:], scalar1=v8[:, 15:16], scalar2=NEGB,
                                                                op0=ALU.is_lt, op1=ALU.mult),
                               reads=[b_impm, b_v8], writes=[b_Mb])
                        pv7 = ps[7].bitcast(BF16)
                        sch.op("pe", lambda e, pv7=pv7: e.transpose(pv7[0:NSLC, 0:128], Mb[:, 0:NSLC], identb[:]),
                               reads=[b_Mb, b_c], writes=[psb[7]])
                        sch.op("act", lambda e, pv7=pv7, tc=tc, T0=T0: e.activation(out=MbT[:, T0 + tc * 128:T0 + (tc + 1) * 128],
                                                                           in_=pv7[0:NSLC, 0:128], func=AF.Copy),
                               reads=[psb[7]], writes=[b_MbT])
                    for r in range(4):
                        qT = QT[:, r, T0:T0 + N]
                        chunks = []
                        for sc in range((T0 + N) // 128):
                            extra = [(Eall[:, sc * 128:(sc + 1) * 128], MbT[:, T0:T0 + N], [b_c, b_MbT])]
                            if sc * 128 + 127 > T0:
                                dk = (sc * 128 - T0) // 128
                                extra.append((identb[:], BT[:, dk, 0:N], [b_c]))
                            chunks.append(dict(kT=KsT[:, sc * 128:(sc + 1) * 128], kn=128, v=Vs[:, sc, :], kvbufs=[b_kv], extra=extra))
                        ni, di = attn_tile(a, qT, [b_q], N, chunks, ones, b_c)
                        if "c" not in DBG_BR:
                            sch.op("dve", lambda e, r=r: e.memset(oacc[r][:, 0:N], 0.0), writes=[b_oacc[r]])
                        if "s" in DBG_BR:
                            attn_epilogue(a, ni, di, N, Grep[:, r * 3 + 1, :], [b_G], oacc[r][:, 0:N], b_oacc[r], False)
                        chunks = []
                        for k in range(8):
                            s0 = T0 - 512 + 128 * k
                            if s0 < 0 or s0 >= S or s0 >= T0 + N:
                                continue
                            sc = s0 // 128
                            chunks.append(dict(kT=KwT[:, sc * 128:(sc + 1) * 128], kn=128, v=Vw[:, sc, :], kvbufs=[b_kv],
                                               extra=[(identb[:], BT[:, 4 + k, 0:N], [b_c])]))
                        ni, di = attn_tile(a, qT, [b_q], N, chunks, ones, b_c)
                        if "w" in DBG_BR:
                            attn_epilogue(a, ni, di, N, Grep[:, r * 3 + 2, :], [b_G], oacc[r][:, 0:N], b_oacc[r], False)
                        oi = n_ost % 2
                        n_ost += 1
                        sch.op("act", lambda e, oi=oi, r=r: e.activation(out=ost[oi][:, 0:N], in_=oacc[r][:, 0:N], func=AF.Copy),
                               reads=[b_oacc[r]], writes=[b_ost[oi]])
                        hd = 4 * g + r
                        sch.dma("sp", lambda e, oi=oi, hd=hd, T0=T0: e.dma_start(out=ON[hd * 128:(hd + 1) * 128, T0:T0 + N], in_=ost[oi][:, 0:N]),
                                d_ost[oi], reads=[b_ost[oi]], writes=[bON])
                if debug:
                    dmb = dscr("dbg_mbt%d" % g, [NSLC, S], BF16)
                    sch.dma("sp", lambda e: e.dma_start(out=dmb, in_=MbT[:]), d_G, reads=[b_MbT], writes=[Buf()])
                sch.barrier()
                sch.emit()

    if upto >= 2:
        stage_nsa(0)
        stage_nsa(1)

    def stage_dil(hd):
        tg = "d%d_" % hd
        with ExitStack() as es:
            def sb(name, shape, dt):
                return es.enter_context(nc.sbuf_tensor(tg + name, shape, dt))
            QT = sb("QT", [128, 3, S], BF16)
            KT = sb("KT", [128, 3, S], BF16)
            kns = [min(128, S // dl) for dl in DILS]
            V = [sb("V%d" % gi, [kns[gi], S // kns[gi], 128], BF16) for gi in range(3)]
            BT = sb("BT", [128, 5, 512], BF16)
            ones = sb("ones", [128, 128], BF16)
            anum = sb("anum", [128, S], F32)
            aden = sb("aden", [128, S], F32)
            ost = [sb("ost%d" % i, [128, 512], BF16) for i in range(2)]
            rdf = [sb("rdf%d" % i, [128, 512], F32) for i in range(2)]
            a = make_actx(sb, "")
            b_q, b_kv, b_c, b_acc = Buf(), Buf(), Buf(), Buf()
            b_ost, b_rdf = [Buf(), Buf()], [Buf(), Buf()]
            d_l = sch.dsem()
            d_ost = [sch.dsem(), sch.dsem()]
            ld = lambda o, i, bufs: sch.dma("sp", lambda e: e.dma_start(out=o, in_=i), d_l, writes=bufs)
            for gi in range(3):
                cq = 16 + gi * 4 + hd
                ck = 28 + gi * 4 + hd
                ld(QT[:, gi, :], PF[cq * 128:(cq + 1) * 128, :], [b_q])
                ld(KT[:, gi, :], PF[ck * 128:(ck + 1) * 128, :], [b_kv])
                ld(V[gi][:], PTd[gi][:, hd * 128:(hd + 1) * 128].rearrange("(c p) d -> p c d", p=kns[gi]), [b_kv])
            ld(BT[:], cst["BT"][:, 12:17, :], [b_c])
            sch.op("dve", lambda e: e.memset(ones[:], 1.0), writes=[b_c])
            for gi in range(3):
                dl = DILS[gi]
                L = S // dl
                kn = kns[gi]
                N_ = min(512, L)
                for rho in range(dl):
                    for qi in range(L // N_):
                        I0 = qi * N_
                        qT = QT[:, gi, rho * L + I0:rho * L + I0 + N_]
                        chunks = []
                        s0 = max(0, I0 - 128)
                        while s0 < I0 + N_:
                            bi = (s0 - I0 + 128) // 128
                            chunks.append(dict(kT=KT[:, gi, rho * L + s0:rho * L + s0 + kn], kn=kn,
                                               v=V[gi][:, (rho * L + s0) // kn, :], kvbufs=[b_kv],
                                               extra=[(ones[0:kn, 0:kn] if False else identb_g[0:kn, 0:kn], BT[0:kn, bi, 0:N_], [b_c, b_idg])]))
                            s0 += kn
                        ni, di = attn_tile(a, qT, [b_q], N_, chunks, ones, b_c)
                        c0 = rho + dl * I0
                        c1 = rho + dl * (I0 + N_ - 1) + 1
                        if gi == 0:
                            sch.op("act", lambda e, ni=ni, c0=c0, c1=c1, dl=dl, N_=N_: e.activation(
                                out=anum[:, c0:c1:dl], in_=ps[ni][:, 0:N_], func=AF.Copy), reads=[psb[ni]], writes=[b_acc])
                            sch.op("dve", lambda e, di=di, c0=c0, c1=c1, dl=dl, N_=N_: e.tensor_copy(
                                out=aden[:, c0:c1:dl], in_=ps[di][:, 0:N_]), reads=[psb[di]], writes=[b_acc])
                        else:
                            sch.op("dve", lambda e, ni=ni, c0=c0, c1=c1, dl=dl, N_=N_: e.tensor_tensor(
                                out=anum[:, c0:c1:dl], in0=anum[:, c0:c1:dl], in1=ps[ni][:, 0:N_], op=ALU.add),
                                reads=[psb[ni], b_acc], writes=[b_acc])
                            sch.op("dve", lambda e, di=di, c0=c0, c1=c1, dl=dl, N_=N_: e.tensor_tensor(
                                out=aden[:, c0:c1:dl], in0=aden[:, c0:c1:dl], in1=ps[di][:, 0:N_], op=ALU.add),
                                reads=[psb[di], b_acc], writes=[b_acc])
            for qt in range(S // QN):
                T0 = qt * QN
                oi = qt % 2
                sch.op("dve", lambda e, oi=oi, T0=T0: e.reciprocal(out=rdf[oi][:, 0:QN], in_=aden[:, T0:T0 + QN]),
                       reads=[b_acc], writes=[b_rdf[oi]])
                sch.op("dve", lambda e, oi=oi, T0=T0: e.tensor_tensor(out=ost[oi][:, 0:QN], in0=anum[:, T0:T0 + QN], in1=rdf[oi][:, 0:QN],
                                                                      op=ALU.mult), reads=[b_acc, b_rdf[oi]], writes=[b_ost[oi]])
                sch.dma("sp", lambda e, oi=oi, T0=T0: e.dma_start(out=OD[hd * 128:(hd + 1) * 128, T0:T0 + QN], in_=ost[oi][:, 0:QN]),
                        d_ost[oi], reads=[b_ost[oi]], writes=[bOD])
            sch.barrier()
            sch.emit()

    if upto >= 3:
        identb_g = nc.alloc_sbuf_tensor("identb_g", [128, 128], BF16).ap()
        b_idg = Buf()
        d_idg = sch.dsem()
        sch.dma("sp", lambda e: e.dma_start(out=identb_g, in_=cst["identb"]), d_idg, writes=[b_idg])
        for hd in range(4):
            stage_dil(hd)


    CAP = CAPB * 128
    NROW = NE * CAP
    w_br_nsa = din("w_br_nsa", [1024, D])
    w_br_dil = din("w_br_dil", [512, D])
    w_out = din("w_out", [D, D])
    ln_gb = din("ln_gb", [4, D])
    w_router = din("w_router", [D, NE])
    b_router = din("b_router", [1, NE])
    MT = dscr("MT", [D, S], BF16)
    H1 = dscr("H1", [S, D], F32)
    NQX, NQY = (4, 8) if ep else (1, 1)
    WX, WY = D // NQX, D // NQY
    XG = [dscr("XG%d" % q, [NROW, WX], BF16) for q in range(NQX)]
    RI = dscr("RI", [S, 8], F32)
    ROWI = [dscr("ROWI%d" % k_, [S, 1], I32) for k_ in range(4)]
    bMT, bH1, bXG, bRI = Buf(), Buf(), Buf(), Buf()

    def stage_merge():
        with ExitStack() as es:
            def sb(name, shape, dt):
                return es.enter_context(nc.sbuf_tensor("m_" + name, shape, dt))
            Wa = sb("Wa", [128, 8, D], BF16)
            Wb = sb("Wb", [128, 4, D], BF16)
            ONt = [sb("ONt%d" % i, [128, 8, QN], BF16) for i in range(2)]
            ODt = [sb("ODt%d" % i, [128, 4, QN], BF16) for i in range(2)]
            ga = sb("ga", [128, 16, QN], BF16)
            gb = sb("gb", [128, 16, QN], BF16)
            mst = [sb("mst%d" % i, [128, 16, QN], BF16) for i in range(2)]
            m1 = [sb("m1%d" % i, [128, QN], F32) for i in range(2)]
            m2 = [sb("m2%d" % i, [128, QN], F32) for i in range(2)]
            b_W, b_g = Buf(), Buf()
            b_in_, b_mst, b_m1, b_m2 = [Buf(), Buf()], [Buf(), Buf()], [Buf(), Buf()], [Buf(), Buf()]
            d_W, d_g = sch.dsem(), sch.dsem()
            d_in_, d_mst = [sch.dsem(), sch.dsem()], [sch.dsem(), sch.dsem()]
            sch.dma("pool", lambda e: e.dma_start(out=Wa[:], in_=w_br_nsa.rearrange("(f p) n -> p f n", p=128)), d_W, writes=[b_W])
            sch.dma("pool", lambda e: e.dma_start(out=Wb[:], in_=w_br_dil.rearrange("(f p) n -> p f n", p=128)), d_W, writes=[b_W])
            k = 0
            for qt in range(NQT):
                T0 = qt * QN
                bi = qt % 2
                sch.dma("sp", lambda e, bi=bi, T0=T0: e.dma_start(out=ONt[bi][:], in_=ON[:, T0:T0 + QN].rearrange("(f p) t -> p f t", p=128)),
                        d_in_[bi], writes=[b_in_[bi]])
                sch.dma("sp", lambda e, bi=bi, T0=T0: e.dma_start(out=ODt[bi][:], in_=OD[:, T0:T0 + QN].rearrange("(f p) t -> p f t", p=128)),
                        d_in_[bi], writes=[b_in_[bi]])
                sch.dma("sp", lambda e, T0=T0: e.dma_start(out=ga[:], in_=PF[40 * 128:56 * 128, T0:T0 + QN].rearrange("(c p) t -> p c t", p=128)),
                        d_g, writes=[b_g])
                sch.dma("sp", lambda e, T0=T0: e.dma_start(out=gb[:], in_=PF[56 * 128:72 * 128, T0:T0 + QN].rearrange("(c p) t -> p c t", p=128)),
                        d_g, writes=[b_g])
                for n in range(16):
                    pa = (2 * k) % 8
                    pb = (2 * k + 1) % 8
                    ti = k % 2
                    k += 1
                    for f in range(8):
                        sch.op("pe", mm(ps[pa][:, 0:QN], Wa[:, f, n * 128:(n + 1) * 128], ONt[bi][:, f, :], f == 0, f == 7),
                               reads=[b_W, b_in_[bi]], writes=[psb[pa]])
                    for f in range(4):
                        sch.op("pe", mm(ps[pb][:, 0:QN], Wb[:, f, n * 128:(n + 1) * 128], ODt[bi][:, f, :], f == 0, f == 3),
                               reads=[b_W, b_in_[bi]], writes=[psb[pb]])
                    sch.op("dve", lambda e, pa=pa, ti=ti, n=n: e.tensor_tensor(out=m1[ti][:], in0=ps[pa][:, 0:QN], in1=ga[:, n, :], op=ALU.mult),
                           reads=[psb[pa], b_g], writes=[b_m1[ti]])
                    sch.op("dve", lambda e, pb=pb, ti=ti, n=n: e.tensor_tensor(out=m2[ti][:], in0=ps[pb][:, 0:QN], in1=gb[:, n, :], op=ALU.mult),
                           reads=[psb[pb], b_g], writes=[b_m2[ti]])
                    sch.op("pool", lambda e, ti=ti, n=n, bi=bi: e.tensor_tensor(out=mst[bi][:, n, :], in0=m1[ti][:], in1=m2[ti][:], op=ALU.add),
                           reads=[b_m1[ti], b_m2[ti]], writes=[b_mst[bi]])
                sch.dma("sp", lambda e, bi=bi, T0=T0: e.dma_start(out=MT[:, T0:T0 + QN].rearrange("(c p) t -> p c t", p=128), in_=mst[bi][:]),
                        d_mst[bi], reads=[b_mst[bi]], writes=[bMT])
            sch.barrier()
            sch.emit()

    def layer_norm_tile(sbt, hp, b_hp, outt, b_out, gbc, bbc, b_gb, sq, b_sq, st, b_st):
        sch.op("dve", lambda e: e.tensor_reduce(out=st[:, 0:1], in_=hp[:], axis=AX.X, op=ALU.add), reads=[b_hp], writes=[b_st])
        sch.op("dve", lambda e: e.tensor_scalar(out=st[:, 1:2], in0=st[:, 0:1], scalar1=-1.0 / D, scalar2=None, op0=ALU.mult),
               reads=[b_st], writes=[b_st])
        sch.op("act", lambda e: e.activation(out=hp[:], in_=hp[:], func=AF.Identity, bias=st[:, 1:2]), reads=[b_hp, b_st], writes=[b_hp])
        sch.op("act", lambda e: e.activation(out=sq[:], in_=hp[:], func=AF.Square, accum_out=st[:, 2:3]), reads=[b_hp], writes=[b_sq, b_st], same=False)
        sch.op("dve", lambda e: e.tensor_scalar(out=st[:, 3:4], in0=st[:, 2:3], scalar1=1.0 / D, scalar2=LN_EPS, op0=ALU.mult, op1=ALU.add),
               reads=[b_st], writes=[b_st])
        sch.op("act", lambda e: e.activation(out=st[:, 4:5], in_=st[:, 3:4], func=AF.Sqrt), reads=[b_st], writes=[b_st])
        sch.op("dve", lambda e: e.reciprocal(out=st[:, 5:6], in_=st[:, 4:5]), reads=[b_st], writes=[b_st])
        sch.op("dve", lambda e: e.scalar_tensor_tensor(out=outt[:], in0=hp[:], scalar=st[:, 5:6], in1=gbc[:], op0=ALU.mult, op1=ALU.mult),
               reads=[b_hp, b_st, b_gb], writes=[b_out])
        sch.op("pool", lambda e: e.tensor_tensor(out=outt[:], in0=outt[:], in1=bbc[:], op=ALU.add), reads=[b_out, b_gb], writes=[b_out])

    ebase_y_in = din("ebase_y", [128, NE]) if ep else None

    def stage_out():
        with ExitStack() as es:
            def sb(name, shape, dt):
                return es.enter_context(nc.sbuf_tensor("o_" + name, shape, dt))
            Wo = sb("Wo", [128, KC, D], BF16)
            mTt = sb("mTt", [128, KC, QN], BF16)
            xc = [sb("xc%d" % i, [128, D], F32) for i in range(2)]
            hp = sb("hp", [128, D], F32)
            sq = sb("sq", [128, D], BF16)
            h1 = [sb("h1%d" % i, [128, D], F32) for i in range(2)]
            h1b = [sb("h1b%d" % i, [128, D], BF16) for i in range(2)]
            h1T = sb("h1T", [128, KC, 128], F32)
            gbc = sb("gbc", [128, D], F32)
            bbc = sb("bbc", [128, D], F32)
            Wr = sb("Wr", [128, KC, NE], F32)
            brb = sb("brb", [128, NE], F32)
            identf = sb("identf", [128, 128], F32)
            ones = sb("ones", [128, 128], BF16)
            ustr = sb("ustr", [128, 128], BF16)
            ebase = sb("ebase", [128, NE], F32)
            base = sb("base", [128, NE], F32)
            zt = sb("zt", [128, 4096], BF16)
            st = sb("st", [128, 8], F32)
            rt = {n_: sb(n_, [128, NE], F32) for n_ in ("lg", "m4", "ex", "gd", "slot", "okm", "rowf", "oh", "t1", "t2")}
            mb16 = sb("mb16", [128, NE], BF16)
            oh4 = sb("oh4", [128, 4, NE], F32)
            t14 = sb("t14", [128, 4, NE], F32)
            v8 = sb("v8", [128, 8], F32)
            sm = sb("sm", [128, 8], F32)
            ri = [sb("ri%d" % i, [128, 8], F32) for i in range(2)]
            idxf = sb("idxf", [128, 4], F32)
            idxi = [[sb("idxi%d_%d" % (i, k_), [128, 1], I32) for k_ in range(4)] for i in range(2)]
            rowi = [sb("rowi%d" % i, [128, 4], I32) for i in range(2)]
            b_W, b_mT, b_c, b_hp, b_sq, b_st, b_h1T, b_r, b_base = [Buf() for _ in range(9)]
            b_xc, b_h1, b_h1b, b_ri, b_idx, b_rowi = [[Buf(), Buf()] for _ in range(6)]
            d_W, d_mT, d_c = sch.dsem(), sch.dsem(), sch.dsem()
            d_xc, d_h1, d_sc, d_ri = [[sch.dsem(), sch.dsem()] for _ in range(4)]
            d_z = sch.dsem()
            sch.dma("pool", lambda e: e.dma_start(out=Wo[:], in_=w_out.rearrange("(k p) n -> p k n", p=128)), d_W, writes=[b_W])
            cl = lambda o, i: sch.dma("sp", lambda e: e.dma_start(out=o, in_=i), d_c, writes=[b_c])
            cl(gbc[:], ln_gb[0:1, :].partition_broadcast(128))
            cl(bbc[:], ln_gb[1:2, :].partition_broadcast(128))
            cl(Wr[:], w_router.rearrange("(k p) n -> p k n", p=128))
            cl(brb[:], b_router.partition_broadcast(128))
            cl(identf[:], cst["identf"])
            cl(ustr[:], cst["ustr"])
            cl(ebase[:], cst["ebase"])
            if ep:
                ebasey = sb("ebasey", [128, NE], F32)
                rowfy = sb("rowfy", [128, NE], F32)
                riy = sb("riy", [128, 4], F32)
                cl(ebasey[:], ebase_y_in)
            sch.op("dve", lambda e: e.memset(ones[:], 1.0), writes=[b_c])
            sch.op("dve", lambda e: e.memset(base[:], 0.0), writes=[b_base])
            sch.op("dve", lambda e: e.memset(zt[:], 0.0), writes=[b_c])
            nbz = 4096 // WX
            for q in range(NQX):
                for bz in range(NROW // (128 * nbz)):
                    dst = XG[q][bz * 128 * nbz:(bz + 1) * 128 * nbz, :].rearrange("(b p) f -> p b f", p=128)
                    sch.dma("sp", lambda e, dst=dst: e.dma_start(out=dst, in_=zt[:].rearrange("p (b f) -> p b f", b=nbz)),
                            d_z, reads=[b_c], writes=[bXG])
            npp = 0
            bc_reg = {}
            sch.prog["pool"].append(lambda e: bc_reg.__setitem__("r", e.to_reg(NROW - 1)))
            for c in range(NT):
                tok0 = c * 128
                cb = c % 2
                if c % (QN // 128) == 0:
                    sch.dma("sp", lambda e, tok0=tok0: e.dma_start(out=mTt[:], in_=MT[:, tok0:tok0 + QN].rearrange("(k p) t -> p k t", p=128)),
                            d_mT, reads=[bMT], writes=[b_mT])
                cl_ = c % (QN // 128)
                sch.dma("sp", lambda e, cb=cb, tok0=tok0: e.dma_start(out=xc[cb][:], in_=x[tok0:tok0 + 128, :]), d_xc[cb], writes=[b_xc[cb]])
                for nt in range(4):
                    pi = npp % 4
                    npp += 1
                    for k_ in range(KC):
                        sch.op("pe", mm(ps[pi][:, :], mTt[:, k_, cl_ * 128:(cl_ + 1) * 128], Wo[:, k_, nt * 512:(nt + 1) * 512], k_ == 0, k_ == KC - 1),
                               reads=[b_W, b_mT], writes=[psb[pi]])
                    sch.op("dve", lambda e, pi=pi, cb=cb, nt=nt: e.scalar_tensor_tensor(
                        out=hp[:, nt * 512:(nt + 1) * 512], in0=xc[cb][:, nt * 512:(nt + 1) * 512], scalar=DN_ALPHA, in1=ps[pi][:, :],
                        op0=ALU.mult, op1=ALU.add), reads=[psb[pi], b_xc[cb]], writes=[b_hp])
                layer_norm_tile(sb, hp, b_hp, h1[cb], b_h1[cb], gbc, bbc, b_c, sq, b_sq, st, b_st)
                sch.dma("sp", lambda e, cb=cb, tok0=tok0: e.dma_start(out=H1[tok0:tok0 + 128, :], in_=h1[cb][:]), d_h1[cb],
                        reads=[b_h1[cb]], writes=[bH1])
                sch.op("act", lambda e, cb=cb: e.activation(out=h1b[cb][:], in_=h1[cb][:], func=AF.Copy), reads=[b_h1[cb]], writes=[b_h1b[cb]])
                for q4 in range(4):
                    pi = 4 + q4 % 2
                    for j in range(4):
                        k_ = q4 * 4 + j
                        sch.op("pe", lambda e, pi=pi, j=j, k_=k_, cb=cb: e.transpose(ps[pi][:, j * 128:(j + 1) * 128],
                                                                                  h1[cb][:, k_ * 128:(k_ + 1) * 128], identf[:]),
                               reads=[b_h1[cb], b_c], writes=[psb[pi]])
                    sch.op("act", lambda e, pi=pi, q4=q4: e.activation(out=h1T[:, q4 * 4:(q4 + 1) * 4, :],
                                                                     in_=ps[pi][:, :].rearrange("p (a b) -> p a b", a=4), func=AF.Copy),
                           reads=[psb[pi]], writes=[b_h1T])
                for k_ in range(KC):
                    sch.op("pe", mm(ps[6][:, 0:NE], h1T[:, k_, :], Wr[:, k_, :], k_ == 0, k_ == KC - 1), reads=[b_h1T, b_c], writes=[psb[6]])
                R_ = rt
                dv = lambda fn, rd_=(), wr_=(): sch.op("dve", fn, reads=[b_r, b_c] + list(rd_), writes=[b_r] + list(wr_))
                dv(lambda e: e.tensor_tensor(out=R_["lg"][:], in0=ps[6][:, 0:NE], in1=brb[:], op=ALU.add), rd_=[psb[6]])
                dv(lambda e: e.max(out=v8[:], in_=R_["lg"][:]))
                dv(lambda e: e.tensor_scalar(out=R_["m4"][:], in0=R_["lg"][:], scalar1=v8[:, 3:4], scalar2=None, op0=ALU.is_ge))
                dv(lambda e: e.tensor_scalar(out=sm[:, 0:1], in0=v8[:, 0:1], scalar1=-1.0, scalar2=None, op0=ALU.mult))
                sch.op("act", lambda e: e.activation(out=R_["ex"][:], in_=R_["lg"][:], func=AF.Exp, bias=sm[:, 0:1]), reads=[b_r], writes=[b_r])
                dv(lambda e: e.tensor_tensor(out=R_["ex"][:], in0=R_["ex"][:], in1=R_["m4"][:], op=ALU.mult))
                dv(lambda e: e.tensor_reduce(out=sm[:, 1:2], in_=R_["ex"][:], axis=AX.X, op=ALU.add))
                dv(lambda e: e.reciprocal(out=sm[:, 2:3], in_=sm[:, 1:2]))
                dv(lambda e: e.tensor_scalar(out=R_["gd"][:], in0=R_["ex"][:], scalar1=sm[:, 2:3], scalar2=None, op0=ALU.mult))
                dv(lambda e: e.tensor_copy(out=mb16[:], in_=R_["m4"][:]))
                sch.op("pe", mm(ps[7][:, 0:NE], ustr[:], mb16[:], True, True), reads=[b_r, b_c], writes=[psb[7]])
                sch.op("pe", mm(ps[7][:, NE:2 * NE], ones[:], mb16[:], True, True), reads=[b_r, b_c], writes=[psb[7]])
                dv(lambda e: e.tensor_tensor(out=R_["slot"][:], in0=ps[7][:, 0:NE], in1=base[:], op=ALU.add), rd_=[psb[7], b_base])
                dv(lambda e: e.tensor_tensor(out=base[:], in0=ps[7][:, NE:2 * NE], in1=base[:], op=ALU.add), rd_=[psb[7], b_base], wr_=[b_base])
                dv(lambda e: e.tensor_scalar(out=R_["okm"][:], in0=R_["slot"][:], scalar1=float(CAP), scalar2=None, op0=ALU.is_lt))
                dv(lambda e: e.tensor_tensor(out=R_["okm"][:], in0=R_["okm"][:], in1=R_["m4"][:], op=ALU.mult))
                dv(lambda e: e.tensor_tensor(out=R_["rowf"][:], in0=R_["slot"][:], in1=ebase[:], op=ALU.add))
                if ep:
                    dv(lambda e: e.tensor_tensor(out=rowfy[:], in0=R_["slot"][:], in1=ebasey[:], op=ALU.add))
                bc_k = lambda ap: ap.unsqueeze(1).to_broadcast([128, 4, NE])
                dv(lambda e: e.tensor_tensor(out=oh4[:], in0=bc_k(R_["lg"][:]), in1=v8[:, 0:4].unsqueeze(2).to_broadcast([128, 4, NE]),
                                             op=ALU.is_equal))
                dv(lambda e: e.tensor_tensor(out=oh4[:], in0=oh4[:], in1=bc_k(R_["okm"][:]), op=ALU.mult))
                dv(lambda e: e.tensor_reduce(out=sm[:, 4:8], in_=oh4[:], axis=AX.X, op=ALU.add))
                dv(lambda e: e.tensor_tensor(out=t14[:], in0=oh4[:], in1=bc_k(R_["rowf"][:]), op=ALU.mult))
                dv(lambda e, cb=cb: e.tensor_reduce(out=ri[cb][:, 0:4], in_=t14[:], axis=AX.X, op=ALU.add), rd_=[b_ri[cb]], wr_=[b_ri[cb]])
                if ep:
                    dv(lambda e: e.tensor_tensor(out=t14[:], in0=oh4[:], in1=bc_k(rowfy[:]), op=ALU.mult))
                    dv(lambda e: e.tensor_reduce(out=riy[:, 0:4], in_=t14[:], axis=AX.X, op=ALU.add))
                dv(lambda e: e.tensor_tensor(out=t14[:], in0=oh4[:], in1=bc_k(R_["gd"][:]), op=ALU.mult))
                dv(lambda e, cb=cb: e.tensor_reduce(out=ri[cb][:, 4:8], in_=t14[:], axis=AX.X, op=ALU.add), rd_=[b_ri[cb]], wr_=[b_ri[cb]])
                dv(lambda e: e.tensor_scalar(out=idxf[:], in0=sm[:, 4:8], scalar1=-1.0e6, scalar2=1.0e6, op0=ALU.mult, op1=ALU.add))
                dv(lambda e, cb=cb: e.tensor_tensor(out=idxf[:], in0=idxf[:], in1=ri[cb][:, 0:4], op=ALU.add), rd_=[b_ri[cb]])
                for k_ in range(4):
                    dv(lambda e, k_=k_, cb=cb: e.tensor_copy(out=idxi[cb][k_][:], in_=idxf[:, k_:k_ + 1]), rd_=[b_idx[cb]], wr_=[b_idx[cb]])
                if ep:
                    dv(lambda e, cb=cb: e.tensor_copy(out=rowi[cb][:], in_=riy[:, 0:4]), rd_=[b_ri[cb], b_rowi[cb]], wr_=[b_rowi[cb]])
                else:
                    dv(lambda e, cb=cb: e.tensor_copy(out=rowi[cb][:], in_=ri[cb][:, 0:4]), rd_=[b_ri[cb], b_rowi[cb]], wr_=[b_rowi[cb]])
                for k_ in range(4):
                    for q in range(NQX):
                        sch.dma("pool", lambda e, k_=k_, cb=cb, q=q: e.indirect_dma_start(
                            out=XG[q][:, :], out_offset=bass.IndirectOffsetOnAxis(ap=idxi[cb][k_][:, 0:1], axis=0),
                            in_=h1b[cb][:, q * WX:(q + 1) * WX], in_offset=None, bounds_check=bc_reg["r"], oob_is_err=False),
                            d_sc[cb], reads=[b_h1b[cb], b_idx[cb], bXG], writes=[Buf()])
                sch.dma("sp", lambda e, cb=cb, tok0=tok0: e.dma_start(out=RI[tok0:tok0 + 128, :], in_=ri[cb][:]), d_ri[cb],
                        reads=[b_ri[cb]], writes=[bRI])
                for k_ in range(4):
                    sch.dma("sp", lambda e, cb=cb, tok0=tok0, k_=k_: e.dma_start(out=ROWI[k_][tok0:tok0 + 128, :], in_=rowi[cb][:, k_:k_ + 1]), d_ri[cb],
                            reads=[b_rowi[cb]], writes=[bRI])
            sch.barrier()
            sch.emit()

    if upto >= 4:
        stage_merge()
    if upto >= 5:
        stage_out()


    NEL = NE // 8 if ep else NE
    w_up = din("w_up", [NEL, D, 2 * D])
    w_down = din("w_down", [NEL, D, D])
    b_up_fm = din("b_up_fm", [128, NEL * 32])
    b_down = din("b_down", [NEL, D])
    Y = [dscr("Y%d" % q, [NROW, WY], F32) for q in range(NQY)]
    bY = Buf()
    if ep:
        XGr = [nc.dram_tensor("XGall%d" % q, [8 * NROW, WX], BF16, kind="Internal").ap() for q in range(NQX)]
        Yr = [nc.dram_tensor("Yall%d" % q, [8 * NROW, WY], F32, kind="Internal").ap() for q in range(NQY)]
        bXGr, bYr = Buf(), Buf()
        d_cc = sch.dsem()
        xg_idx = din("xg_idx", [128, NE * CAPB], I32)
    else:
        XGr, Yr, bXGr, bYr = XG, Y, bXG, bY

    def all_gather(srcs, dsts, bsrc, bdst):
        for src, dst in zip(srcs, dsts):
            sch.dma("pool", lambda e, src=src, dst=dst: e.collective_compute(
                "AllGather", ALU.bypass, replica_groups=[list(range(8))], ins=[src[:, :]], outs=[dst[:, :]]),
                d_cc, reads=[bsrc], writes=[bdst])
        sch.barrier()
        sch.emit()

    def stage_experts():
        with ExitStack() as es:
            def sb(name, shape, dt):
                return es.enter_context(nc.sbuf_tensor("e_" + name, shape, dt))
            xg = [sb("xg%d" % i, [128, D], BF16) for i in range(2)]
            xgT = sb("xgT", [128, KC, CAP], BF16)
            actT = sb("actT", [128, KC, CAP], BF16)
            NUPB, NDNB = 4, 2
            Wu = [sb("Wu%d" % i, [128, KC, 2, 256], BF16) for i in range(NUPB)]
            Wd = [sb("Wd%d" % i, [128, KC, 512], BF16) for i in range(NDNB)]
            bup = sb("bup", [128, 2, 32], F32)
            b_bup = [Buf(), Buf()]
            d_bup = [sch.dsem(), sch.dsem()]
            bdn = [sb("bdn%d" % i, [128, D], F32) for i in range(2)]
            identb = sb("identb", [128, 128], BF16)
            HN = CAP // 2
            tg_ = [sb("tg%d" % i, [128, HN], F32) for i in range(3)]
            ts_ = [sb("ts%d" % i, [128, HN], F32) for i in range(3)]
            tl_ = [sb("tl%d" % i, [128, HN], F32) for i in range(3)]
            yst = [sb("yst%d" % i, [128, 512], F32) for i in range(3)]
            b_xg, b_Wu, b_Wd = [Buf(), Buf()], [Buf() for _ in range(NUPB)], [Buf() for _ in range(NDNB)]
            b_xgT, b_actT, b_c = Buf(), Buf(), Buf()
            b_bdn = [Buf(), Buf()]
            b_tg, b_ts, b_tl = [Buf(), Buf(), Buf()], [Buf(), Buf(), Buf()], [Buf(), Buf(), Buf()]
            b_yst = [Buf() for _ in range(3)]
            d_xg = [sch.dsem(), sch.dsem()]
            d_Wu = [sch.dsem() for _ in range(NUPB)]
            d_Wd = [sch.dsem() for _ in range(NDNB)]
            d_bdn = [sch.dsem(), sch.dsem()]
            d_yst = [sch.dsem() for _ in range(3)]
            d_c = sch.dsem()
            sch.dma("sp", lambda e: e.dma_start(out=identb[:], in_=cst["identb"]), d_c, writes=[b_c])
            if ep:
                xgi = sb("xgi", [128, NE * CAPB], I32)
                sch.dma("sp", lambda e: e.dma_start(out=xgi[:], in_=xg_idx), d_c, writes=[b_c])
            upieces, dpieces = [], []
            for ex in range(NE):
                wex_ = ex // 8 if ep else ex
                for pc in range(8):
                    upieces.append((wex_, pc))
                for nt in range(4):
                    dpieces.append((wex_, nt))
            PDU, PDD = NUPB, NDNB

            def issue_u(i):
                if i >= len(upieces):
                    return
                wex_, pc = upieces[i]
                bi = i % NUPB
                for hl in range(2):
                    src = w_up[wex_, :, hl * D + pc * 256:hl * D + (pc + 1) * 256].rearrange("(k p) n -> p k n", p=128)
                    sch.dma("pool", lambda e, bi=bi, hl=hl, src=src: e.dma_start(out=Wu[bi][:, :, hl, :], in_=src), d_Wu[bi], writes=[b_Wu[bi]])

            def issue_d(i):
                if i >= len(dpieces):
                    return
                wex_, nt = dpieces[i]
                bi = i % NDNB
                src = w_down[wex_, :, nt * 512:(nt + 1) * 512].rearrange("(k p) n -> p k n", p=128)
                sch.dma("pool", lambda e, bi=bi, src=src: e.dma_start(out=Wd[bi][:], in_=src), d_Wd[bi], writes=[b_Wd[bi]])
            for i in range(PDU):
                issue_u(i)
            for i in range(PDD):
                issue_d(i)
            uidx = 0
            didx = 0
            nps = 0
            nxg = 0
            nys = 0
            nt_ = 0
            for ex in range(NE):
                eb = ex % 2
                if ep:
                    wex = ex // 8
                    rbase = (ex % 8) * (NE // 8) * CAP + wex * CAP
                else:
                    wex = ex
                    rbase = ex * CAP
                sch.dma("sp", lambda e, eb=eb, wex=wex: e.dma_start(out=bdn[eb][:], in_=b_down[wex:wex + 1, :].partition_broadcast(128)),
                        d_bdn[eb], writes=[b_bdn[eb]])
                sch.dma("sp", lambda e, eb=eb, wex=wex: e.dma_start(out=bup[:, eb, :], in_=b_up_fm[:, wex * 32:(wex + 1) * 32]),
                        d_bup[eb], writes=[b_bup[eb]])
                for blk in range(CAPB):
                    xi = nxg % 2
                    nxg += 1
                    r0 = rbase + blk * 128
                    for q in range(NQX):
                        if ep:
                            jcol = ex * CAPB + blk
                            sch.dma("pool", lambda e, xi=xi, jcol=jcol, q=q: e.indirect_dma_start(
                                out=xg[xi][:, q * WX:(q + 1) * WX], out_offset=None, in_=XGr[q][:, :],
                                in_offset=bass.IndirectOffsetOnAxis(ap=xgi[:, jcol:jcol + 1], axis=0)),
                                d_xg[xi], reads=[bXGr, b_c], writes=[b_xg[xi]])
                        else:
                            sch.dma("sp", lambda e, xi=xi, r0=r0, q=q: e.dma_start(out=xg[xi][:, q * WX:(q + 1) * WX], in_=XGr[q][r0:r0 + 128, :]),
                                    d_xg[xi], reads=[bXGr], writes=[b_xg[xi]])
                    for q4 in range(4):
                        pi = 6 + nps % 2
                        nps += 1
                        pv = ps[pi].bitcast(BF16)
                        for j in range(4):
                            k_ = q4 * 4 + j
                            sch.op("pe", lambda e, pv=pv, j=j, k_=k_, xi=xi: e.transpose(pv[:, j * 128:(j + 1) * 128],
                                                                                      xg[xi][:, k_ * 128:(k_ + 1) * 128], identb[:]),
                                   reads=[b_xg[xi], b_c], writes=[psb[pi]])
                        sch.op("act", lambda e, pv=pv, q4=q4, blk=blk: e.activation(
                            out=xgT[:, q4 * 4:(q4 + 1) * 4, blk * 128:(blk + 1) * 128],
                            in_=pv[:, 0:512].rearrange("p (a b) -> p a b", a=4), func=AF.Copy), reads=[psb[pi]], writes=[b_xgT])
                for pc in range(8):
                    bi = uidx % NUPB
                    for jj in range(2):
                        j = pc * 2 + jj
                        for hf in range(2):
                            pg = (2 * nt_) % 6
                            pl = (2 * nt_ + 1) % 6
                            ti = nt_ % 3
                            nt_ += 1
                            for k_ in range(KC):
                                sch.op("pe", mm(ps[pg][:, 0:HN], Wu[bi][:, k_, 0, jj * 128:(jj + 1) * 128], xgT[:, k_, hf * HN:(hf + 1) * HN],
                                                k_ == 0, k_ == KC - 1), reads=[b_Wu[bi], b_xgT], writes=[psb[pg]])
                            for k_ in range(KC):
                                sch.op("pe", mm(ps[pl][:, 0:HN], Wu[bi][:, k_, 1, jj * 128:(jj + 1) * 128], xgT[:, k_, hf * HN:(hf + 1) * HN],
                                                k_ == 0, k_ == KC - 1), reads=[b_Wu[bi], b_xgT], writes=[psb[pl]])
                            bg = bup[:, eb, j:j + 1]
                            bl = bup[:, eb, 16 + j:17 + j]
                            sch.op("dve", lambda e, ti=ti, pg=pg, bg=bg: e.tensor_scalar(out=tg_[ti][:], in0=ps[pg][:, 0:HN], scalar1=bg, scalar2=7.0,
                                                                                     op0=ALU.add, op1=ALU.min),
                                   reads=[psb[pg], b_bup[eb]], writes=[b_tg[ti]], same=False)
                            sch.op("act", lambda e, ti=ti: e.activation(out=ts_[ti][:], in_=tg_[ti][:], func=AF.Sigmoid, scale=1.702),
                                   reads=[b_tg[ti]], writes=[b_ts[ti]])
                            sch.op("dve", lambda e, ti=ti, pl=pl, bl=bl: e.tensor_scalar(out=tl_[ti][:], in0=ps[pl][:, 0:HN], scalar1=bl, scalar2=7.0,
                                                                                     op0=ALU.add, op1=ALU.min),
                                   reads=[psb[pl], b_bup[eb]], writes=[b_tl[ti]], same=False)
                            sch.op("dve", lambda e, ti=ti: e.tensor_scalar(out=tl_[ti][:], in0=tl_[ti][:], scalar1=-7.0, scalar2=1.0,
                                                                          op0=ALU.max, op1=ALU.add), reads=[b_tl[ti]], writes=[b_tl[ti]], same=False)
                            sch.op("dve", lambda e, ti=ti: e.tensor_tensor(out=tg_[ti][:], in0=tg_[ti][:], in1=ts_[ti][:], op=ALU.mult),
                                   reads=[b_tg[ti], b_ts[ti]], writes=[b_tg[ti]], same=False)
                            sch.op("dve", lambda e, ti=ti, j=j, hf=hf: e.tensor_tensor(out=actT[:, j, hf * HN:(hf + 1) * HN], in0=tg_[ti][:], in1=tl_[ti][:],
                                                                                      op=ALU.mult),
                                   reads=[b_tg[ti], b_tl[ti]], writes=[b_actT], same=False)
                    issue_u(uidx + PDU)
                    uidx += 1
                for nt in range(4):
                    bi = didx % NDNB
                    for blk in range(CAPB):
                        pi = 6 + nps % 2
                        nps += 1
                        for k_ in range(KC):
                            sch.op("pe", mm(ps[pi][:, :], actT[:, k_, blk * 128:(blk + 1) * 128], Wd[bi][:, k_, :], k_ == 0, k_ == KC - 1),
                                   reads=[b_Wd[bi], b_actT], writes=[psb[pi]])
                        yi = nys % 3
                        nys += 1
                        sch.op("dve", lambda e, yi=yi, pi=pi, eb=eb, nt=nt: e.tensor_tensor(out=yst[yi][:], in0=ps[pi][:, :],
                                                                                       in1=bdn[eb][:, nt * 512:(nt + 1) * 512], op=ALU.add),
                               reads=[psb[pi], b_bdn[eb]], writes=[b_yst[yi]])
                        r0 = rbase + blk * 128
                        wst = min(WY, 512)
                        for hh in range(512 // wst):
                            c0 = nt * 512 + hh * wst
                            qy, cq = c0 // WY, c0 % WY
                            sch.dma("sp", lambda e, yi=yi, r0=r0, qy=qy, cq=cq, hh=hh, wst=wst: e.dma_start(
                                out=Y[qy][r0:r0 + 128, cq:cq + wst], in_=yst[yi][:, hh * wst:(hh + 1) * wst]),
                                d_yst[yi], reads=[b_yst[yi]], writes=[bY])
                    issue_d(didx + PDD)
                    didx += 1
            if debug:
                dx = dscr("dbg_xgT", [128, KC, CAP], BF16)
                da = dscr("dbg_actT", [128, KC, CAP], BF16)
                sch.dma("sp", lambda e: e.dma_start(out=dx, in_=xgT[:]), d_c, reads=[b_xgT], writes=[Buf()])
                sch.dma("sp", lambda e: e.dma_start(out=da, in_=actT[:]), d_c, reads=[b_actT], writes=[Buf()])
            sch.barrier()
            sch.emit()

    def stage_combine():
        with ExitStack() as es:
            def sb(name, shape, dt):
                return es.enter_context(nc.sbuf_tensor("c_" + name, shape, dt))
            yk = [[sb("yk%d_%d" % (i, k_), [128, D], F32) for k_ in range(4)] for i in range(2)]
            h1 = [sb("h1%d" % i, [128, D], F32) for i in range(2)]
            hp = sb("hp", [128, D], F32)
            sq = sb("sq", [128, D], BF16)
            ot = [sb("ot%d" % i, [128, D], F32) for i in range(2)]
            gbc = sb("gbc", [128, D], F32)
            bbc = sb("bbc", [128, D], F32)
            st = sb("st", [128, 8], F32)
            ri = [sb("ri%d" % i, [128, 8], F32) for i in range(2)]
            rw = [[sb("rw%d_%d" % (i, k_), [128, 1], I32) for k_ in range(4)] for i in range(2)]
            b_yk, b_h1, b_ot, b_ri, b_rw = [[Buf(), Buf()] for _ in range(5)]
            b_c, b_hp, b_sq, b_st = Buf(), Buf(), Buf(), Buf()
            d_c = sch.dsem()
            d_yk, d_h1, d_ot, d_ri, d_rw = [[sch.dsem(), sch.dsem()] for _ in range(5)]
            bout = Buf()
            sch.dma("sp", lambda e: e.dma_start(out=gbc[:], in_=ln_gb[2:3, :].partition_broadcast(128)), d_c, writes=[b_c])
            sch.dma("sp", lambda e: e.dma_start(out=bbc[:], in_=ln_gb[3:4, :].partition_broadcast(128)), d_c, writes=[b_c])
            for c in range(NT):
                tok0 = c * 128
                cb = c % 2
                sch.dma("sp", lambda e, cb=cb, tok0=tok0: e.dma_start(out=ri[cb][:], in_=RI[tok0:tok0 + 128, :]), d_ri[cb],
                        reads=[bRI], writes=[b_ri[cb]])
                for k_ in range(4):
                    sch.dma("sp", lambda e, cb=cb, tok0=tok0, k_=k_: e.dma_start(out=rw[cb][k_][:], in_=ROWI[k_][tok0:tok0 + 128, :]), d_rw[cb],
                            reads=[bRI], writes=[b_rw[cb]])
                sch.dma("sp", lambda e, cb=cb, tok0=tok0: e.dma_start(out=h1[cb][:], in_=H1[tok0:tok0 + 128, :]), d_h1[cb],
                        reads=[bH1], writes=[b_h1[cb]])
                for k_ in range(4):
                    for q in range(NQY):
                        sch.dma("pool", lambda e, cb=cb, k_=k_, q=q: e.indirect_dma_start(
                            out=yk[cb][k_][:, q * WY:(q + 1) * WY], out_offset=None, in_=Yr[q][:, :],
                            in_offset=bass.IndirectOffsetOnAxis(ap=rw[cb][k_][:, 0:1], axis=0)),
                            d_yk[cb], reads=[bYr, b_rw[cb]], writes=[b_yk[cb]])
                sch.op("act", lambda e, cb=cb: e.activation(out=hp[:], in_=h1[cb][:], func=AF.Copy, scale=DN_ALPHA), reads=[b_h1[cb]], writes=[b_hp])
                for k_ in range(4):
                    sch.op("dve", lambda e, cb=cb, k_=k_: e.scalar_tensor_tensor(out=hp[:], in0=yk[cb][k_][:], scalar=ri[cb][:, 4 + k_:5 + k_],
                                                                              in1=hp[:], op0=ALU.mult, op1=ALU.add),
                           reads=[b_yk[cb], b_ri[cb], b_hp], writes=[b_hp], same=False)
                layer_norm_tile(sb, hp, b_hp, ot[cb], b_ot[cb], gbc, bbc, b_c, sq, b_sq, st, b_st)
                sch.dma("sp", lambda e, cb=cb, tok0=tok0: e.dma_start(out=out[tok0:tok0 + 128, :], in_=ot[cb][:]), d_ot[cb],
                        reads=[b_ot[cb]], writes=[bout])
            sch.barrier()
            sch.emit()

    if upto >= 6:
        if ep:
            all_gather(XG, XGr, bXG, bXGr)
        stage_experts()
    if upto >= 7:
        if ep:
            all_gather(Y, Yr, bY, bYr)
        stage_combine()

    sch.barrier()
    sch.emit()
    return nc


def prep_inputs(inp, b, S, consts=None, ep=False):
    m = {}
    m["x"] = np.ascontiguousarray(inp["x"][b, :S])
    m["w_in"] = inp["w_in"][0]
    b_in = inp["b_in"][0]
    b_fm = np.zeros((128, NPF + 1), np.float32)
    for (kind, col0, ncols, dl, pfc) in fm_jobs():
        b_fm[:ncols, pfc] = b_in[col0:col0 + ncols]
    cols = list(range(1792, 2048)) + list(range(2304, 2560))
    for gi in range(3):
        c0 = 2584 + gi * 1536 + 1024
        cols += list(range(c0, c0 + 512))
    m["b_fm"] = b_fm
    m["b_tm"] = np.ascontiguousarray(b_in[cols][None, :])
    m.update(consts if consts is not None else host_consts(S))
    for k in ("cmp_k_w1", "cmp_v_w1", "cmp_k_w2", "cmp_v_w2"):
        m[k] = inp[k][0]
    m["posT_k"] = np.ascontiguousarray(inp["cmp_pos_k"][0].T)
    m["posT_v"] = np.ascontiguousarray(inp["cmp_pos_v"][0].T)
    m["w_br_nsa"] = inp["w_br_nsa"][0]
    m["w_br_dil"] = inp["w_br_dil"][0]
    m["w_out"] = inp["w_out"][0]
    m["ln_gb"] = np.ascontiguousarray(np.stack([inp["ln1_g"][0], inp["ln1_b"][0], inp["ln2_g"][0], inp["ln2_b"][0]], 0))
    m["w_router"] = inp["w_router"][0]
    m["b_router"] = inp["b_router"]
    esl = slice(b * (NE // 8), (b + 1) * (NE // 8)) if ep else slice(0, NE)
    nel = NE // 8 if ep else NE
    m["w_up"] = inp["w_up"][0][esl]
    m["w_down"] = inp["w_down"][0][esl]
    m["b_up_fm"] = np.ascontiguousarray(inp["b_up"][0][esl].reshape(nel, 32, 128).transpose(2, 0, 1).reshape(128, nel * 32))
    m["b_down"] = inp["b_down"][0][esl]
    if ep:
        cap = CAPB * 128
        nrow = NE * cap
        e_ = np.arange(NE)
        m["ebase_y"] = np.tile(((e_ // 4) * nrow + b * 4 * cap + (e_ % 4) * cap).astype(np.float32)[None, :], (128, 1))
        ex = np.arange(NE)
        le, sc = ex // 8, ex % 8
        blk = np.arange(CAPB)
        base = (sc[:, None] * nrow + (b * 4 + le[:, None]) * cap + blk[None, :] * 128).reshape(-1)
        m["xg_idx"] = np.ascontiguousarray((base[None, :] + np.arange(128)[:, None]).astype(np.int32))
    return m


def kernel(**inputs):
    S = 4096
    inp = {k: np.asarray(v) for k, v in inputs.items()}
    consts = host_consts(S)
    nc = build(S, ep=False)
    in_maps = [prep_inputs(inp, b, S, consts, ep=False) for b in range(8)]
    res = run_bass_kernel_spmd(nc, in_maps, core_ids=list(range(8)))
    return np.stack([np.asarray(r["out"], dtype=np.float32) for r in res.results], 0)
```
